# Optimizing a Trainium2 kernel written in Bass

```python
import jax, jax.numpy as jnp
from jax import lax
import numpy as np

D_MODEL = 1024
BATCH = 4
SEQ = 8192
DEPTH = 2

N_EVEN = (DEPTH + 1) // 2
N_ODD = DEPTH // 2
BLOCK = 128
NORM_EPS = 1e-6
ADA_CHUNKS = 6

A_HEADS = 8
A_KV_HEADS = 2
A_GROUP = A_HEADS // A_KV_HEADS
A_HEAD_DIM = 64
A_WINDOW = 128
A_Q = A_HEADS * A_HEAD_DIM
A_KV = A_KV_HEADS * A_HEAD_DIM

B_GROUPS = 8
B_GROUP_DIM = 64
B_WIDTH = B_GROUPS * B_GROUP_DIM
B_CHUNK = 128

C_GROUPS = 8
C_GROUP_DIM = 64
C_WIDTH = C_GROUPS * C_GROUP_DIM
C_CONV = 3

D_HEADS = 8
D_NOPE = 64
D_ROPE = 32
D_VDIM = 64
D_Q_RANK = 512
D_KV_RANK = 256
ROPE_THETA = 10000.0

EVEN_IN = A_Q + 2 * A_KV + 2 * B_WIDTH
EVEN_MIX = A_Q + B_WIDTH
ODD_IN = 3 * C_WIDTH + D_Q_RANK + D_KV_RANK + D_ROPE
ODD_MIX = C_WIDTH + D_HEADS * D_VDIM

FF_DENSE = 2816
N_EXPERTS = 8
TOP_K = 2
FF_EXPERT = 3584
MOE_BLOCK = 512

kernel_name = 'hybrid_swa_gmlp_conv_mla_moe_block'


def rmsnorm(x, g):
    xf = x.astype(jnp.float32)
    r = lax.rsqrt(jnp.mean(xf * xf, axis=-1, keepdims=True) + NORM_EPS)
    return (xf * r).astype(x.dtype) * g


def modulate(x, g, shift, scale):
    return rmsnorm(x, g) * (1.0 + scale[:, None, :]) + shift[:, None, :]


def alibi_slopes(n):
    return 2.0 ** (-8.0 * jnp.arange(1, n + 1, dtype=jnp.float32) / n)


def swa_sink_attention(q, k, v, sinks):
    Bn, S = q.shape[0], q.shape[1]
    nb = S // BLOCK
    qb = q.reshape(Bn, nb, BLOCK, A_KV_HEADS, A_GROUP, A_HEAD_DIM)
    kb = k.reshape(Bn, nb, BLOCK, A_KV_HEADS, A_HEAD_DIM)
    vb = v.reshape(Bn, nb, BLOCK, A_KV_HEADS, A_HEAD_DIM)
    kband = jnp.concatenate([jnp.concatenate([jnp.zeros_like(kb[:, :1]), kb[:, :-1]], axis=1), kb], axis=2)
    vband = jnp.concatenate([jnp.concatenate([jnp.zeros_like(vb[:, :1]), vb[:, :-1]], axis=1), vb], axis=2)
    scores = jnp.einsum('bnqhgd,bnshd->bnhgqs', qb, kband).astype(jnp.float32) * (A_HEAD_DIM ** -0.5)
    qi = jnp.arange(BLOCK)[:, None]
    kj = jnp.arange(2 * BLOCK)[None, :]
    dist = qi + BLOCK - kj
    valid = (dist >= 0) & (dist < A_WINDOW)
    valid = valid[None] & ((jnp.arange(nb)[:, None, None] > 0) | (kj[None] >= BLOCK))
    slopes = alibi_slopes(A_HEADS).reshape(A_KV_HEADS, A_GROUP)
    scores = scores - slopes[:, :, None, None] * dist.astype(jnp.float32)
    scores = jnp.where(valid[None, :, None, None], scores, -jnp.inf)
    sink = sinks.astype(jnp.float32).reshape(A_KV_HEADS, A_GROUP)[None, None, :, :, None, None]
    m = jnp.maximum(jnp.max(scores, axis=-1, keepdims=True), sink)
    p = jnp.exp(scores - m)
    p = p / (jnp.sum(p, axis=-1, keepdims=True) + jnp.exp(sink - m))
    out = jnp.einsum('bnhgqs,bnshd->bnqhgd', p.astype(v.dtype), vband)
    return out.reshape(Bn, S, A_Q)


def chunk_spatial_gate(u, v, ln_g, ln_b, w_s, b_s):
    Bn, S, _ = v.shape
    vf = v.astype(jnp.float32)
    mu = jnp.mean(vf, axis=-1, keepdims=True)
    var = jnp.mean((vf - mu) ** 2, axis=-1, keepdims=True)
    vn = ((vf - mu) * lax.rsqrt(var + NORM_EPS)).astype(v.dtype) * ln_g + ln_b
    nc = S // B_CHUNK
    vc = vn.reshape(Bn, nc, B_CHUNK, B_GROUPS, B_GROUP_DIM)
    causal = jnp.tril(jnp.ones((B_CHUNK, B_CHUNK), dtype=bool))
    w = jnp.where(causal[None], w_s, 0.0).astype(v.dtype)
    s = jnp.einsum('gts,bnsgc->bntgc', w, vc) + b_s.T[None, None, :, :, None]
    return u * s.reshape(Bn, S, B_WIDTH)


def short_conv_mixer(gate_b, gate_c, xin, conv_w):
    S = xin.shape[1]
    z = gate_c * xin
    zp = jnp.pad(z, ((0, 0), (C_CONV - 1, 0), (0, 0)))
    y = sum(conv_w[j] * zp[:, j:j + S] for j in range(C_CONV))
    return gate_b * y


def apply_rope(x, cos, sin):
    half = x.shape[-1] // 2
    x1, x2 = x[..., :half], x[..., half:]
    cos = cos.astype(x.dtype)
    sin = sin.astype(x.dtype)
    return jnp.concatenate([x1 * cos - x2 * sin, x1 * sin + x2 * cos], axis=-1)


def mla_attention(q_lat, kv_lat, k_rope, q_norm_g, w_q_b, kv_norm_g, w_kv_b):
    Bn, S, _ = q_lat.shape
    q = (rmsnorm(q_lat, q_norm_g) @ w_q_b).reshape(Bn, S, D_HEADS, D_NOPE + D_ROPE)
    kv = (rmsnorm(kv_lat, kv_norm_g) @ w_kv_b).reshape(Bn, S, D_HEADS, D_NOPE + D_VDIM)
    q_nope, q_pe = q[..., :D_NOPE], q[..., D_NOPE:]
    k_nope, v = kv[..., :D_NOPE], kv[..., D_NOPE:]
    inv = ROPE_THETA ** (-jnp.arange(0, D_ROPE, 2, dtype=jnp.float32) / D_ROPE)
    ang = jnp.arange(S, dtype=jnp.float32)[:, None] * inv[None, :]
    cos, sin = jnp.cos(ang), jnp.sin(ang)
    q_pe = apply_rope(q_pe, cos[:, None, :], sin[:, None, :])
    k_pe = apply_rope(k_rope, cos, sin)
    scale = (D_NOPE + D_ROPE) ** -0.5
    nb = S // BLOCK
    qn_b = q_nope.reshape(Bn, nb, BLOCK, D_HEADS, D_NOPE).transpose(1, 0, 2, 3, 4)
    qp_b = q_pe.reshape(Bn, nb, BLOCK, D_HEADS, D_ROPE).transpose(1, 0, 2, 3, 4)
    kpos = jnp.arange(S)

    def one_block(args):
        qn, qp, blk = args
        s = (jnp.einsum('bqhd,bshd->bhqs', qn, k_nope)
             + jnp.einsum('bqhr,bsr->bhqs', qp, k_pe)).astype(jnp.float32) * scale
        qpos = blk * BLOCK + jnp.arange(BLOCK)
        s = jnp.where(kpos[None, :] <= qpos[:, None], s, -jnp.inf)
        p = jax.nn.softmax(s, axis=-1).astype(v.dtype)
        return jnp.einsum('bhqs,bshd->bqhd', p, v)

    out = lax.map(one_block, (qn_b, qp_b, jnp.arange(nb)))
    return out.transpose(1, 0, 2, 3, 4).reshape(Bn, S, D_HEADS * D_VDIM)


def even_mixer(h, w_in, sinks, ln_g, ln_b, w_s, b_s, w_o):
    Bn, S, _ = h.shape
    proj = h @ w_in
    q, k, v, u, g = jnp.split(proj, [A_Q, A_Q + A_KV, A_Q + 2 * A_KV, A_Q + 2 * A_KV + B_WIDTH], axis=-1)
    a_out = swa_sink_attention(q.reshape(Bn, S, A_HEADS, A_HEAD_DIM),
                               k.reshape(Bn, S, A_KV_HEADS, A_HEAD_DIM),
                               v.reshape(Bn, S, A_KV_HEADS, A_HEAD_DIM), sinks)
    b_out = chunk_spatial_gate(jax.nn.gelu(u, approximate=False), jax.nn.gelu(g, approximate=False),
                               ln_g, ln_b, w_s, b_s)
    return jnp.concatenate([a_out, b_out], axis=-1) @ w_o


def odd_mixer(h, w_in, conv_w, q_norm_g, w_q_b, kv_norm_g, w_kv_b, w_o):
    proj = h @ w_in
    o1 = 3 * C_WIDTH
    gb, gc, xc, q_lat, kv_lat, k_rope = jnp.split(
        proj, [C_WIDTH, 2 * C_WIDTH, o1, o1 + D_Q_RANK, o1 + D_Q_RANK + D_KV_RANK], axis=-1)
    c_out = short_conv_mixer(gb, gc, xc, conv_w)
    d_out = mla_attention(q_lat, kv_lat, k_rope, q_norm_g, w_q_b, kv_norm_g, w_kv_b)
    return jnp.concatenate([c_out, d_out], axis=-1) @ w_o


def swiglu(h, w_gate, w_up, w_down):
    return (jax.nn.silu(h @ w_gate) * (h @ w_up)) @ w_down


def moe_swiglu(h, w_router, b_router, w_gate, w_up, w_down):
    Bn, S, Dm = h.shape
    xt = h.reshape(-1, Dm)
    N = xt.shape[0]
    NA = N * TOP_K
    logits = (xt @ w_router).astype(jnp.float32) + b_router.astype(jnp.float32)
    top_logit, top_e = lax.top_k(logits, TOP_K)
    gates = jax.nn.softmax(top_logit, axis=-1).astype(h.dtype)
    flat_e = top_e.reshape(-1).astype(jnp.int32)
    order = jnp.argsort(flat_e, stable=True)
    sorted_e = flat_e[order]
    counts = jnp.bincount(flat_e, length=N_EXPERTS).astype(jnp.int32)
    padded = (counts + MOE_BLOCK - 1) // MOE_BLOCK * MOE_BLOCK
    pad_end = jnp.cumsum(padded)
    pad_start = pad_end - padded
    grp_start = jnp.cumsum(counts) - counts
    dest_sorted = pad_start[sorted_e] + jnp.arange(NA, dtype=jnp.int32) - grp_start[sorted_e]
    dest = jnp.zeros((NA,), jnp.int32).at[order].set(dest_sorted)
    n_blk = -(-NA // MOE_BLOCK) + N_EXPERTS
    L = n_blk * MOE_BLOCK
    tok = jnp.arange(NA, dtype=jnp.int32) // TOP_K
    slot_tok = jnp.zeros((L,), jnp.int32).at[dest].set(tok)
    xs = xt[slot_tok].reshape(n_blk, MOE_BLOCK, Dm)
    blk_e = jnp.minimum(jnp.searchsorted(pad_end, jnp.arange(n_blk, dtype=jnp.int32) * MOE_BLOCK, side='right'),
                        N_EXPERTS - 1)

    def expert_block(args):
        xb, e = args
        return (jax.nn.silu(xb @ w_gate[e]) * (xb @ w_up[e])) @ w_down[e]

    ys = lax.map(expert_block, (xs, blk_e)).reshape(L, Dm)
    y = ys[dest].reshape(N, TOP_K, Dm)
    out = jnp.einsum('nk,nkd->nd', gates, y)
    return out.reshape(Bn, S, Dm)


def setup_inputs(seed: int = 0) -> dict:
    key = jax.random.key(seed)
    ks = iter(jax.random.split(key, 40))
    D = D_MODEL
    E_, O_ = N_EVEN, N_ODD

    def nrm(shape, scale):
        return jax.random.normal(next(ks), shape, jnp.float32) * scale

    def gain(shape):
        return 1.0 + nrm(shape, 0.02)

    return {
        'x': nrm((BATCH, SEQ, D), 1.0),
        'c': nrm((BATCH, D), 1.0),
        'even_ada_w': nrm((E_, D, ADA_CHUNKS * D), 0.5 * D ** -0.5),
        'even_ada_b': nrm((E_, ADA_CHUNKS * D), 0.01),
        'even_norm_mix_g': gain((E_, D)),
        'even_w_in': nrm((E_, D, EVEN_IN), D ** -0.5),
        'even_sinks': nrm((E_, A_HEADS), 0.5),
        'even_gmlp_ln_g': gain((E_, B_WIDTH)),
        'even_gmlp_ln_b': nrm((E_, B_WIDTH), 0.02),
        'even_w_s': nrm((E_, B_GROUPS, B_CHUNK, B_CHUNK), B_CHUNK ** -0.5),
        'even_b_s': 1.0 + nrm((E_, B_GROUPS, B_CHUNK), 0.1),
        'even_w_o': nrm((E_, EVEN_MIX, D), EVEN_MIX ** -0.5),
        'even_norm_ffn_g': gain((E_, D)),
        'even_ffn_w_gate': nrm((E_, D, FF_DENSE), D ** -0.5),
        'even_ffn_w_up': nrm((E_, D, FF_DENSE), D ** -0.5),
        'even_ffn_w_down': nrm((E_, FF_DENSE, D), FF_DENSE ** -0.5),
        'odd_ada_w': nrm((O_, D, ADA_CHUNKS * D), 0.5 * D ** -0.5),
        'odd_ada_b': nrm((O_, ADA_CHUNKS * D), 0.01),
        'odd_norm_mix_g': gain((O_, D)),
        'odd_w_in': nrm((O_, D, ODD_IN), D ** -0.5),
        'odd_conv_w': nrm((O_, C_CONV, C_WIDTH), C_CONV ** -0.5),
        'odd_q_norm_g': gain((O_, D_Q_RANK)),
        'odd_w_q_b': nrm((O_, D_Q_RANK, D_HEADS * (D_NOPE + D_ROPE)), D_Q_RANK ** -0.5),
        'odd_kv_norm_g': gain((O_, D_KV_RANK)),
        'odd_w_kv_b': nrm((O_, D_KV_RANK, D_HEADS * (D_NOPE + D_VDIM)), D_KV_RANK ** -0.5),
        'odd_w_o': nrm((O_, ODD_MIX, D), ODD_MIX ** -0.5),
        'odd_norm_ffn_g': gain((O_, D)),
        'odd_router_w': nrm((O_, D, N_EXPERTS), D ** -0.5),
        'odd_router_b': nrm((O_, N_EXPERTS), 0.01),
        'odd_exp_w_gate': nrm((O_, N_EXPERTS, D, FF_EXPERT), D ** -0.5),
        'odd_exp_w_up': nrm((O_, N_EXPERTS, D, FF_EXPERT), D ** -0.5),
        'odd_exp_w_down': nrm((O_, N_EXPERTS, FF_EXPERT, D), FF_EXPERT ** -0.5),
        'final_norm_g': gain((D,)),
    }


def reference(x, c, even_ada_w, even_ada_b, even_norm_mix_g, even_w_in, even_sinks, even_gmlp_ln_g,
              even_gmlp_ln_b, even_w_s, even_b_s, even_w_o, even_norm_ffn_g, even_ffn_w_gate, even_ffn_w_up,
              even_ffn_w_down, odd_ada_w, odd_ada_b, odd_norm_mix_g, odd_w_in, odd_conv_w, odd_q_norm_g,
              odd_w_q_b, odd_kv_norm_g, odd_w_kv_b, odd_w_o, odd_norm_ffn_g, odd_router_w, odd_router_b,
              odd_exp_w_gate, odd_exp_w_up, odd_exp_w_down, final_norm_g):
    cond = jax.nn.silu(c)
    h = x
    for layer in range(DEPTH):
        i = layer // 2
        if layer % 2 == 0:
            mod = cond @ even_ada_w[i] + even_ada_b[i]
            sh1, sc1, g1, sh2, sc2, g2 = jnp.split(mod, ADA_CHUNKS, axis=-1)
            hn = modulate(h, even_norm_mix_g[i], sh1, sc1)
            h = h + g1[:, None, :] * even_mixer(hn, even_w_in[i], even_sinks[i], even_gmlp_ln_g[i],
                                                even_gmlp_ln_b[i], even_w_s[i], even_b_s[i], even_w_o[i])
            hn = modulate(h, even_norm_ffn_g[i], sh2, sc2)
            h = h + g2[:, None, :] * swiglu(hn, even_ffn_w_gate[i], even_ffn_w_up[i], even_ffn_w_down[i])
        else:
            mod = cond @ odd_ada_w[i] + odd_ada_b[i]
            sh1, sc1, g1, sh2, sc2, g2 = jnp.split(mod, ADA_CHUNKS, axis=-1)
            hn = modulate(h, odd_norm_mix_g[i], sh1, sc1)
            h = h + g1[:, None, :] * odd_mixer(hn, odd_w_in[i], odd_conv_w[i], odd_q_norm_g[i], odd_w_q_b[i],
                                               odd_kv_norm_g[i], odd_w_kv_b[i], odd_w_o[i])
            hn = modulate(h, odd_norm_ffn_g[i], sh2, sc2)
            h = h + g2[:, None, :] * moe_swiglu(hn, odd_router_w[i], odd_router_b[i], odd_exp_w_gate[i],
                                                odd_exp_w_up[i], odd_exp_w_down[i])
    return rmsnorm(h, final_norm_g)
```

```python
import contextlib
import numpy as np
import concourse.bass as bass
import concourse.mybir as mybir
from concourse.bass_utils import run_bass_kernel_spmd

F32 = mybir.dt.float32
BF16 = mybir.dt.bfloat16
AF = mybir.ActivationFunctionType
ALU = mybir.AluOpType
AX = mybir.AxisListType

NDMA_SLOTS = 8
D = 1024
S = 8192
TB = 512
NPOS = 16
NOWN = 8
EPS = 1e-6
OWN = ([0, 3, 4, 7, 8, 11, 12, 15], [1, 2, 5, 6, 9, 10, 13, 14])
FF0 = 2816
FFE = 3584
NEXP = 8
NEG = -30000.0


class Buf:
    __slots__ = ("lw", "rd")

    def __init__(self):
        self.lw = None
        self.rd = {}


class Prog:
    ENG = ("pe", "act", "dve", "pool", "sp")

    def __init__(self, nc):
        self.nc = nc
        self.ops = {e: [] for e in self.ENG}
        self.cnt = {e: 0 for e in self.ENG}
        self.waited = {e: {} for e in self.ENG}
        self.dma_n = {e: 0 for e in self.ENG}
        self.last_dma = {}

    def _need(self, eng, tok, waits):
        if tok is None:
            return
        key, val, src = tok
        if src == eng and key[0] == "c" and eng == "pe":
            return
        w = self.waited[eng]
        if w.get(key, 0) >= val:
            return
        w[key] = val
        waits.append((key, val))

    def _deps(self, eng, reads, writes):
        waits = []
        for b in reads:
            self._need(eng, b.lw, waits)
        for b in writes:
            self._need(eng, b.lw, waits)
            for t in b.rd.values():
                self._need(eng, t, waits)
        m = {}
        for k, v in waits:
            m[k] = max(m.get(k, 0), v)
        return list(m.items())

    def _mark(self, tok, reads, writes):
        for b in reads:
            o = b.rd.get(tok[0])
            if o is None or o[1] < tok[1]:
                b.rd[tok[0]] = tok
        for b in writes:
            b.lw = tok
            b.rd = {}

    def op(self, eng, fn, reads=(), writes=()):
        waits = self._deps(eng, reads, writes)
        self.cnt[eng] += 1
        key = "c:" + eng
        tok = (key, self.cnt[eng], eng)
        self.ops[eng].append((waits, fn, (key, 1)))
        self._mark(tok, reads, writes)
        return tok

    def dma(self, eng, out, in_, reads=(), writes=(), **kw):
        n = self.dma_n[eng]
        self.dma_n[eng] += 1
        slot = n % NDMA_SLOTS
        key = "d:%s:%d" % (eng, slot)
        val = 16 * (n // NDMA_SLOTS + 1)
        waits = self._deps(eng, reads, writes)
        if n >= NDMA_SLOTS:
            pv = val - 16
            if self.waited[eng].get(key, 0) < pv:
                self.waited[eng][key] = pv
                waits.append((key, pv))
        tok = (key, val, eng)
        self.last_dma[key] = tok

        def fn(e, out=out, in_=in_, kw=kw):
            return e.dma_start(out=out, in_=in_, **kw)
        self.ops[eng].append((waits, fn, (key, 16)))
        self._mark(tok, reads, writes)
        return tok

    def idma(self, fn, reads=(), writes=()):
        eng = "pool"
        n = self.dma_n[eng]
        self.dma_n[eng] += 1
        slot = n % NDMA_SLOTS
        key = "d:%s:%d" % (eng, slot)
        val = 16 * (n // NDMA_SLOTS + 1)
        waits = self._deps(eng, reads, writes)
        if n >= NDMA_SLOTS:
            pv = val - 16
            if self.waited[eng].get(key, 0) < pv:
                self.waited[eng][key] = pv
                waits.append((key, pv))
        tok = (key, val, eng)
        self.last_dma[key] = tok
        self.ops[eng].append((waits, fn, (key, 16)))
        self._mark(tok, reads, writes)
        return tok

    def wait_tok(self, eng, tok):
        waits = []
        self._need(eng, tok, waits)
        if waits:
            self.ops[eng].append((waits, None, None))

    def barrier(self):
        toks = [("c:" + e, self.cnt[e], e) for e in self.ENG if self.cnt[e] > 0]
        toks += list(self.last_dma.values())
        for e in self.ENG:
            for t in toks:
                self.wait_tok(e, t)

    def emit(self):
        nc = self.nc
        keys = set()
        for e in self.ENG:
            for waits, fn, inc in self.ops[e]:
                for k, _ in waits:
                    keys.add(k)
                if inc is not None:
                    keys.add(inc[0])
        sems = {}
        with contextlib.ExitStack() as st:
            for k in sorted(keys):
                sems[k] = st.enter_context(nc.semaphore(k.replace(":", "_")))
            block = st.enter_context(nc.Block())

            def run(e, lst):
                for waits, fn, inc in lst:
                    for k, v in waits:
                        e.wait_ge(sems[k], v)
                    if fn is not None:
                        ins = fn(e)
                        ins.then_inc(sems[inc[0]], inc[1])

            @block.tensor
            def _(e):
                run(e, self.ops["pe"])

            @block.scalar
            def _(e):
                run(e, self.ops["act"])

            @block.vector
            def _(e):
                run(e, self.ops["dve"])

            @block.gpsimd
            def _(e):
                run(e, self.ops["pool"])

            @block.sync
            def _(e):
                run(e, self.ops["sp"])


class Tl:
    __slots__ = ("t", "b")

    def __init__(self, t):
        self.t = t
        self.b = Buf()

    def __getitem__(self, k):
        return self.t[k]


def build(dbg=False):
    nc = bass.Bass("TRN2", target_bir_lowering=False)
    p = Prog(nc)

    def din(name, shape):
        return nc.dram_tensor(name, list(shape), F32, kind="ExternalInput").ap()

    def dscr(name, shape, dt):
        return nc.dram_tensor(name, list(shape), dt).ap()

    xm = din("xm", [NPOS * TB, D])
    xh = din("xh", [NPOS * 128, D])
    xh2 = din("xh2", [NPOS * 128, D])
    c_pc = din("c_pc", [128, 8])
    vecs = din("vecs", [128, 160])
    fl_swa_d = din("fl_swa", [128, NPOS])
    fl_conv_d = din("fl_conv", [128, NPOS])
    fl_oth_d = din("fl_oth", [128, NOWN])
    ropek_d = din("ropek", [2, 32, S])
    ropeq_d = din("ropeq", [2, 32, NOWN * TB])
    swab_d = din("swab", [128, 2 * 8 * 128])
    mlam_d = din("mlam", [128, 4 * TB])
    esk_d = din("esk_sink", [128, 4])
    lng_d = din("lng", [1, 512])
    lnb_d = din("lnb", [1, 512])
    bsb_d = din("bsb", [128, 4 * 128])
    brt_d = din("brt", [1, 8])
    ada_w = [din("ada_w0", [D, 6 * D]), din("ada_w1", [D, 6 * D])]
    w_in0 = din("w_in0", [D, 1792])
    w_s = din("w_s", [8, 128, 128])
    w_o0 = din("w_o0", [D, D])
    wg0 = din("wg0", [D, FF0])
    wu0 = din("wu0", [D, FF0])
    wd0 = din("wd0", [FF0, D])
    w_in1 = din("w_in1", [D, 2336])
    w_qb = din("w_qb", [512, 768])
    w_kvb = din("w_kvb", [256, 1024])
    w_o1 = din("w_o1", [D, D])
    w_rt = din("w_rt", [D, 8])
    NWR = NEXP * 7 * 128
    ewg_h = [nc.dram_tensor("ewg_h%d" % h, [NWR, 2048], F32, kind="ExternalInput") for h in range(2)]
    ewu_h = [nc.dram_tensor("ewu_h%d" % h, [NWR, 2048], F32, kind="ExternalInput") for h in range(2)]
    ewd_h = [nc.dram_tensor("ewd_h%d" % h, [NWR, 2048], F32, kind="ExternalInput") for h in range(2)]
    cst_d = din("cst", [128, 32])
    fgrow_d = din("fgrow", [1, D])
    y_out = nc.dram_tensor("y", [NOWN * TB, D], F32, kind="ExternalOutput").ap()

    NCOL = NPOS * TB + NPOS * 128
    hT_d = dscr("hT_d", [D, NCOL], F32)
    cT_d = dscr("cT_d", [512, NPOS * TB], BF16)
    qT_d = dscr("qT_d", [8, 96, NOWN * TB], BF16)
    knT_d = dscr("knT_d", [512, S], BF16)
    kpeT_d = dscr("kpeT_d", [32, S], BF16)
    v_d = dscr("v_d", [S, 512], BF16)
    dT_d = dscr("dT_d", [512, NOWN * TB], BF16)
    NBLK = 23
    LSLOT = NBLK * TB
    ew_src = {"g": ewg_h, "u": ewu_h, "d": ewd_h}
    ew_bf = {k: [nc.dram_tensor("ew%s_b%d" % (k, h), [NWR, 2048], BF16) for h in range(2)] for k in "gud"}
    cv_jobs = [(k, h, r) for r in range(7) for k in "gud" for h in range(2)]
    b_cv = [Buf() for _ in cv_jobs]
    cv_next = [0]

    def emit_conversions(n):
        for _ in range(n):
            if cv_next[0] >= len(cv_jobs):
                return
            j = cv_next[0]
            cv_next[0] += 1
            k, h, r = cv_jobs[j]
            p.dma("pool", ew_bf[k][h].ap()[r * 1024:(r + 1) * 1024, :], ew_src[k][h].ap()[r * 1024:(r + 1) * 1024, :],
                  [], [b_cv[j]])
    h3tok_d = dscr("h3tok_d", [NOWN * TB, D], F32)
    hn3tok_d = dscr("hn3tok_d", [NOWN * TB, D], F32)
    xs_h = nc.dram_tensor("xs_d", [LSLOT, D], F32)
    ys_h = nc.dram_tensor("ys_d", [LSLOT, D], F32)
    xs_d = xs_h.ap()
    ys_d = ys_h.ap()
    g2row_d = dscr("g2row_d", [D], F32)
    b_xs = Buf(); b_ys = [Buf() for _ in range(NBLK)]; b_g2row = Buf()
    b_h3t = [Buf() for _ in range(32)]; b_hn3t = [Buf() for _ in range(32)]
    b_hT = [Buf() for _ in range(NPOS + 4)]
    b_cT = [Buf() for _ in range(NPOS)]
    b_q = Buf(); b_kn = Buf(); b_kpe = Buf(); b_v = Buf()
    b_dT = [Buf() for _ in range(NOWN)]
    dbg_out = {}
    if dbg:
        dbg_out["dbg_h"] = nc.dram_tensor("dbg_h", [D, NCOL], F32, kind="ExternalOutput").ap()

    def bl(xs):
        return [x.b if isinstance(x, Tl) else x for x in xs]

    def MM(out, lhsT, rhs, start, stop, R, W):
        p.op("pe", lambda e: e.matmul(out, lhsT=lhsT, rhs=rhs, start=start, stop=stop), bl(R), bl(W))

    def TR(out, in_, ident, R, W):
        p.op("pe", lambda e: e.transpose(out, in_, ident), bl(R), bl(W))

    def ACT(out, in_, func, R, W, **kw):
        p.op("act", lambda e: e.activation(out=out, in_=in_, func=func, **kw), bl(R), bl(W))

    def TT(eng, out, in0, in1, op, R, W):
        p.op(eng, lambda e: e.tensor_tensor(out=out, in0=in0, in1=in1, op=op), bl(R), bl(W))

    def TS(eng, out, in0, s1, op0, R, W, s2=None, op1=None):
        if op1 is None:
            p.op(eng, lambda e: e.tensor_scalar(out=out, in0=in0, scalar1=s1, scalar2=None, op0=op0), bl(R), bl(W))
        else:
            p.op(eng, lambda e: e.tensor_scalar(out=out, in0=in0, scalar1=s1, scalar2=s2, op0=op0, op1=op1), bl(R), bl(W))

    def STT(eng, out, in0, scalar, in1, op0, op1, R, W):
        p.op(eng, lambda e: e.scalar_tensor_tensor(out=out, in0=in0, scalar=scalar, in1=in1, op0=op0, op1=op1),
             bl(R), bl(W))

    def CP(eng, out, in_, R, W):
        if eng == "act":
            ACT(out, in_, AF.Copy, R, W)
        else:
            p.op(eng, lambda e: e.tensor_copy(out=out, in_=in_), bl(R), bl(W))

    def RCP(out, in_, R, W):
        p.op("dve", lambda e: e.reciprocal(out=out, in_=in_), bl(R), bl(W))

    def RED(out, in_, op, R, W):
        p.op("dve", lambda e: e.tensor_reduce(out=out, in_=in_, axis=AX.X, op=op), bl(R), bl(W))

    def MEMSET(eng, ap, val, W):
        p.op(eng, lambda e: e.memset(ap, val), (), bl(W))

    def DMA(q, out, in_, R, W, **kw):
        return p.dma(q, out, in_, bl(R), bl(W), **kw)

    evac_rr = [0]

    def EVAC(out, in_, R, W):
        evac_rr[0] ^= 1
        CP("act" if evac_rr[0] else "dve", out, in_, R, W)

    with contextlib.ExitStack() as top:
        uid = [0]

        def sbt(st, name, shape, dt):
            uid[0] += 1
            return Tl(st.enter_context(nc.sbuf_tensor("s%d_%s" % (uid[0], name), list(shape), dt)))

        pb = [Tl(top.enter_context(nc.psum_tensor("pb%d" % i, [128, 512], F32))) for i in range(8)]
        acc_rr = [0]

        def nacc():
            acc_rr[0] ^= 1
            return pb[acc_rr[0]]

        ident = sbt(top, "ident", [128, 128], F32)
        ones_f = sbt(top, "ones_f", [128, 128], F32)
        ones_b = sbt(top, "ones_b", [128, 128], BF16)
        vec = sbt(top, "vec", [128, 160], F32)
        modT = [sbt(top, "modT%d" % l, [128, 48], F32) for l in range(2)]
        gsc = sbt(top, "gsc", [128, 32], F32)
        flsw = sbt(top, "flsw", [128, NPOS], F32)
        flcv = sbt(top, "flcv", [128, NPOS], F32)
        flot = sbt(top, "flot", [128, NOWN], F32)
        hcomp = sbt(top, "hcomp", [128, 8, 32], F32)
        MEMSET("pool", ident[:], 0.0, [ident])
        p.op("pool", lambda e: e.affine_select(out=ident[:], in_=ident[:], pattern=[[-1, 128]],
                                               compare_op=ALU.not_equal, fill=1.0, base=0, channel_multiplier=1),
             (), [ident.b])
        MEMSET("pool", ones_f[:], 1.0, [ones_f])
        MEMSET("pool", ones_b[:], 1.0, [ones_b])
        DMA("sp", vec[:], vecs, [], [vec])
        DMA("sp", flsw[:], fl_swa_d, [], [flsw])
        DMA("sp", flcv[:], fl_conv_d, [], [flcv])
        DMA("sp", flot[:], fl_oth_d, [], [flot])
        V_G = [0, 8, 16, 24]
        V_FG = 32
        V_AB = [40, 88]
        V_QG = 136
        V_KG = 140
        V_CW = 142

        with contextlib.ExitStack() as st:
            cT = sbt(st, "cT", [128, 8], F32)
            condT = sbt(st, "condT", [128, 8], F32)
            awp = [sbt(st, "awp%d" % i, [128, 8, 512], F32) for i in range(2)]
            DMA("sp", cT[:], c_pc, [], [cT])
            ACT(condT[:], cT[:], AF.Silu, [cT], [condT])
            for l in range(2):
                mp = pb[2 + l]
                for nb in range(12):
                    a = awp[nb % 2]
                    DMA("sp", a[:], ada_w[l][:, nb * 512:(nb + 1) * 512].rearrange("(c p) f -> p c f", p=128), [], [a])
                    for k4 in range(4):
                        k = nb * 4 + k4
                        for c in range(8):
                            MM(mp[:, k:k + 1], a[:, c, k4 * 128:(k4 + 1) * 128], condT[:, c:c + 1], c == 0, c == 7,
                               [a, condT], [mp])
                TT("dve", modT[l][:], mp[:, 0:48], vec[:, V_AB[l]:V_AB[l] + 48], ALU.add, [mp, vec], [modT[l]])
            for n, (l, so) in enumerate([(0, 8), (0, 32), (1, 8), (1, 32)]):
                TS("dve", gsc[:, n * 8:(n + 1) * 8], modT[l][:, so:so + 8], 1.0, ALU.add, [modT[l]], [gsc])
                TT("dve", gsc[:, n * 8:(n + 1) * 8], gsc[:, n * 8:(n + 1) * 8], vec[:, V_G[n]:V_G[n] + 8], ALU.mult,
                   [vec], [gsc])
        p.barrier()

        def SH(l, which):
            o = 0 if which == 1 else 24
            return modT[l][:, o:o + 8]

        def GATE(l, which):
            o = 16 if which == 1 else 40
            return modT[l][:, o:o + 8]

        def norm_mod(src, KC, N, dfeat, gs_ap, sh_ap, dst, scr):
            sq, tmp, rr = scr
            stat = pb[7]
            for c in range(KC):
                s = sq[c % 2]
                ACT(s[:, :N], src[:, c, :], AF.Square, [src], [s])
                MM(stat[:, :N], ones_f[:], s[:, :N], c == 0, c == KC - 1, [ones_f, s], [stat])
            ACT(rr[:, :N], stat[:, :N], AF.Sqrt, [stat], [rr], bias=EPS_AP[:, 0:1], scale=1.0 / dfeat)
            RCP(rr[:, :N], rr[:, :N], [rr], [rr])
            for c in range(KC):
                t = tmp[c % 2]
                TT("dve", t[:, :N], src[:, c, :], rr[:, :N], ALU.mult, [src, rr], [t])
                if sh_ap is None:
                    ACT(dst[:, c, :], t[:, :N], AF.Identity, [t, gsc, vec], [dst], scale=gs_ap[:, c:c + 1],
                        bias=ZERO_AP[:, 0:1])
                else:
                    ACT(dst[:, c, :], t[:, :N], AF.Identity, [t, gsc, vec, modT[0], modT[1]], [dst],
                        scale=gs_ap[:, c:c + 1], bias=sh_ap[:, c:c + 1])

        epsT = sbt(top, "epsT", [128, 1], F32)
        MEMSET("pool", epsT[:], EPS, [epsT])
        EPS_AP = epsT.t
        zeroT = sbt(top, "zeroT", [128, 1], F32)
        MEMSET("pool", zeroT[:], 0.0, [zeroT])
        ZERO_AP = zeroT.t

        def load_hT(tile, blk, q="sp"):
            DMA(q, tile[:], hT_d[:, blk * TB:(blk + 1) * TB].rearrange("(c p) n -> p c n", p=128), [b_hT[blk]], [tile])

        def store_hT(tile, blk, q="sp"):
            DMA(q, hT_d[:, blk * TB:(blk + 1) * TB].rearrange("(c p) n -> p c n", p=128), tile[:], [tile], [b_hT[blk]])

        with contextlib.ExitStack() as st:
            Win0 = sbt(st, "Win0", [128, 8, 1792], BF16)
            Wo0 = sbt(st, "Wo0", [128, 8, D], BF16)
            WsT = sbt(st, "WsT", [128, 8, 128], BF16)
            biasT = sbt(st, "biasT", [128, 2, 8 * 128], F32)
            eskf = sbt(st, "eskf", [128, 4, 128], F32)
            esk = sbt(st, "esk", [128, 4], F32)
            lngb = sbt(st, "lngb", [128, 512], F32)
            lnbb = sbt(st, "lnbb", [128, 512], F32)
            bsb = sbt(st, "bsb", [128, 4, 128], F32)
            xt = sbt(st, "xt", [128, 4, D], F32)
            xT2 = None
            hnT2 = None
            sq = [sbt(st, "sq%d" % i, [128, TB], F32) for i in range(2)]
            tmpn = [sbt(st, "tmpn%d" % i, [128, TB], F32) for i in range(2)]
            rr = sbt(st, "rr", [128, TB], F32)
            scr = (sq, tmpn, rr)
            QT = sbt(st, "QT", [128, 4, TB], BF16)
            KTc = sbt(st, "KTc", [128, TB], BF16)
            Vc = sbt(st, "Vc", [128, 4, 128], BF16)
            KTh = sbt(st, "KTh", [128, NPOS, 128], BF16)
            Vh = sbt(st, "Vh", [128, NPOS, 128], BF16)
            KTh2 = sbt(st, "KTh2", [128, NPOS, 128], BF16)
            Vh2 = sbt(st, "Vh2", [128, NPOS, 128], BF16)
            uT = sbt(st, "uT", [128, 4, TB], BF16)
            gg = [sbt(st, "gg%d" % i, [128, 512], F32) for i in range(2)]
            vn = [sbt(st, "vn%d" % i, [128, 512], BF16) for i in range(2)]
            st8 = [sbt(st, "st8_%d" % i, [128, 8], F32) for i in range(2)]
            sstage = sbt(st, "sstage", [128, 4, TB], F32)
            scs = [sbt(st, "scs%d" % i, [128, 512], F32) for i in range(2)]
            PT = [[sbt(st, "PT%d%d" % (g, pc), [128, 512], BF16) for pc in range(2)] for g in range(2)]
            den1 = sbt(st, "den", [128, 512], F32)
            den = [den1, den1]

            dnb = [Tl(den1.t), Tl(den1.t)]
            mixT = sbt(st, "mixT", [128, 8, TB], BF16)

            Wq_v = Win0[:, :, 0:512].rearrange("p c (j g d) -> p c j g d", g=2, d=64)
            for g in range(2):
                for c in range(8):
                    DMA("pool", Wq_v[:, c, :, g, :],
                        w_in0[c * 128:(c + 1) * 128, g * 256:(g + 1) * 256].rearrange("p (j d) -> p j d", d=64),
                        [], [Win0])
            DMA("pool", Win0[:, :, 512:1792], w_in0[:, 512:1792].rearrange("(c p) f -> p c f", p=128), [], [Win0])
            for g in range(2):
                DMA("pool", Wo0[g * 64:(g + 1) * 64, 0:4, :],
                    w_o0[g * 256:(g + 1) * 256, :].rearrange("(j d) m -> d j m", d=64), [], [Wo0])
            DMA("pool", Wo0[:, 4:8, :], w_o0[512:1024, :].rearrange("(c p) m -> p c m", p=128), [], [Wo0])
            DMA("sp", biasT[:], swab_d.rearrange("p (a b) -> p a b", a=2), [], [biasT])
            DMA("sp", esk[:], esk_d, [], [esk])
            DMA("sp", lngb[:], lng_d.partition_broadcast(128), [], [lngb])
            DMA("sp", lnbb[:], lnb_d.partition_broadcast(128), [], [lnbb])
            DMA("sp", bsb[:], bsb_d.rearrange("p (a b) -> p a b", a=4), [], [bsb])
            st_setup = contextlib.ExitStack()
            tri = sbt(st_setup, "tri", [128, 128], F32)
            wsn = sbt(st_setup, "wsn", [128, 8, 128], F32)
            DMA("sp", wsn[:], w_s.rearrange("g t s -> t g s"), [], [wsn])
            MEMSET("pool", tri[:], 1.0, [tri])
            p.op("pool", lambda e: e.affine_select(out=tri[:], in_=tri[:], pattern=[[1, 128]],
                                                   compare_op=ALU.is_ge, fill=0.0, base=0, channel_multiplier=-1),
                 (), [tri.b])
            for g8 in range(8):
                a = nacc()
                TR(a[:, 0:128], wsn[:, g8, :], ident[:], [wsn, ident], [a])
                TT("dve", WsT[:, g8, :], a[:, 0:128], tri[:], ALU.mult, [a, tri], [WsT])
            ACT(esk[:], esk[:], AF.Exp, [esk], [esk])
            for j in range(4):
                TS("dve", eskf[:, j, :], ones_f[:], esk[:, j:j + 1], ALU.mult, [ones_f, esk], [eskf])

            p.barrier()
            st_setup.close()
            xts = [xt, xt]
            xT2 = [sbt(st, "xTa", [128, 8, TB], F32), sbt(st, "xTb", [128, 8, TB], F32)]
            hnT1 = sbt(st, "hnTa", [128, 8, TB], BF16)
            hnT2 = [hnT1, hnT1]
            gg4 = gg + [sbt(st, "gg%d" % i, [128, 512], F32) for i in (2, 3)]
            vn4 = vn + [sbt(st, "vn%d" % i, [128, 512], BF16) for i in (2, 3)]
            st84 = st8 + [sbt(st, "st8_%d" % i, [128, 8], F32) for i in (2, 3)]
            PT2 = [PT] + [[[sbt(st, "PT%s%d%d" % (k, g, pc), [128, 512], BF16) for pc in range(2)] for g in range(2)]
                          for k in "bcd"]
            scs4 = gg4

            def a1_stageA(xt, n):
                xT, hnT = xT2[n % 2], hnT2[n % 2]
                for c in range(8):
                    a = nacc()
                    for s in range(4):
                        TR(a[:, s * 128:(s + 1) * 128], xt[:, s, c * 128:(c + 1) * 128], ident[:], [xt, ident], [a])
                    EVAC(xT[:, c, :], a[:], [a], [xT])
                norm_mod(xT, 8, TB, float(D), gsc[:, 0:8], SH(0, 1), hnT, scr)

            def a1_stageB1(n, kind, idx):
                xT, hnT = xT2[n % 2], hnT2[n % 2]
                a = nacc()
                for c in range(8):
                    MM(a[:], Win0[:, c, 512:640], hnT[:, c, :], c == 0, c == 7, [Win0, hnT], [a])
                EVAC(KTc[:], a[:], [a], [KTc])
                a = nacc()
                for s in range(4):
                    for c in range(8):
                        MM(a[:, s * 128:(s + 1) * 128], hnT[:, c, s * 128:(s + 1) * 128], Win0[:, c, 640:768],
                           c == 0, c == 7, [Win0, hnT], [a])
                EVAC(Vc[:].rearrange("p s d -> p (s d)"), a[:], [a], [Vc])
                if kind == "h2":
                    CP("pool", KTh2[:, idx * 4:(idx + 1) * 4, :].rearrange("p s d -> p (s d)"), KTc[:], [KTc], [KTh2])
                    CP("pool", Vh2[:, idx * 4:(idx + 1) * 4, :], Vc[:], [Vc], [Vh2])
                    return
                if kind == "halo":
                    CP("pool", KTh[:, idx * 4:(idx + 1) * 4, :].rearrange("p s d -> p (s d)"), KTc[:], [KTc], [KTh])
                    CP("pool", Vh[:, idx * 4:(idx + 1) * 4, :], Vc[:], [Vc], [Vh])
                for s in range(4):
                    a = nacc()
                    for c in range(8):
                        MM(a[:], hnT[:, c, s * 128:(s + 1) * 128], Win0[:, c, 1280:1792], c == 0, c == 7,
                           [Win0, hnT], [a])
                    g_, s8, v_ = gg4[s], st84[s], vn4[s]
                    MEMSET("pool", s8[:, 0:2], 0.0, [s8])
                    ACT(g_[:], a[:], AF.Gelu, [a], [g_, s8], accum_out=s8[:, 0:1])
                    ACT(den1[:], g_[:], AF.Square, [g_], [den1, dnb[0], dnb[1], s8], accum_out=s8[:, 1:2])
                R4 = range(4)
                for s in R4:
                    TS("dve", st84[s][:, 2:3], st84[s][:, 0:1], 1.0 / 512, ALU.mult, [st84[s]], [st84[s]])
                for s in R4:
                    TT("dve", st84[s][:, 3:4], st84[s][:, 2:3], st84[s][:, 2:3], ALU.mult, [st84[s]], [st84[s]])
                for s in R4:
                    STT("dve", st84[s][:, 4:5], st84[s][:, 1:2], 1.0 / 512, st84[s][:, 3:4], ALU.mult, ALU.subtract,
                        [st84[s]], [st84[s]])
                for s in R4:
                    ACT(st84[s][:, 5:6], st84[s][:, 4:5], AF.Sqrt, [st84[s]], [st84[s]], bias=EPS_AP[:, 0:1], scale=1.0)
                for s in R4:
                    RCP(st84[s][:, 5:6], st84[s][:, 5:6], [st84[s]], [st84[s]])
                for s in R4:
                    STT("dve", st84[s][:, 6:7], st84[s][:, 2:3], -1.0, st84[s][:, 5:6], ALU.mult, ALU.mult,
                        [st84[s]], [st84[s]])
                for s in R4:
                    ACT(gg4[s][:], gg4[s][:], AF.Identity, [gg4[s], st84[s]], [gg4[s]], scale=st84[s][:, 5:6],
                        bias=st84[s][:, 6:7])
                for s in R4:
                    TT("dve", gg4[s][:], gg4[s][:], lngb[:], ALU.mult, [gg4[s], lngb], [gg4[s]])
                for s in R4:
                    TT("pool", vn4[s][:], gg4[s][:], lnbb[:], ALU.add, [gg4[s], lnbb], [vn4[s]])
                for j in range(4):
                    a = nacc()
                    for c in range(8):
                        MM(a[:], Win0[:, c, j * 128:(j + 1) * 128], hnT[:, c, :], c == 0, c == 7, [Win0, hnT], [a])
                    ACT(QT[:, j, :], a[:], AF.Copy, [a], [QT], scale=0.125)
                for uc in range(4):
                    a = nacc()
                    for c in range(8):
                        MM(a[:], Win0[:, c, 768 + uc * 128:768 + (uc + 1) * 128], hnT[:, c, :], c == 0, c == 7,
                           [Win0, hnT], [a])
                    ACT(uT[:, uc, :], a[:], AF.Gelu, [a], [uT])

            def a1_stageB1b(n, kind, idx):
                if kind == "h2":
                    return
                xT, hnT = xT2[n % 2], hnT2[n % 2]

                def kv_prev(s):
                    if kind == "halo":
                        return KTh2[:, idx * 4 + s, :], Vh2[:, idx * 4 + s, :], [KTh2, Vh2]
                    if s > 0:
                        return KTc[:, (s - 1) * 128:s * 128], Vc[:, s - 1, :], [KTc, Vc]
                    return KTh[:, idx, :], Vh[:, idx, :], [KTh, Vh]

                def swa1(s):
                    ktp, vp, rp = kv_prev(s)
                    ktc = KTc[:, s * 128:(s + 1) * 128]
                    P_ = PT2[s]
                    for g in range(2):
                        r0 = g * 64
                        for pc in range(2):
                            un = (s * 4 + g * 2 + pc) % 4
                            sbk = pb[2 + un]
                            kt = ktp if pc == 0 else ktc
                            for j in range(4):
                                MM(sbk[:, j * 128:(j + 1) * 128], kt[r0:r0 + 64, :], QT[r0:r0 + 64, j, s * 128:(s + 1) * 128],
                                   True, True, [QT, KTc] + rp, [sbk])
                            sc_ = scs4[un]
                            bia = biasT[:, pc, g * 512:(g + 1) * 512]
                            if kind == "main" and s == 0 and pc == 0:
                                STT("dve", sc_[:], sbk[:], flsw[:, idx:idx + 1], bia, ALU.add, ALU.add,
                                    [sbk, flsw, biasT], [sc_])
                            else:
                                TT("dve", sc_[:], sbk[:], bia, ALU.add, [sbk, biasT], [sc_])
                            ACT(P_[g][pc][:], sc_[:], AF.Exp, [sc_], [P_[g][pc]])

                def swa2(s):
                    ktp, vp, rp = kv_prev(s)
                    vc = Vc[:, s, :]
                    P_ = PT2[s]
                    for g in range(2):
                        px, pd = pb[4 + g], pb[6 + g]
                        for j in range(4):
                            js = slice(j * 128, (j + 1) * 128)
                            MM(px[:, js], vp, P_[g][0][:, js], True, False, [P_[g][0], Vc] + rp, [px])
                            MM(px[:, js], vc, P_[g][1][:, js], False, True, [P_[g][1], Vc], [px])
                            MM(pd[:, js], ones_b[:], P_[g][0][:, js], True, False, [P_[g][0], ones_b], [pd])
                            MM(pd[:, js], ones_b[:], P_[g][1][:, js], False, True, [P_[g][1], ones_b], [pd])
                    for g in range(2):
                        r0 = g * 64
                        TT("dve", dnb[g][r0:r0 + 64, :], pb[6 + g][r0:r0 + 64, :],
                           eskf[r0:r0 + 64, :, :].rearrange("p a b -> p (a b)"), ALU.add, [pb[6 + g], eskf], [dnb[g]])
                    for g in range(2):
                        r0 = g * 64
                        RCP(dnb[g][r0:r0 + 64, :], dnb[g][r0:r0 + 64, :], [dnb[g]], [dnb[g]])
                    for g in range(2):
                        r0 = g * 64
                        TT("dve", mixT[r0:r0 + 64, 0:4, s * 128:(s + 1) * 128],
                           pb[4 + g][r0:r0 + 64, :].rearrange("p (a b) -> p a b", a=4),
                           dnb[g][r0:r0 + 64, :].rearrange("p (a b) -> p a b", a=4), ALU.mult, [pb[4 + g], dnb[g]], [mixT])

                for s in range(4):
                    swa1(s)
                    v_ = vn4[s]
                    for pr in range(4):
                        for hf in range(2):
                            MM(pb[hf][:, pr * 128:(pr + 1) * 128], v_[:, pr * 128:(pr + 1) * 128],
                               WsT[:, 2 * pr + hf, :], True, True, [v_, WsT], [pb[hf]])
                    for hf in range(2):
                        r0 = hf * 64
                        TT("dve", sstage[r0:r0 + 64, :, s * 128:(s + 1) * 128],
                           pb[hf][r0:r0 + 64, :].rearrange("p (a b) -> p a b", a=4), bsb[r0:r0 + 64, :, :],
                           ALU.add, [pb[hf], bsb], [sstage])
                for s in range(4):
                    swa2(s)
                TT("pool", mixT[:, 4:8, :], uT[:], sstage[:], ALU.mult, [uT, sstage], [mixT])

            def a1_stageB2(n, kind, idx):
                xT = xT2[n % 2]
                if kind == "h2":
                    return
                for dc in range(8):
                    a = nacc()
                    for mc in range(8):
                        MM(a[:], Wo0[:, mc, dc * 128:(dc + 1) * 128], mixT[:, mc, :], mc == 0, mc == 7, [Wo0, mixT], [a])
                    STT("dve", xT[:, dc, :], a[:], GATE(0, 1)[:, dc:dc + 1], xT[:, dc, :], ALU.mult, ALU.add,
                        [a, modT[0], xT], [xT])
                if kind == "main":
                    store_hT(xT, idx)
                else:
                    for s in range(4):
                        c0 = (idx * 4 + s) * 2
                        CP("pool", hcomp[:, :, c0:c0 + 2], xT[:, :, s * 128 + 126:s * 128 + 128], [xT], [hcomp])

            work = [(xh2[gq * 512:(gq + 1) * 512, :], "h2", gq) for gq in range(4)]
            work += [(xh[gq * 512:(gq + 1) * 512, :], "halo", gq) for gq in range(4)]
            work += [(xm[pos * 512:(pos + 1) * 512, :], "main", pos) for pos in range(NPOS)]

            def x_fetch(n):
                DMA("sp", xts[n % 2][:], work[n][0].rearrange("(s p) d -> p s d", p=128), [], [xts[n % 2]])

            x_fetch(0)
            a1_stageA(xt, 0)
            for n in range(len(work)):
                if n + 1 < len(work):
                    x_fetch(n + 1)
                a1_stageB1(n, work[n][1], work[n][2])
                if n + 1 < len(work):
                    a1_stageA(xt, n + 1)
                a1_stageB1b(n, work[n][1], work[n][2])
                a1_stageB2(n, work[n][1], work[n][2])
                emit_conversions(2)
        p.barrier()

        with contextlib.ExitStack() as st:
            Wg = sbt(st, "Wg", [128, 8, FF0], BF16)
            Wu = sbt(st, "Wu", [128, 8, FF0], BF16)
            Wdp = [sbt(st, "Wdp%d" % i, [128, 22, 128], BF16) for i in range(3)]
            hb = [sbt(st, "hb%d" % i, [128, 8, TB], F32) for i in range(2)]
            hn = sbt(st, "hn2", [128, 8, TB], BF16)
            actT = sbt(st, "actT", [128, 22, TB], BF16)
            sq = [sbt(st, "sq%d" % i, [128, TB], F32) for i in range(2)]
            tmpn = [sbt(st, "tmpn%d" % i, [128, TB], F32) for i in range(2)]
            rr = sbt(st, "rr", [128, TB], F32)
            sg = [sbt(st, "sg%d" % i, [128, TB], F32) for i in range(2)]
            for c in range(8):
                DMA("pool", Wg[:, c, :], wg0[c * 128:(c + 1) * 128, :], [], [Wg])
                DMA("pool", Wu[:, c, :], wu0[c * 128:(c + 1) * 128, :], [], [Wu])
            hnc = sbt(st, "hnc", [128, 8, 32], BF16)
            actc = sbt(st, "actc", [128, 22, 32], BF16)
            wdn = [0]

            def a2_norm(h, hn_t, N):
                norm_mod(h, 8, N, float(D), gsc[:, 8:16], SH(0, 2), hn_t, (sq, tmpn, rr))

            def a2_body(h, hn_t, act_t, N, mid=None):
                for fc in range(22):
                    ag, au = pb[2 + 2 * (fc % 2)], pb[3 + 2 * (fc % 2)]
                    for c in range(8):
                        MM(ag[:, :N], Wg[:, c, fc * 128:(fc + 1) * 128], hn_t[:, c, :], c == 0, c == 7, [Wg, hn_t], [ag])
                    for c in range(8):
                        MM(au[:, :N], Wu[:, c, fc * 128:(fc + 1) * 128], hn_t[:, c, :], c == 0, c == 7, [Wu, hn_t], [au])
                    s_ = sg[fc % 2]
                    ACT(s_[:, :N], ag[:, :N], AF.Silu, [ag], [s_])
                    TT("dve", act_t[:, fc, :], s_[:, :N], au[:, :N], ALU.mult, [s_, au], [act_t])
                if mid is not None:
                    mid()
                for dc in range(8):
                    w = Wdp[wdn[0] % 3]
                    wdn[0] += 1
                    DMA("pool", w[:], wd0[:, dc * 128:(dc + 1) * 128].rearrange("(f p) m -> p f m", p=128), [], [w])
                    a = nacc()
                    for fc in range(22):
                        MM(a[:, :N], w[:, fc, :], act_t[:, fc, :], fc == 0, fc == 21, [w, act_t], [a])
                    STT("dve", h[:, dc, :], a[:, :N], GATE(0, 2)[:, dc:dc + 1], h[:, dc, :], ALU.mult, ALU.add,
                        [a, modT[0], h], [h])

            hnB = sbt(st, "hn2b", [128, 8, TB], BF16)
            hns = [hn, hnB]
            load_hT(hb[0], 0)
            a2_norm(hcomp, hnc, 32)
            a2_body(hcomp, hnc, actc, 32, mid=lambda: a2_norm(hb[0], hns[0], TB))
            for blk in range(NPOS):
                h = hb[blk % 2]
                if blk + 1 < NPOS:
                    load_hT(hb[(blk + 1) % 2], blk + 1)
                    nxt = (lambda b=blk: a2_norm(hb[(b + 1) % 2], hns[(b + 1) % 2], TB))
                else:
                    nxt = None
                a2_body(h, hns[blk % 2], actT, TB, mid=nxt)
                store_hT(h, blk)
                if dbg:
                    DMA("sp", dbg_out["dbg_h"][:, blk * TB:(blk + 1) * TB].rearrange("(c p) n -> p c n", p=128), h[:],
                        [h], [])
        p.barrier()

        with contextlib.ExitStack() as st:
            Win1 = sbt(st, "Win1", [128, 8, 2336], BF16)
            Wkpe = sbt(st, "Wkpe", [128, 8, 96], BF16)
            Wkper = sbt(st, "Wkper", [128, 8, 96], BF16)
            Wqb = sbt(st, "Wqb", [128, 4, 768], BF16)
            Wqbr = sbt(st, "Wqbr", [128, 4, 768], BF16)
            Wkn = sbt(st, "Wkn", [128, 2, 512], BF16)
            Wv = sbt(st, "Wv", [128, 2, 512], BF16)
            hb = [sbt(st, "hb%d" % i, [128, 8, TB], F32) for i in range(2)]
            hn2 = [sbt(st, "hn1", [128, 8, TB], BF16), sbt(st, "hn1b", [128, 8, TB], BF16)]
            hncur = [hn2[0]]
            sq = [sbt(st, "sq%d" % i, [128, TB], F32) for i in range(2)]
            tmpn = [sbt(st, "tmpn%d" % i, [128, TB], F32) for i in range(2)]
            rr = sbt(st, "rr", [128, TB], F32)
            scr = (sq, tmpn, rr)
            gbT = sbt(st, "gbT", [128, 4, TB], F32)
            gcT = sbt(st, "gcT", [128, 4, TB], F32)
            z = sbt(st, "z", [128, 4, TB + 2], F32)
            zh = sbt(st, "zh", [128, 4, NPOS, 2], F32)
            yc = [sbt(st, "yc%d" % i, [128, TB], F32) for i in range(2)]
            cTt = sbt(st, "cTt", [128, 4, TB], BF16)
            qlT = sbt(st, "qlT", [128, 4, TB], F32)
            qnT = sbt(st, "qnT", [128, 4, TB], BF16)
            kvlT = sbt(st, "kvlT", [128, 2, TB], F32)
            kvnT = sbt(st, "kvnT", [128, 2, TB], BF16)
            QTh = [sbt(st, "QTh%d" % i, [128, TB], BF16) for i in range(2)]
            rt = [sbt(st, "rt%d" % i, [128, TB], F32) for i in range(2)]
            knt = [sbt(st, "knt%d" % i, [128, TB], BF16) for i in range(2)]
            vt = sbt(st, "vt", [128, 4, 512], BF16)
            kpe = sbt(st, "kpe", [128, TB], BF16)
            rk = sbt(st, "rk", [128, 2, TB], F32)
            rq = sbt(st, "rq", [128, 2, TB], F32)

            DMA("pool", Win1[:], w_in1.rearrange("(c p) f -> p c f", p=128), [], [Win1])
            MEMSET("pool", Wkpe[:], 0.0, [Wkpe])
            MEMSET("pool", Wkper[:], 0.0, [Wkper])
            MEMSET("pool", Wqbr[:], 0.0, [Wqbr])
            CP("pool", Wkpe[:, :, 64:96], Win1[:, :, 2304:2336], [Win1], [Wkpe])
            TS("pool", Wkper[:, :, 64:80], Win1[:, :, 2320:2336], -1.0, ALU.mult, [Win1], [Wkper])
            CP("pool", Wkper[:, :, 80:96], Win1[:, :, 2304:2320], [Win1], [Wkper])
            DMA("pool", Wqb[:], w_qb.rearrange("(c p) f -> p c f", p=128), [], [Wqb])
            Wqb_v = Wqb[:].rearrange("p c (h e) -> p c h e", e=96)
            Wqbr_v = Wqbr[:].rearrange("p c (h e) -> p c h e", e=96)
            for c in range(4):
                TS("pool", Wqbr_v[:, c, :, 64:80], Wqb_v[:, c, :, 80:96], -1.0, ALU.mult, [Wqb], [Wqbr])
                CP("pool", Wqbr_v[:, c, :, 80:96], Wqb_v[:, c, :, 64:80], [Wqb], [Wqbr])
            kv_v = w_kvb.rearrange("(c p) (h t d) -> p c h t d", p=128, t=2, d=64)
            for c in range(2):
                DMA("pool", Wkn[:, c, :].rearrange("p (h d) -> p h d", d=64), kv_v[:, c, :, 0, :], [], [Wkn])
                DMA("pool", Wv[:, c, :].rearrange("p (h d) -> p h d", d=64), kv_v[:, c, :, 1, :], [], [Wv])
            nqh = [0]

            def proj_fm(col0, nchunk, dst_fn):
                for k in range(nchunk):
                    a = nacc()
                    for c in range(8):
                        MM(a[:], Win1[:, c, col0 + k * 128:col0 + (k + 1) * 128], hncur[0][:, c, :], c == 0, c == 7,
                           [Win1, hncur[0]], [a])
                    dst_fn(k, a)

            hnc1 = sbt(st, "hnc1", [128, 8, 32], BF16)
            gcc = sbt(st, "gcc", [128, 4, 32], F32)
            zh_v = zh[:].rearrange("p c n k -> p c (n k)")
            norm_mod(hcomp, 8, 32, float(D), gsc[:, 16:24], SH(1, 1), hnc1, scr)
            for k in range(4):
                a = nacc()
                for c in range(8):
                    MM(a[:, :32], Win1[:, c, 512 + k * 128:512 + (k + 1) * 128], hnc1[:, c, :], c == 0, c == 7, [Win1, hnc1], [a])
                EVAC(gcc[:, k, :], a[:, :32], [a], [gcc])
            for k in range(4):
                a = nacc()
                for c in range(8):
                    MM(a[:, :32], Win1[:, c, 1024 + k * 128:1024 + (k + 1) * 128], hnc1[:, c, :], c == 0, c == 7, [Win1, hnc1], [a])
                TT("dve", zh_v[:, k, :], gcc[:, k, :], a[:, :32], ALU.mult, [gcc, a], [zh])
            load_hT(hb[0], 0)
            norm_mod(hb[0], 8, TB, float(D), gsc[:, 16:24], SH(1, 1), hn2[0], scr)
            for pos in range(NPOS):
                blk = pos
                h = hb[pos % 2]
                hncur[0] = hn2[pos % 2]
                if pos + 1 < NPOS:
                    load_hT(hb[(pos + 1) % 2], pos + 1)
                proj_fm(512, 4, lambda k, a: EVAC(gcT[:, k, :], a[:], [a], [gcT]))
                proj_fm(1024, 4, lambda k, a: TT("dve", z[:, k, 2:TB + 2], gcT[:, k, :], a[:], ALU.mult, [gcT, a], [z]))
                TS("pool", z[:, :, 0:2], zh[:, :, pos, :], flcv[:, pos:pos + 1], ALU.mult, [zh, flcv], [z])
                proj_fm(0, 4, lambda k, a: EVAC(gbT[:, k, :], a[:], [a], [gbT]))
                for cc in range(4):
                    y_ = yc[cc % 2]
                    TS("dve", y_[:], z[:, cc, 0:TB], vec[:, V_CW + cc:V_CW + cc + 1], ALU.mult, [z, vec], [y_])
                    STT("dve", y_[:], z[:, cc, 1:TB + 1], vec[:, V_CW + 4 + cc:V_CW + 5 + cc], y_[:], ALU.mult, ALU.add,
                        [z, vec, y_], [y_])
                    STT("dve", y_[:], z[:, cc, 2:TB + 2], vec[:, V_CW + 8 + cc:V_CW + 9 + cc], y_[:], ALU.mult, ALU.add,
                        [z, vec, y_], [y_])
                    TT("pool", cTt[:, cc, :], gbT[:, cc, :], y_[:], ALU.mult, [gbT, y_], [cTt])
                DMA("sp", cT_d[:, pos * TB:(pos + 1) * TB].rearrange("(c p) n -> p c n", p=128), cTt[:], [cTt], [b_cT[pos]])
                if pos + 1 < NPOS:
                    norm_mod(hb[(pos + 1) % 2], 8, TB, float(D), gsc[:, 16:24], SH(1, 1), hn2[(pos + 1) % 2], scr)
                DMA("sp", rk[64:96, :, :], ropek_d[:, :, pos * TB:(pos + 1) * TB].rearrange("a r n -> r a n"), [], [rk])
                if pos < NOWN:
                    DMA("sp", rq[64:96, :, :], ropeq_d[:, :, pos * TB:(pos + 1) * TB].rearrange("a r n -> r a n"), [], [rq])
                    proj_fm(1536, 4, lambda k, a: EVAC(qlT[:, k, :], a[:], [a], [qlT]))
                    norm_mod(qlT, 4, TB, 512.0, vec[:, V_QG:V_QG + 4], None, qnT, scr)
                    for hh in range(8):
                        aa, ab = pb[2 + 2 * (hh % 2)], pb[3 + 2 * (hh % 2)]
                        for c in range(4):
                            MM(aa[0:96, :], Wqb[:, c, hh * 96:(hh + 1) * 96], qnT[:, c, :], c == 0, c == 3, [Wqb, qnT], [aa])
                        for c in range(4):
                            MM(ab[0:96, :], Wqbr[:, c, hh * 96:(hh + 1) * 96], qnT[:, c, :], c == 0, c == 3, [Wqbr, qnT], [ab])
                        qt = QTh[nqh[0] % 2]
                        r1, r2 = rt[0], rt[1]
                        nqh[0] += 1
                        ACT(qt[0:64, :], aa[0:64, :], AF.Copy, [aa], [qt], scale=float(96 ** -0.5))
                        TT("dve", r1[64:96, :], aa[64:96, :], rq[64:96, 0, :], ALU.mult, [aa, rq], [r1])
                        TT("dve", r2[64:96, :], ab[64:96, :], rq[64:96, 1, :], ALU.mult, [ab, rq], [r2])
                        TT("pool", qt[64:96, :], r1[64:96, :], r2[64:96, :], ALU.add, [r1, r2], [qt])
                        DMA("sp", qT_d[hh, :, pos * TB:(pos + 1) * TB], qt[0:96, :], [qt], [b_q])
                proj_fm(2048, 2, lambda k, a: EVAC(kvlT[:, k, :], a[:], [a], [kvlT]))
                norm_mod(kvlT, 2, TB, 256.0, vec[:, V_KG:V_KG + 2], None, kvnT, scr)
                for hc in range(4):
                    a = nacc()
                    for c in range(2):
                        MM(a[:], Wkn[:, c, hc * 128:(hc + 1) * 128], kvnT[:, c, :], c == 0, c == 1, [Wkn, kvnT], [a])
                    kt_ = knt[hc % 2]
                    EVAC(kt_[:], a[:], [a], [kt_])
                    DMA("sp", knT_d[hc * 128:(hc + 1) * 128, pos * TB:(pos + 1) * TB], kt_[:], [kt_], [b_kn])
                for s in range(4):
                    a = nacc()
                    for c in range(2):
                        MM(a[:], kvnT[:, c, s * 128:(s + 1) * 128], Wv[:, c, :], c == 0, c == 1, [Wv, kvnT], [a])
                    EVAC(vt[:, s, :], a[:], [a], [vt])
                DMA("sp", v_d[pos * TB:(pos + 1) * TB, :].rearrange("(s p) f -> p s f", p=128), vt[:], [vt], [b_v])
                aa, ab = pb[2], pb[3]
                for c in range(8):
                    MM(aa[0:96, :], Wkpe[:, c, :], hncur[0][:, c, :], c == 0, c == 7, [Wkpe, hncur[0]], [aa])
                for c in range(8):
                    MM(ab[0:96, :], Wkper[:, c, :], hncur[0][:, c, :], c == 0, c == 7, [Wkper, hncur[0]], [ab])
                r1, r2 = rt[0], rt[1]
                TT("dve", r1[64:96, :], aa[64:96, :], rk[64:96, 0, :], ALU.mult, [aa, rk], [r1])
                TT("dve", r2[64:96, :], ab[64:96, :], rk[64:96, 1, :], ALU.mult, [ab, rk], [r2])
                TT("pool", kpe[64:96, :], r1[64:96, :], r2[64:96, :], ALU.add, [r1, r2], [kpe])
                DMA("sp", kpeT_d[:, pos * TB:(pos + 1) * TB], kpe[64:96, :], [kpe], [b_kpe])
        p.barrier()

        with contextlib.ExitStack() as st:
            KT = [sbt(st, "KT%d" % i, [128, S], BF16) for i in range(2)]
            VA = [sbt(st, "VA%d" % i, [128, 64, 128], BF16) for i in range(2)]
            QA = [sbt(st, "QA%d" % i, [128, NOWN * TB], BF16) for i in range(2)]
            PTm = [sbt(st, "PTm%d" % i, [128, TB], BF16) for i in range(4)]
            msk = sbt(st, "msk", [128, 4, TB], BF16)
            ODs = [sbt(st, "ODs%d" % i, [128, TB], F32) for i in range(2)]
            rden = [sbt(st, "rden%d" % i, [128, TB], F32) for i in range(2)]
            dout = [sbt(st, "dout%d" % i, [128, TB], BF16) for i in range(2)]
            SEL = [sbt(st, "SEL%d" % i, [128, 128], F32) for i in range(2)]
            DMA("pool", msk[:], mlam_d.rearrange("p (a b) -> p a b", a=4), [], [msk])
            flob = sbt(st, "flob", [128, NOWN], F32)
            TS("dve", flob[:], flot[:], -NEG, ALU.mult, [flot], [flob], s2=NEG, op1=ALU.add)
            for i_, off in ((0, -64), (1, 64)):
                MEMSET("pool", SEL[i_][:], 0.0, [SEL[i_]])
                p.op("pool", lambda e, t=SEL[i_], off=off: e.affine_select(
                    out=t[:], in_=t[:], pattern=[[-1, 128]], compare_op=ALU.not_equal, fill=1.0, base=off,
                    channel_multiplier=1), (), [SEL[i_].b])
            MEMSET("pool", VA[0][:, :, 64:128], 1.0, [VA[0]])
            MEMSET("pool", VA[1][:, :, 0:64], 1.0, [VA[1]])
            def b2_load(hh):
                par = hh % 2
                kt, va, qa = KT[par], VA[par], QA[par]
                voff = 0 if par == 0 else 64
                DMA("sp", kt[0:64, :], knT_d[hh * 64:(hh + 1) * 64, :], [b_kn], [kt])
                DMA("sp", kt[64:96, :], kpeT_d[:, :], [b_kpe], [kt])
                for q4 in range(4):
                    DMA("sp", va[:, q4 * 16:(q4 + 1) * 16, voff:voff + 64],
                        v_d[q4 * 2048:(q4 + 1) * 2048, hh * 64:(hh + 1) * 64].rearrange("(kb p) d -> p kb d", p=128),
                        [b_v], [va])
                DMA("sp", qa[0:96, :], qT_d[hh, :, :], [b_q], [qa])

            items = []
            for hh in range(8):
                for i in range(NOWN):
                    klist = []
                    for i2 in range(i):
                        for sub in range(4):
                            klist.append((i2, sub, None))
                            klist.append((8 + i2, sub, None))
                    for sub in range(4):
                        klist.append((8 + i, sub, "oth"))
                    for sub in range(4):
                        klist.append((i, sub, "diag"))
                    for n, (pos, sub, kind) in enumerate(klist):
                        items.append((hh, i, n, len(klist), pos, sub, kind))
            sb3 = [pb[1], pb[2], pb[3]]

            def emit_S(t):
                hh, i, n, nk, pos, sub, kind = items[t]
                par = hh % 2
                kt, qa = KT[par], QA[par]
                kc = pos * TB + sub * 128
                sbk = sb3[t % 3]
                pt = PTm[t % 4]
                MM(sbk[:], kt[0:96, kc:kc + 128], qa[0:96, i * TB:(i + 1) * TB], True, True, [kt, qa], [sbk])
                if kind == "oth":
                    ACT(pt[:], sbk[:], AF.Exp, [sbk, flob], [pt], bias=flob[:, i:i + 1], scale=1.0)
                else:
                    ACT(pt[:], sbk[:], AF.Exp, [sbk], [pt])
                if kind == "diag":
                    TT("dve", pt[:], pt[:], msk[:, sub, :], ALU.mult, [pt, msk], [pt])

            b2_load(0)
            SKEW = 2
            for t in range(min(SKEW, len(items))):
                emit_S(t)
            nod = 0
            for t in range(len(items)):
                hh, i, n, nk, pos, sub, kind = items[t]
                par = hh % 2
                va = VA[par]
                if i == 0 and n == 0 and hh + 1 < 8:
                    b2_load(hh + 1)
                if t + SKEW < len(items):
                    emit_S(t + SKEW)
                od = pb[4 + nod % 2]
                MM(od[:], va[:, pos * 4 + sub, :], PTm[t % 4][:], n == 0, n == nk - 1, [va, PTm[t % 4]], [od])
                if n == nk - 1:
                    os_ = ODs[nod % 2]
                    rd = rden[nod % 2]
                    do = dout[nod % 2]
                    nod += 1
                    CP("act", os_[:], od[:], [od], [os_])
                    db = pb[6]
                    MM(db[:], SEL[par][:], os_[:], True, True, [SEL[par], os_], [db])
                    r0 = par * 64
                    RCP(rd[r0:r0 + 64, :], db[r0:r0 + 64, :], [db], [rd])
                    TT("dve", do[r0:r0 + 64, :], os_[r0:r0 + 64, :], rd[r0:r0 + 64, :], ALU.mult, [os_, rd], [do])
                    DMA("sp", dT_d[hh * 64:(hh + 1) * 64, i * TB:(i + 1) * TB], do[r0:r0 + 64, :], [do], [b_dT[i]])
        p.barrier()

        Mall = sbt(top, "Mall", [128, 32, 8], F32)
        Gall = sbt(top, "Gall", [128, 32, 8], F32)
        with contextlib.ExitStack() as st:
            Wo1 = sbt(st, "Wo1", [128, 8, D], BF16)
            wr = sbt(st, "wr", [128, 8, 8], F32)
            brb = sbt(st, "brb", [128, 8], F32)
            mix = [sbt(st, "mix%d" % i, [128, 8, TB], BF16) for i in range(2)]
            hb = [sbt(st, "hb%d" % i, [128, 8, TB], F32) for i in range(2)]
            hnf = sbt(st, "hnf", [128, 8, TB], F32)
            sq = [sbt(st, "sq%d" % i, [128, TB], F32) for i in range(2)]
            tmpn = [sbt(st, "tmpn%d" % i, [128, TB], F32) for i in range(2)]
            rr = sbt(st, "rr", [128, TB], F32)
            lg = [sbt(st, "lg%d" % i, [128, 8], F32) for i in range(2)]
            e8 = [sbt(st, "e8_%d" % i, [128, 8], F32) for i in range(2)]
            s4 = [sbt(st, "s4_%d" % i, [128, 4], F32) for i in range(2)]
            tk = [sbt(st, "tk%d" % i, [128, D], F32) for i in range(4)]
            ntk = [0]
            DMA("pool", Wo1[:], w_o1.rearrange("(c p) m -> p c m", p=128), [], [Wo1])
            DMA("sp", wr[:], w_rt.rearrange("(c p) e -> p c e", p=128), [], [wr])
            DMA("sp", brb[:], brt_d.partition_broadcast(128), [], [brb])
            DMA("sp", g2row_d.rearrange("(c p) -> p c", p=128), modT[1][:, 40:48], [modT[1]], [b_g2row],
                allow_slow_non_contiguous=True)

            def to_rows(src, dst_d, bufs, i):
                for s in range(4):
                    t_ = tk[ntk[0] % 4]
                    ntk[0] += 1
                    for half in range(2):
                        a = nacc()
                        for c4 in range(4):
                            TR(a[:, c4 * 128:(c4 + 1) * 128], src[:, half * 4 + c4, s * 128:(s + 1) * 128], ident[:],
                               [src, ident], [a])
                        EVAC(t_[:, half * 512:(half + 1) * 512], a[:], [a], [t_])
                    ch = i * 4 + s
                    DMA("sp", dst_d[ch * 128:(ch + 1) * 128, :], t_[:], [t_], [bufs[ch]])

            for i in range(NOWN):
                m_ = mix[i % 2]
                h = hb[i % 2]
                DMA("sp", m_[:, 0:4, :], cT_d[:, i * TB:(i + 1) * TB].rearrange("(c p) n -> p c n", p=128), [b_cT[i]], [m_])
                DMA("sp", m_[:, 4:8, :], dT_d[:, i * TB:(i + 1) * TB].rearrange("(c p) n -> p c n", p=128), [b_dT[i]], [m_])
                load_hT(h, i)
                for dc in range(8):
                    a = nacc()
                    for mc in range(8):
                        MM(a[:], Wo1[:, mc, dc * 128:(dc + 1) * 128], m_[:, mc, :], mc == 0, mc == 7, [Wo1, m_], [a])
                    STT("dve", h[:, dc, :], a[:], GATE(1, 1)[:, dc:dc + 1], h[:, dc, :], ALU.mult, ALU.add,
                        [a, modT[1], h], [h])
                to_rows(h, h3tok_d, b_h3t, i)
                norm_mod(h, 8, TB, float(D), gsc[:, 24:32], SH(1, 2), hnf, (sq, tmpn, rr))
                to_rows(hnf, hn3tok_d, b_hn3t, i)
                for s in range(4):
                    ch = i * 4 + s
                    a = nacc()
                    for c in range(8):
                        MM(a[:, 0:8], hnf[:, c, s * 128:(s + 1) * 128], wr[:, c, :], c == 0, c == 7, [hnf, wr], [a])
                    L, E8, S4 = lg[s % 2], e8[s % 2], s4[s % 2]
                    W8 = Mall[:, ch, :]
                    TT("dve", L[:], a[:, 0:8], brb[:], ALU.add, [a, brb], [L])
                    RED(S4[:, 0:1], L[:], ALU.max, [L], [S4])
                    TS("dve", W8, L[:], S4[:, 0:1], ALU.is_equal, [L, S4], [Mall])
                    STT("dve", W8, W8, -1e30, L[:], ALU.mult, ALU.add, [Mall, L], [Mall])
                    RED(S4[:, 1:2], W8, ALU.max, [Mall], [S4])
                    TS("dve", W8, L[:], S4[:, 1:2], ALU.is_ge, [L, S4], [Mall])
                    TS("dve", S4[:, 2:3], S4[:, 0:1], -1.0, ALU.mult, [S4], [S4])
                    ACT(E8[:], L[:], AF.Exp, [L, S4], [E8], bias=S4[:, 2:3], scale=1.0)
                    TT("dve", E8[:], E8[:], W8, ALU.mult, [E8, Mall], [E8])
                    RED(S4[:, 3:4], E8[:], ALU.add, [E8], [S4])
                    RCP(S4[:, 3:4], S4[:, 3:4], [S4], [S4])
                    TS("dve", Gall[:, ch, :], E8[:], S4[:, 3:4], ALU.mult, [E8, S4], [Gall])
        p.barrier()

        I32 = mybir.dt.int32
        idx_hi = sbt(top, "idx_hi", [128, 32], I32)
        idx_lo = sbt(top, "idx_lo", [128, 32], I32)
        g_hi = sbt(top, "g_hi", [128, 32], F32)
        g_lo = sbt(top, "g_lo", [128, 32], F32)
        widx = sbt(top, "widx", [128, NBLK, 7], I32)
        with contextlib.ExitStack() as st:
            cst = sbt(st, "cst", [128, 32], F32)
            Uex = sbt(st, "Uex", [128, 128], F32)
            tot = sbt(st, "tot", [128, 32, 8], F32)
            offs = sbt(st, "offs", [128, 32, 8], F32)
            slot = sbt(st, "slot", [128, 32, 8], F32)
            vidx = sbt(st, "vidx", [128, 32, 8], F32)
            t256 = sbt(st, "t256", [128, 32, 8], F32)
            eqh = sbt(st, "eqh", [128, 32, 8], F32)
            ne = sbt(st, "ne", [128, 8], F32)
            nb8 = sbt(st, "nb8", [128, 8], F32)
            c8 = sbt(st, "c8", [128, 8], F32)
            pend = sbt(st, "pend", [128, 8], F32)
            pstart = sbt(st, "pstart", [128, 8], F32)
            hi_f = sbt(st, "hi_f", [128, 32], F32)
            lo_f = sbt(st, "lo_f", [128, 32], F32)
            gs = sbt(st, "gs", [128, 32], F32)
            be = sbt(st, "be", [128, NBLK], F32)
            t24 = sbt(st, "t24", [128, NBLK], F32)
            wf = sbt(st, "wf", [128, NBLK, 7], F32)
            DMA("sp", cst[:], cst_d, [], [cst])
            MEMSET("pool", Uex[:], 1.0, [Uex])
            p.op("pool", lambda e: e.affine_select(out=Uex[:], in_=Uex[:], pattern=[[1, 128]],
                                                   compare_op=ALU.is_gt, fill=0.0, base=0, channel_multiplier=-1),
                 (), [Uex.b])
            Mf = Mall[:].rearrange("p a b -> p (a b)")
            MM(pb[2][:, 0:256], Uex[:], Mf, True, True, [Uex, Mall], [pb[2]])
            MM(pb[3][:, 0:256], ones_f[:], Mf, True, True, [ones_f, Mall], [pb[3]])
            CP("dve", tot[:].rearrange("p a b -> p (a b)"), pb[3][:, 0:256], [pb[3]], [tot])
            MEMSET("pool", offs[:, 0, :], 0.0, [offs])
            for ch in range(1, 32):
                TT("dve", offs[:, ch, :], offs[:, ch - 1, :], tot[:, ch - 1, :], ALU.add, [offs, tot], [offs])
            TT("dve", ne[:], offs[:, 31, :], tot[:, 31, :], ALU.add, [offs, tot], [ne])
            TS("dve", nb8[:], ne[:], 0.0, ALU.is_gt, [ne], [nb8])
            for k in range(1, 8):
                TS("dve", c8[:], ne[:], 512.0 * k, ALU.is_gt, [ne], [c8])
                TT("dve", nb8[:], nb8[:], c8[:], ALU.add, [nb8, c8], [nb8])
            CP("dve", pend[:, 0:1], nb8[:, 0:1], [nb8], [pend])
            for e in range(1, 8):
                TT("dve", pend[:, e:e + 1], pend[:, e - 1:e], nb8[:, e:e + 1], ALU.add, [pend, nb8], [pend])
            TT("dve", pstart[:], pend[:], nb8[:], ALU.subtract, [pend, nb8], [pstart])
            TS("dve", pstart[:], pstart[:], 512.0, ALU.mult, [pstart], [pstart])
            TT("dve", slot[:].rearrange("p a b -> p (a b)"), pb[2][:, 0:256], offs[:].rearrange("p a b -> p (a b)"),
               ALU.add, [pb[2], offs], [slot])
            for e in range(8):
                TS("dve", slot[:, :, e], slot[:, :, e], pstart[:, e:e + 1], ALU.add, [slot, pstart], [slot])
            TT("dve", vidx[:], slot[:], Mall[:], ALU.mult, [slot, Mall], [vidx])
            RED(hi_f[:], vidx[:], ALU.max, [vidx], [hi_f])
            BIG = 1.0e6
            TS("dve", t256[:], Mall[:], -BIG, ALU.mult, [Mall], [t256], s2=BIG, op1=ALU.add)
            TT("dve", t256[:], t256[:], slot[:], ALU.add, [t256, slot], [t256])
            RED(lo_f[:], t256[:], ALU.min, [t256], [lo_f])
            TS("dve", lo_f[:], lo_f[:], float(LSLOT - 1), ALU.min, [lo_f], [lo_f])
            for e in range(8):
                TT("dve", eqh[:, :, e], vidx[:, :, e], hi_f[:], ALU.is_equal, [vidx, hi_f], [eqh])
            TT("dve", eqh[:], eqh[:], Gall[:], ALU.mult, [eqh, Gall], [eqh])
            RED(g_hi[:], eqh[:], ALU.add, [eqh], [g_hi])
            RED(gs[:], Gall[:], ALU.add, [Gall], [gs])
            TT("dve", g_lo[:], gs[:], g_hi[:], ALU.subtract, [gs, g_hi], [g_lo])
            CP("dve", idx_hi[:], hi_f[:], [hi_f], [idx_hi])
            CP("dve", idx_lo[:], lo_f[:], [lo_f], [idx_lo])
            MEMSET("pool", be[:], 0.0, [be])
            for e in range(8):
                TS("dve", t24[:], cst[:, 0:NBLK], pend[:, e:e + 1], ALU.is_ge, [cst, pend], [t24])
                TT("dve", be[:], be[:], t24[:], ALU.add, [be, t24], [be])
            TS("dve", be[:], be[:], 7.0, ALU.min, [be], [be])
            TS("dve", be[:], be[:], 896.0, ALU.mult, [be], [be], s2=cst[:, 24:25], op1=ALU.add)
            for fg in range(7):
                TS("dve", wf[:, :, fg], be[:], 128.0 * fg, ALU.add, [be], [wf])
            CP("dve", widx[:], wf[:], [wf], [widx])
            xr = [sbt(st, "xr%d" % i, [128, D], F32) for i in range(3)]
            for ch in range(32):
                x_ = xr[ch % 3]
                DMA("sp", x_[:], hn3tok_d[ch * 128:(ch + 1) * 128, :], [b_hn3t[ch]], [x_])
                for it in (idx_hi, idx_lo):
                    p.idma(lambda e, x_=x_, it=it, ch=ch: e.indirect_dma_start(
                        out=xs_h[:, :], out_offset=bass.IndirectOffsetOnAxis(ap=it[:, ch:ch + 1], axis=0),
                        in_=x_[:, :], in_offset=None),
                        [x_.b, it.b], [b_xs])
        p.barrier()

        with contextlib.ExitStack() as st:
            WG = [sbt(st, "WG%d" % i, [128, 8, 512], BF16) for i in range(2)]
            WU = [sbt(st, "WU%d" % i, [128, 8, 512], BF16) for i in range(2)]
            WD = [sbt(st, "WD%d" % i, [128, 4, D], BF16) for i in range(2)]
            XT = [sbt(st, "XT%d" % i, [128, 8, TB], BF16) for i in range(2)]
            Yb = [sbt(st, "Yb%d" % i, [128, 4, D], F32) for i in range(2)]
            xrow = [sbt(st, "xrow%d" % i, [128, D], F32) for i in range(8)]
            actm = [sbt(st, "actm%d" % i, [128, 4, TB], BF16) for i in range(2)]
            sg = [sbt(st, "sgm%d" % i, [128, TB], F32) for i in range(2)]
            nsg = [0]
            nxr = [0]

            def w_load(n):
                b_, fg_ = n // 7, n % 7
                k_ = n % 2
                for (tiles, srcs, v) in ((WG, ew_bf["g"], "g"), (WU, ew_bf["u"], "u"), (WD, ew_bf["d"], "d")):
                    t_ = tiles[k_]
                    for h in range(2):
                        if v == "d":
                            o_ = t_[:, 2 * h:2 * h + 2, :].rearrange("p a b -> p (a b)")
                        else:
                            o_ = t_[:, 4 * h:4 * h + 4, :].rearrange("p a b -> p (a b)")
                        p.idma(lambda e, o_=o_, src=srcs[h], b_=b_, fg_=fg_: e.indirect_dma_start(
                            out=o_, out_offset=None, in_=src[:, :],
                            in_offset=bass.IndirectOffsetOnAxis(ap=widx[:, b_, fg_:fg_ + 1], axis=0)),
                            [widx.b] + b_cv, [t_.b])

            def x_issue(b_):
                for s in range(4):
                    xr_ = xrow[(b_ % 2) * 4 + s]
                    DMA("sp", xr_[:], xs_d[b_ * TB + s * 128:b_ * TB + (s + 1) * 128, :], [b_xs], [xr_])

            def x_trans(b_):
                xt_ = XT[b_ % 2]
                for s in range(4):
                    xr_ = xrow[(b_ % 2) * 4 + s]
                    for half in range(2):
                        a = nacc()
                        for c4 in range(4):
                            TR(a[:, c4 * 128:(c4 + 1) * 128], xr_[:, (half * 4 + c4) * 128:(half * 4 + c4 + 1) * 128], ident[:],
                               [xr_, ident], [a])
                        EVAC(xt_[:, half * 4:(half + 1) * 4, s * 128:(s + 1) * 128],
                             a[:].rearrange("p (a b) -> p a b", a=4), [a], [xt_])

            w_load(0)
            x_issue(0)
            x_trans(0)
            for b_ in range(NBLK):
                xt_ = XT[b_ % 2]
                yb = Yb[b_ % 2]
                if b_ + 1 < NBLK:
                    x_issue(b_ + 1)
                for fg in range(7):
                    n_ = b_ * 7 + fg
                    if fg == 3 and b_ + 1 < NBLK:
                        x_trans(b_ + 1)
                    k_ = n_ % 2
                    wgt, wut, wdt = WG[k_], WU[k_], WD[k_]
                    if n_ + 1 < NBLK * 7:
                        w_load(n_ + 1)
                    am = actm[n_ % 2]
                    for f4 in range(4):
                        ag, au = pb[2 + 2 * (f4 % 2)], pb[3 + 2 * (f4 % 2)]
                        for c in range(8):
                            MM(ag[:], wgt[:, c, f4 * 128:(f4 + 1) * 128], xt_[:, c, :], c == 0, c == 7, [wgt, xt_], [ag])
                        for c in range(8):
                            MM(au[:], wut[:, c, f4 * 128:(f4 + 1) * 128], xt_[:, c, :], c == 0, c == 7, [wut, xt_], [au])
                        s_ = sg[nsg[0] % 2]
                        nsg[0] += 1
                        ACT(s_[:], ag[:], AF.Silu, [ag], [s_])
                        TT("dve", am[:, f4, :], s_[:], au[:], ALU.mult, [s_, au], [am])
                    for s4_ in range(4):
                        for half in range(2):
                            a = nacc()
                            for f4 in range(4):
                                MM(a[:], am[:, f4, s4_ * 128:(s4_ + 1) * 128], wdt[:, f4, half * 512:(half + 1) * 512],
                                   f4 == 0, f4 == 3, [wdt, am], [a])
                            dst = yb[:, s4_, half * 512:(half + 1) * 512]
                            if fg == 0:
                                EVAC(dst, a[:], [a], [yb])
                            else:
                                TT("dve", dst, dst, a[:], ALU.add, [yb, a], [yb])
                DMA("sp", ys_d[b_ * TB:(b_ + 1) * TB, :].rearrange("(s p) m -> p s m", p=128), yb[:], [yb], [b_ys[b_]])
        p.barrier()

        fin = []
        with contextlib.ExitStack() as st:
            g2b = sbt(st, "g2b", [128, D], F32)
            fgb = sbt(st, "fgb", [128, D], F32)
            Yh = [sbt(st, "Yh%d" % i, [128, D], F32) for i in range(2)]
            Yl = [sbt(st, "Yl%d" % i, [128, D], F32) for i in range(2)]
            h3r = [sbt(st, "h3r%d" % i, [128, D], F32) for i in range(2)]
            ot = [sbt(st, "ot%d" % i, [128, D], F32) for i in range(2)]
            junk = sbt(st, "junk", [128, D], F32)
            ss = [sbt(st, "ss%d" % i, [128, 2], F32) for i in range(2)]
            DMA("sp", g2b[:], g2row_d.rearrange("(o n) -> o n", o=1).partition_broadcast(128), [b_g2row], [g2b])
            DMA("sp", fgb[:], fgrow_d.partition_broadcast(128), [], [fgb])
            for ch in range(32):
                yh, yl, hr, o, s2 = Yh[ch % 2], Yl[ch % 2], h3r[ch % 2], ot[ch % 2], ss[ch % 2]
                for (yt, it) in ((yh, idx_hi), (yl, idx_lo)):
                    p.idma(lambda e, yt=yt, it=it, ch=ch: e.indirect_dma_start(
                        out=yt[:, :], out_offset=None, in_=ys_h[:, :],
                        in_offset=bass.IndirectOffsetOnAxis(ap=it[:, ch:ch + 1], axis=0)),
                        [it.b] + b_ys, [yt.b])
                DMA("sp", hr[:], h3tok_d[ch * 128:(ch + 1) * 128, :], [b_h3t[ch]], [hr])
                ACT(yl[:], yl[:], AF.Copy, [yl, g_lo], [yl], scale=g_lo[:, ch:ch + 1])
                STT("dve", yh[:], yh[:], g_hi[:, ch:ch + 1], yl[:], ALU.mult, ALU.add, [yh, g_hi, yl], [yh])
                TT("dve", yh[:], yh[:], g2b[:], ALU.mult, [yh, g2b], [yh])
                TT("dve", hr[:], hr[:], yh[:], ALU.add, [hr, yh], [hr])
                MEMSET("pool", s2[:], 0.0, [s2])
                ACT(junk[:], hr[:], AF.Square, [hr], [junk, s2], accum_out=s2[:, 0:1])
                ACT(s2[:, 1:2], s2[:, 0:1], AF.Sqrt, [s2], [s2], bias=EPS_AP[:, 0:1], scale=1.0 / D)
                RCP(s2[:, 1:2], s2[:, 1:2], [s2], [s2])
                STT("dve", o[:], hr[:], s2[:, 1:2], fgb[:], ALU.mult, ALU.mult, [hr, s2, fgb], [o])
                fin.append(DMA("sp", y_out[ch * 128:(ch + 1) * 128, :], o[:], [o], []))
        for t in fin:
            p.wait_tok("sp", t)
        for t in list(p.last_dma.values()):
            p.wait_tok("sp", t)
        p.emit()
    return nc


def _pc(v, k):
    return np.ascontiguousarray(np.asarray(v, np.float32).reshape(k, 128).T)


def _host_inputs(inp, dbg_cores=None):
    f = lambda k: np.asarray(inp[k], np.float32)
    x = f("x")
    B = x.shape[0]
    slopes = (2.0 ** (-8.0 * np.arange(1, 9, dtype=np.float32) / 8)).astype(np.float32)
    k_i = np.arange(128)[:, None]
    q_i = np.arange(128)[None, :]
    swab = np.zeros((128, 2, 2, 4, 128), np.float32)
    for g in range(2):
        for j in range(4):
            sl = slopes[g * 4 + j]
            dist_prev = q_i + 128 - k_i
            dist_cur = q_i - k_i
            bp = np.where((dist_prev >= 0) & (dist_prev < 128), -sl * dist_prev, NEG)
            bc = np.where((dist_cur >= 0) & (dist_cur < 128), -sl * dist_cur, NEG)
            swab[:, 0, g, j, :] = bp
            swab[:, 1, g, j, :] = bc
    swab = swab.reshape(128, 2 * 8 * 128)
    mlam = np.zeros((128, 4, TB), np.float32)
    for d in range(4):
        mlam[:, d, :] = ((d * 128 + np.arange(128))[:, None] <= np.arange(TB)[None, :]).astype(np.float32)
    mlam = mlam.reshape(128, 4 * TB)
    inv = (10000.0 ** (-np.arange(0, 32, 2, dtype=np.float32) / 32)).astype(np.float32)
    ang = np.arange(S, dtype=np.float32)[:, None] * inv[None, :]
    cosf = np.cos(ang).astype(np.float32).T
    sinf = np.sin(ang).astype(np.float32).T
    cos32 = np.concatenate([cosf, cosf], 0)
    sin32 = np.concatenate([sinf, sinf], 0)
    qs = np.float32(96 ** -0.5)

    sinks = f("even_sinks")[0]
    esk = np.zeros((128, 4), np.float32)
    for g in range(2):
        for j in range(4):
            esk[g * 64:(g + 1) * 64, j] = sinks[g * 4 + j]
    bs = f("even_b_s")[0]
    bsb = np.zeros((128, 4, 128), np.float32)
    for pr in range(4):
        bsb[0:64, pr, :] = bs[2 * pr][None, :]
        bsb[64:128, pr, :] = bs[2 * pr + 1][None, :]
    bsb = bsb.reshape(128, 512)
    vecs_common = np.zeros((128, 160), np.float32)
    vecs_common[:, 0:8] = _pc(f("even_norm_mix_g")[0], 8)
    vecs_common[:, 8:16] = _pc(f("even_norm_ffn_g")[0], 8)
    vecs_common[:, 16:24] = _pc(f("odd_norm_mix_g")[0], 8)
    vecs_common[:, 24:32] = _pc(f("odd_norm_ffn_g")[0], 8)
    vecs_common[:, 32:40] = _pc(f("final_norm_g"), 8)
    vecs_common[:, 40:88] = _pc(f("even_ada_b")[0], 48)
    vecs_common[:, 88:136] = _pc(f("odd_ada_b")[0], 48)
    vecs_common[:, 136:140] = _pc(f("odd_q_norm_g")[0], 4)
    vecs_common[:, 140:142] = _pc(f("odd_kv_norm_g")[0], 2)
    cw = f("odd_conv_w")[0]
    for j in range(3):
        vecs_common[:, 142 + j * 4:142 + (j + 1) * 4] = _pc(cw[j], 4)

    shared = {
        "vecs": vecs_common, "swab": swab, "mlam": mlam, "esk_sink": esk,
        "lng": f("even_gmlp_ln_g")[0][None, :].copy(), "lnb": f("even_gmlp_ln_b")[0][None, :].copy(),
        "bsb": bsb, "brt": f("odd_router_b")[0][None, :].copy(),
        "ada_w0": f("even_ada_w")[0], "ada_w1": f("odd_ada_w")[0],
        "w_in0": f("even_w_in")[0], "w_s": f("even_w_s")[0], "w_o0": f("even_w_o")[0],
        "wg0": f("even_ffn_w_gate")[0], "wu0": f("even_ffn_w_up")[0], "wd0": f("even_ffn_w_down")[0],
        "w_in1": f("odd_w_in")[0], "w_qb": f("odd_w_q_b")[0], "w_kvb": f("odd_w_kv_b")[0], "w_o1": f("odd_w_o")[0],
        "w_rt": f("odd_router_w")[0], "fgrow": f("final_norm_g")[None, :].copy(),
    }
    cst = np.zeros((128, 32), np.float32)
    cst[:, 0:24] = np.arange(24, dtype=np.float32)[None, :]
    cst[:, 24] = np.arange(128, dtype=np.float32)
    shared["cst"] = cst
    for nm, key in (("ewg", "odd_exp_w_gate"), ("ewu", "odd_exp_w_up")):
        w = f(key)[0].reshape(NEXP, 2, 4, 128, 7, 512)
        w = w.transpose(1, 0, 4, 3, 2, 5)
        for h in range(2):
            shared["%s_h%d" % (nm, h)] = w[h].reshape(NEXP * 7 * 128, 2048)
    w = f("odd_exp_w_down")[0].reshape(NEXP, 7, 2, 2, 128, D)
    w = w.transpose(2, 0, 1, 4, 3, 5)
    for h in range(2):
        shared["ewd_h%d" % h] = w[h].reshape(NEXP * 7 * 128, 2048)
    shared = {k: np.ascontiguousarray(v, dtype=np.float32) for k, v in shared.items()}
    cvec = f("c")
    maps = []
    orders = []
    cores = range(2 * B) if dbg_cores is None else dbg_cores
    for core in cores:
        b, hf = core // 2, core % 2
        order = list(OWN[hf]) + list(OWN[1 - hf])
        orders.append((b, order))
        xb = x[b]
        xpad = np.concatenate([np.zeros((256, D), np.float32), xb], 0)
        xm_ = np.concatenate([xb[j * TB:(j + 1) * TB] for j in order], 0)
        xh_ = np.concatenate([xpad[j * TB + 128:j * TB + 256] for j in order], 0)
        xh2_ = np.concatenate([xpad[j * TB:j * TB + 128] for j in order], 0)
        fl_swa = np.zeros((128, NPOS), np.float32)
        fl_conv = np.ones((128, NPOS), np.float32)
        for pos, j in enumerate(order):
            if j == 0:
                fl_swa[:, pos] = NEG
                fl_conv[:, pos] = 0.0
        fl_oth = np.zeros((128, NOWN), np.float32)
        for i in range(NOWN):
            fl_oth[:, i] = 1.0 if order[8 + i] < order[i] else 0.0
        tok = np.concatenate([np.arange(j * TB, (j + 1) * TB) for j in order])
        ropek = np.stack([cos32[:, tok], sin32[:, tok]], 0)
        ropeq = np.stack([cos32[:, tok[:NOWN * TB]] * qs, sin32[:, tok[:NOWN * TB]] * qs], 0)
        m = dict(shared)
        m.update({
            "xm": np.ascontiguousarray(xm_), "xh": np.ascontiguousarray(xh_), "xh2": np.ascontiguousarray(xh2_),
            "c_pc": _pc(cvec[b], 8), "fl_swa": fl_swa, "fl_conv": fl_conv, "fl_oth": fl_oth,
            "ropek": np.ascontiguousarray(ropek, dtype=np.float32),
            "ropeq": np.ascontiguousarray(ropeq, dtype=np.float32),
        })
        maps.append(m)
    return maps, orders


def kernel(**inputs):
    x = np.asarray(inputs["x"])
    B = x.shape[0]
    maps, orders = _host_inputs(inputs)
    nc = build(False)
    res = run_bass_kernel_spmd(nc, maps, core_ids=list(range(len(maps))))
    out = np.zeros((B, S, D), np.float32)
    for core, (b, order) in enumerate(orders):
        y = res.results[core]["y"]
        for i in range(NOWN):
            j = order[i]
            out[b, j * TB:(j + 1) * TB, :] = y[i * TB:(i + 1) * TB, :]
    return out
```

```python
import contextlib
import numpy as np
import concourse.bass as bass
import concourse.mybir as mybir
from concourse.bass_utils import run_bass_kernel_spmd

F32 = mybir.dt.float32
BF16 = mybir.dt.bfloat16
AF = mybir.ActivationFunctionType
ALU = mybir.AluOpType
AX = mybir.AxisListType

NDMA_SLOTS = 8
D = 1024
S = 8192
TB = 512
NPOS = 16
NOWN = 8
EPS = 1e-6
OWN = ([0, 3, 4, 7, 8, 11, 12, 15], [1, 2, 5, 6, 9, 10, 13, 14])
FF0 = 2816
FFE = 3584
NEXP = 8
NEG = -30000.0


class Buf:
    __slots__ = ("lw", "rd")

    def __init__(self):
        self.lw = None
        self.rd = {}


class Prog:
    ENG = ("pe", "act", "dve", "pool", "sp")

    def __init__(self, nc):
        self.nc = nc
        self.ops = {e: [] for e in self.ENG}
        self.cnt = {e: 0 for e in self.ENG}
        self.waited = {e: {} for e in self.ENG}
        self.dma_n = {e: 0 for e in self.ENG}
        self.last_dma = {}

    def _need(self, eng, tok, waits):
        if tok is None:
            return
        key, val, src = tok
        if src == eng and key[0] == "c" and eng == "pe":
            return
        w = self.waited[eng]
        if w.get(key, 0) >= val:
            return
        w[key] = val
        waits.append((key, val))

    def _deps(self, eng, reads, writes):
        waits = []
        for b in reads:
            self._need(eng, b.lw, waits)
        for b in writes:
            self._need(eng, b.lw, waits)
            for t in b.rd.values():
                self._need(eng, t, waits)
        m = {}
        for k, v in waits:
            m[k] = max(m.get(k, 0), v)
        return list(m.items())

    def _mark(self, tok, reads, writes):
        for b in reads:
            o = b.rd.get(tok[0])
            if o is None or o[1] < tok[1]:
                b.rd[tok[0]] = tok
        for b in writes:
            b.lw = tok
            b.rd = {}

    def op(self, eng, fn, reads=(), writes=()):
        waits = self._deps(eng, reads, writes)
        self.cnt[eng] += 1
        key = "c:" + eng
        tok = (key, self.cnt[eng], eng)
        self.ops[eng].append((waits, fn, (key, 1)))
        self._mark(tok, reads, writes)
        return tok

    def dma(self, eng, out, in_, reads=(), writes=(), **kw):
        n = self.dma_n[eng]
        self.dma_n[eng] += 1
        slot = n % NDMA_SLOTS
        key = "d:%s:%d" % (eng, slot)
        val = 16 * (n // NDMA_SLOTS + 1)
        waits = self._deps(eng, reads, writes)
        if n >= NDMA_SLOTS:
            pv = val - 16
            if self.waited[eng].get(key, 0) < pv:
                self.waited[eng][key] = pv
                waits.append((key, pv))
        tok = (key, val, eng)
        self.last_dma[key] = tok

        def fn(e, out=out, in_=in_, kw=kw):
            return e.dma_start(out=out, in_=in_, **kw)
        self.ops[eng].append((waits, fn, (key, 16)))
        self._mark(tok, reads, writes)
        return tok

    def idma(self, fn, reads=(), writes=()):
        eng = "pool"
        n = self.dma_n[eng]
        self.dma_n[eng] += 1
        slot = n % NDMA_SLOTS
        key = "d:%s:%d" % (eng, slot)
        val = 16 * (n // NDMA_SLOTS + 1)
        waits = self._deps(eng, reads, writes)
        if n >= NDMA_SLOTS:
            pv = val - 16
            if self.waited[eng].get(key, 0) < pv:
                self.waited[eng][key] = pv
                waits.append((key, pv))
        tok = (key, val, eng)
        self.last_dma[key] = tok
        self.ops[eng].append((waits, fn, (key, 16)))
        self._mark(tok, reads, writes)
        return tok

    def wait_tok(self, eng, tok):
        waits = []
        self._need(eng, tok, waits)
        if waits:
            self.ops[eng].append((waits, None, None))

    def barrier(self):
        toks = [("c:" + e, self.cnt[e], e) for e in self.ENG if self.cnt[e] > 0]
        toks += list(self.last_dma.values())
        for e in self.ENG:
            for t in toks:
                self.wait_tok(e, t)

    def emit(self):
        nc = self.nc
        keys = set()
        for e in self.ENG:
            for waits, fn, inc in self.ops[e]:
                for k, _ in waits:
                    keys.add(k)
                if inc is not None:
                    keys.add(inc[0])
        sems = {}
        with contextlib.ExitStack() as st:
            for k in sorted(keys):
                sems[k] = st.enter_context(nc.semaphore(k.replace(":", "_")))
            block = st.enter_context(nc.Block())

            def run(e, lst):
                for waits, fn, inc in lst:
                    for k, v in waits:
                        e.wait_ge(sems[k], v)
                    if fn is not None:
                        ins = fn(e)
                        ins.then_inc(sems[inc[0]], inc[1])

            @block.tensor
            def _(e):
                run(e, self.ops["pe"])

            @block.scalar
            def _(e):
                run(e, self.ops["act"])

            @block.vector
            def _(e):
                run(e, self.ops["dve"])

            @block.gpsimd
            def _(e):
                run(e, self.ops["pool"])

            @block.sync
            def _(e):
                run(e, self.ops["sp"])


class Tl:
    __slots__ = ("t", "b")

    def __init__(self, t):
        self.t = t
        self.b = Buf()

    def __getitem__(self, k):
        return self.t[k]


def build(dbg=False):
    nc = bass.Bass("TRN2", target_bir_lowering=False)
    p = Prog(nc)

    def din(name, shape):
        return nc.dram_tensor(name, list(shape), F32, kind="ExternalInput").ap()

    def dscr(name, shape, dt):
        return nc.dram_tensor(name, list(shape), dt).ap()

    xm = din("xm", [NPOS * TB, D])
    xh = din("xh", [NPOS * 128, D])
    xh2 = din("xh2", [NPOS * 128, D])
    c_pc = din("c_pc", [128, 8])
    vecs = din("vecs", [128, 160])
    fl_swa_d = din("fl_swa", [128, NPOS])
    fl_conv_d = din("fl_conv", [128, NPOS])
    fl_oth_d = din("fl_oth", [128, NOWN])
    ropek_d = din("ropek", [2, 32, S])
    ropeq_d = din("ropeq", [2, 32, NOWN * TB])
    swab_d = din("swab", [128, 2 * 8 * 128])
    mlam_d = din("mlam", [128, 4 * TB])
    esk_d = din("esk_sink", [128, 4])
    lng_d = din("lng", [1, 512])
    lnb_d = din("lnb", [1, 512])
    bsb_d = din("bsb", [128, 4 * 128])
    brt_d = din("brt", [1, 8])
    ada_w = [din("ada_w0", [D, 6 * D]), din("ada_w1", [D, 6 * D])]
    w_in0 = din("w_in0", [D, 1792])
    w_s = din("w_s", [8, 128, 128])
    w_o0 = din("w_o0", [D, D])
    wg0 = din("wg0", [D, FF0])
    wu0 = din("wu0", [D, FF0])
    wd0 = din("wd0", [FF0, D])
    w_in1 = din("w_in1", [D, 2336])
    w_qb = din("w_qb", [512, 768])
    w_kvb = din("w_kvb", [256, 1024])
    w_o1 = din("w_o1", [D, D])
    w_rt = din("w_rt", [D, 8])
    NWR = NEXP * 7 * 128
    ewg_h = [nc.dram_tensor("ewg_h%d" % h, [NWR, 2048], F32, kind="ExternalInput") for h in range(2)]
    ewu_h = [nc.dram_tensor("ewu_h%d" % h, [NWR, 2048], F32, kind="ExternalInput") for h in range(2)]
    ewd_h = [nc.dram_tensor("ewd_h%d" % h, [NWR, 2048], F32, kind="ExternalInput") for h in range(2)]
    cst_d = din("cst", [128, 32])
    fgrow_d = din("fgrow", [1, D])
    y_out = nc.dram_tensor("y", [NOWN * TB, D], F32, kind="ExternalOutput").ap()

    NCOL = NPOS * TB + NPOS * 128
    hT_d = dscr("hT_d", [D, NCOL], F32)
    cT_d = dscr("cT_d", [512, NPOS * TB], BF16)
    qT_d = dscr("qT_d", [8, 96, NOWN * TB], BF16)
    knT_d = dscr("knT_d", [512, S], BF16)
    kpeT_d = dscr("kpeT_d", [32, S], BF16)
    v_d = dscr("v_d", [S, 512], BF16)
    dT_d = dscr("dT_d", [512, NOWN * TB], BF16)
    NBLK = 23
    LSLOT = NBLK * TB
    ew_src = {"g": ewg_h, "u": ewu_h, "d": ewd_h}
    ew_bf = {k: [nc.dram_tensor("ew%s_b%d" % (k, h), [NWR, 2048], BF16) for h in range(2)] for k in "gud"}
    cv_jobs = [(k, h, r) for r in range(7) for k in "gud" for h in range(2)]
    b_cv = [Buf() for _ in cv_jobs]
    cv_next = [0]

    def emit_conversions(n):
        for _ in range(n):
            if cv_next[0] >= len(cv_jobs):
                return
            j = cv_next[0]
            cv_next[0] += 1
            k, h, r = cv_jobs[j]
            p.dma("pool", ew_bf[k][h].ap()[r * 1024:(r + 1) * 1024, :], ew_src[k][h].ap()[r * 1024:(r + 1) * 1024, :],
                  [], [b_cv[j]])
    h3tok_d = dscr("h3tok_d", [NOWN * TB, D], F32)
    hn3tok_d = dscr("hn3tok_d", [NOWN * TB, D], F32)
    xs_h = nc.dram_tensor("xs_d", [LSLOT, D], F32)
    ys_h = nc.dram_tensor("ys_d", [LSLOT, D], F32)
    xs_d = xs_h.ap()
    ys_d = ys_h.ap()
    g2row_d = dscr("g2row_d", [D], F32)
    b_xs = Buf(); b_ys = [Buf() for _ in range(NBLK)]; b_g2row = Buf()
    b_h3t = [Buf() for _ in range(32)]; b_hn3t = [Buf() for _ in range(32)]
    b_hT = [Buf() for _ in range(NPOS + 4)]
    b_cT = [Buf() for _ in range(NPOS)]
    b_q = Buf(); b_kn = Buf(); b_kpe = Buf(); b_v = Buf()
    b_dT = [Buf() for _ in range(NOWN)]
    dbg_out = {}
    if dbg:
        dbg_out["dbg_h"] = nc.dram_tensor("dbg_h", [D, NCOL], F32, kind="ExternalOutput").ap()

    def bl(xs):
        return [x.b if isinstance(x, Tl) else x for x in xs]

    def MM(out, lhsT, rhs, start, stop, R, W):
        p.op("pe", lambda e: e.matmul(out, lhsT=lhsT, rhs=rhs, start=start, stop=stop), bl(R), bl(W))

    def TR(out, in_, ident, R, W):
        p.op("pe", lambda e: e.transpose(out, in_, ident), bl(R), bl(W))

    def ACT(out, in_, func, R, W, **kw):
        p.op("act", lambda e: e.activation(out=out, in_=in_, func=func, **kw), bl(R), bl(W))

    def TT(eng, out, in0, in1, op, R, W):
        p.op(eng, lambda e: e.tensor_tensor(out=out, in0=in0, in1=in1, op=op), bl(R), bl(W))

    def TS(eng, out, in0, s1, op0, R, W, s2=None, op1=None):
        if op1 is None:
            p.op(eng, lambda e: e.tensor_scalar(out=out, in0=in0, scalar1=s1, scalar2=None, op0=op0), bl(R), bl(W))
        else:
            p.op(eng, lambda e: e.tensor_scalar(out=out, in0=in0, scalar1=s1, scalar2=s2, op0=op0, op1=op1), bl(R), bl(W))

    def STT(eng, out, in0, scalar, in1, op0, op1, R, W):
        p.op(eng, lambda e: e.scalar_tensor_tensor(out=out, in0=in0, scalar=scalar, in1=in1, op0=op0, op1=op1),
             bl(R), bl(W))

    def CP(eng, out, in_, R, W):
        if eng == "act":
            ACT(out, in_, AF.Copy, R, W)
        else:
            p.op(eng, lambda e: e.tensor_copy(out=out, in_=in_), bl(R), bl(W))

    def RCP(out, in_, R, W):
        p.op("dve", lambda e: e.reciprocal(out=out, in_=in_), bl(R), bl(W))

    def RED(out, in_, op, R, W):
        p.op("dve", lambda e: e.tensor_reduce(out=out, in_=in_, axis=AX.X, op=op), bl(R), bl(W))

    def MEMSET(eng, ap, val, W):
        p.op(eng, lambda e: e.memset(ap, val), (), bl(W))

    def DMA(q, out, in_, R, W, **kw):
        return p.dma(q, out, in_, bl(R), bl(W), **kw)

    evac_rr = [0]

    def EVAC(out, in_, R, W):
        evac_rr[0] ^= 1
        CP("act" if evac_rr[0] else "dve", out, in_, R, W)

    with contextlib.ExitStack() as top:
        uid = [0]

        def sbt(st, name, shape, dt):
            uid[0] += 1
            return Tl(st.enter_context(nc.sbuf_tensor("s%d_%s" % (uid[0], name), list(shape), dt)))

        pb = [Tl(top.enter_context(nc.psum_tensor("pb%d" % i, [128, 512], F32))) for i in range(8)]
        acc_rr = [0]

        def nacc():
            acc_rr[0] ^= 1
            return pb[acc_rr[0]]

        ident = sbt(top, "ident", [128, 128], F32)
        ones_f = sbt(top, "ones_f", [128, 128], F32)
        ones_b = sbt(top, "ones_b", [128, 128], BF16)
        vec = sbt(top, "vec", [128, 160], F32)
        modT = [sbt(top, "modT%d" % l, [128, 48], F32) for l in range(2)]
        gsc = sbt(top, "gsc", [128, 32], F32)
        flsw = sbt(top, "flsw", [128, NPOS], F32)
        flcv = sbt(top, "flcv", [128, NPOS], F32)
        flot = sbt(top, "flot", [128, NOWN], F32)
        hcomp = sbt(top, "hcomp", [128, 8, 32], F32)
        MEMSET("pool", ident[:], 0.0, [ident])
        p.op("pool", lambda e: e.affine_select(out=ident[:], in_=ident[:], pattern=[[-1, 128]],
                                               compare_op=ALU.not_equal, fill=1.0, base=0, channel_multiplier=1),
             (), [ident.b])
        MEMSET("pool", ones_f[:], 1.0, [ones_f])
        MEMSET("pool", ones_b[:], 1.0, [ones_b])
        DMA("sp", vec[:], vecs, [], [vec])
        DMA("sp", flsw[:], fl_swa_d, [], [flsw])
        DMA("sp", flcv[:], fl_conv_d, [], [flcv])
        DMA("sp", flot[:], fl_oth_d, [], [flot])
        V_G = [0, 8, 16, 24]
        V_FG = 32
        V_AB = [40, 88]
        V_QG = 136
        V_KG = 140
        V_CW = 142

        with contextlib.ExitStack() as st:
            cT = sbt(st, "cT", [128, 8], F32)
            condT = sbt(st, "condT", [128, 8], F32)
            awp = [sbt(st, "awp%d" % i, [128, 8, 512], F32) for i in range(2)]
            DMA("sp", cT[:], c_pc, [], [cT])
            ACT(condT[:], cT[:], AF.Silu, [cT], [condT])
            for l in range(2):
                mp = pb[2 + l]
                for nb in range(12):
                    a = awp[nb % 2]
                    DMA("sp", a[:], ada_w[l][:, nb * 512:(nb + 1) * 512].rearrange("(c p) f -> p c f", p=128), [], [a])
                    for k4 in range(4):
                        k = nb * 4 + k4
                        for c in range(8):
                            MM(mp[:, k:k + 1], a[:, c, k4 * 128:(k4 + 1) * 128], condT[:, c:c + 1], c == 0, c == 7,
                               [a, condT], [mp])
                TT("dve", modT[l][:], mp[:, 0:48], vec[:, V_AB[l]:V_AB[l] + 48], ALU.add, [mp, vec], [modT[l]])
            for n, (l, so) in enumerate([(0, 8), (0, 32), (1, 8), (1, 32)]):
                TS("dve", gsc[:, n * 8:(n + 1) * 8], modT[l][:, so:so + 8], 1.0, ALU.add, [modT[l]], [gsc])
                TT("dve", gsc[:, n * 8:(n + 1) * 8], gsc[:, n * 8:(n + 1) * 8], vec[:, V_G[n]:V_G[n] + 8], ALU.mult,
                   [vec], [gsc])
        p.barrier()

        def SH(l, which):
            o = 0 if which == 1 else 24
            return modT[l][:, o:o + 8]

        def GATE(l, which):
            o = 16 if which == 1 else 40
            return modT[l][:, o:o + 8]

        def norm_mod(src, KC, N, dfeat, gs_ap, sh_ap, dst, scr):
            sq, tmp, rr = scr
            stat = pb[7]
            for c in range(KC):
                s = sq[c % 2]
                ACT(s[:, :N], src[:, c, :], AF.Square, [src], [s])
                MM(stat[:, :N], ones_f[:], s[:, :N], c == 0, c == KC - 1, [ones_f, s], [stat])
            ACT(rr[:, :N], stat[:, :N], AF.Ln, [stat], [rr], bias=EPS_AP[:, 0:1], scale=1.0 / dfeat)
            ACT(rr[:, :N], rr[:, :N], AF.Exp, [rr], [rr], scale=-0.5)
            for c in range(KC):
                t = tmp[c % 2]
                TT("dve", t[:, :N], src[:, c, :], rr[:, :N], ALU.mult, [src, rr], [t])
                if sh_ap is None:
                    ACT(dst[:, c, :], t[:, :N], AF.Identity, [t, gsc, vec], [dst], scale=gs_ap[:, c:c + 1],
                        bias=ZERO_AP[:, 0:1])
                else:
                    ACT(dst[:, c, :], t[:, :N], AF.Identity, [t, gsc, vec, modT[0], modT[1]], [dst],
                        scale=gs_ap[:, c:c + 1], bias=sh_ap[:, c:c + 1])

        epsT = sbt(top, "epsT", [128, 1], F32)
        MEMSET("pool", epsT[:], EPS, [epsT])
        EPS_AP = epsT.t
        zeroT = sbt(top, "zeroT", [128, 1], F32)
        MEMSET("pool", zeroT[:], 0.0, [zeroT])
        ZERO_AP = zeroT.t

        def load_hT(tile, blk, q="sp"):
            DMA(q, tile[:], hT_d[:, blk * TB:(blk + 1) * TB].rearrange("(c p) n -> p c n", p=128), [b_hT[blk]], [tile])

        def store_hT(tile, blk, q="sp"):
            DMA(q, hT_d[:, blk * TB:(blk + 1) * TB].rearrange("(c p) n -> p c n", p=128), tile[:], [tile], [b_hT[blk]])

        with contextlib.ExitStack() as st:
            Win0 = sbt(st, "Win0", [128, 8, 1792], BF16)
            Wo0 = sbt(st, "Wo0", [128, 8, D], BF16)
            WsT = sbt(st, "WsT", [128, 8, 128], BF16)
            biasT = sbt(st, "biasT", [128, 2, 8 * 128], F32)
            eskf = sbt(st, "eskf", [128, 4, 128], F32)
            esk = sbt(st, "esk", [128, 4], F32)
            lngb = sbt(st, "lngb", [128, 512], F32)
            lnbb = sbt(st, "lnbb", [128, 512], F32)
            bsb = sbt(st, "bsb", [128, 4, 128], F32)
            xt = sbt(st, "xt", [128, 4, D], F32)
            xT2 = None
            hnT2 = None
            sq = [sbt(st, "sq%d" % i, [128, TB], F32) for i in range(2)]
            tmpn = [sbt(st, "tmpn%d" % i, [128, TB], F32) for i in range(2)]
            rr = sbt(st, "rr", [128, TB], F32)
            scr = (sq, tmpn, rr)
            QT = sbt(st, "QT", [128, 4, TB], BF16)
            KTc = sbt(st, "KTc", [128, TB], BF16)
            Vc = sbt(st, "Vc", [128, 4, 128], BF16)
            KTh = sbt(st, "KTh", [128, NPOS, 128], BF16)
            Vh = sbt(st, "Vh", [128, NPOS, 128], BF16)
            KTh2 = sbt(st, "KTh2", [128, NPOS, 128], BF16)
            Vh2 = sbt(st, "Vh2", [128, NPOS, 128], BF16)
            uT = sbt(st, "uT", [128, 4, TB], BF16)
            gg = [sbt(st, "gg%d" % i, [128, 512], F32) for i in range(2)]
            vn = [sbt(st, "vn%d" % i, [128, 512], BF16) for i in range(2)]
            st8 = [sbt(st, "st8_%d" % i, [128, 8], F32) for i in range(2)]
            sstage = sbt(st, "sstage", [128, 4, TB], F32)
            scs = [sbt(st, "scs%d" % i, [128, 512], F32) for i in range(2)]
            PT = [[sbt(st, "PT%d%d" % (g, pc), [128, 512], BF16) for pc in range(2)] for g in range(2)]
            den1 = sbt(st, "den", [128, 512], F32)
            den = [den1, den1]

            dnb = [Tl(den1.t), Tl(den1.t)]
            mixT = sbt(st, "mixT", [128, 8, TB], BF16)

            Wq_v = Win0[:, :, 0:512].rearrange("p c (j g d) -> p c j g d", g=2, d=64)
            for g in range(2):
                for c in range(8):
                    DMA("pool", Wq_v[:, c, :, g, :],
                        w_in0[c * 128:(c + 1) * 128, g * 256:(g + 1) * 256].rearrange("p (j d) -> p j d", d=64),
                        [], [Win0])
            DMA("pool", Win0[:, :, 512:1792], w_in0[:, 512:1792].rearrange("(c p) f -> p c f", p=128), [], [Win0])
            for g in range(2):
                DMA("pool", Wo0[g * 64:(g + 1) * 64, 0:4, :],
                    w_o0[g * 256:(g + 1) * 256, :].rearrange("(j d) m -> d j m", d=64), [], [Wo0])
            DMA("pool", Wo0[:, 4:8, :], w_o0[512:1024, :].rearrange("(c p) m -> p c m", p=128), [], [Wo0])
            DMA("sp", biasT[:], swab_d.rearrange("p (a b) -> p a b", a=2), [], [biasT])
            DMA("sp", esk[:], esk_d, [], [esk])
            DMA("sp", lngb[:], lng_d.partition_broadcast(128), [], [lngb])
            DMA("sp", lnbb[:], lnb_d.partition_broadcast(128), [], [lnbb])
            DMA("sp", bsb[:], bsb_d.rearrange("p (a b) -> p a b", a=4), [], [bsb])
            st_setup = contextlib.ExitStack()
            tri = sbt(st_setup, "tri", [128, 128], F32)
            wsn = sbt(st_setup, "wsn", [128, 8, 128], F32)
            DMA("sp", wsn[:], w_s.rearrange("g t s -> t g s"), [], [wsn])
            MEMSET("pool", tri[:], 1.0, [tri])
            p.op("pool", lambda e: e.affine_select(out=tri[:], in_=tri[:], pattern=[[1, 128]],
                                                   compare_op=ALU.is_ge, fill=0.0, base=0, channel_multiplier=-1),
                 (), [tri.b])
            for g8 in range(8):
                a = nacc()
                TR(a[:, 0:128], wsn[:, g8, :], ident[:], [wsn, ident], [a])
                TT("dve", WsT[:, g8, :], a[:, 0:128], tri[:], ALU.mult, [a, tri], [WsT])
            ACT(esk[:], esk[:], AF.Exp, [esk], [esk])
            for j in range(4):
                TS("dve", eskf[:, j, :], ones_f[:], esk[:, j:j + 1], ALU.mult, [ones_f, esk], [eskf])

            p.barrier()
            st_setup.close()
            xts = [xt, xt]
            xT2 = [sbt(st, "xTa", [128, 8, TB], F32), sbt(st, "xTb", [128, 8, TB], F32)]
            hnT1 = sbt(st, "hnTa", [128, 8, TB], BF16)
            hnT2 = [hnT1, hnT1]
            gg4 = gg + [sbt(st, "gg%d" % i, [128, 512], F32) for i in (2, 3)]
            vn4 = vn + [sbt(st, "vn%d" % i, [128, 512], BF16) for i in (2, 3)]
            st84 = st8 + [sbt(st, "st8_%d" % i, [128, 8], F32) for i in (2, 3)]
            PT2 = [PT] + [[[sbt(st, "PT%s%d%d" % (k, g, pc), [128, 512], BF16) for pc in range(2)] for g in range(2)]
                          for k in "bcd"]
            scs4 = gg4

            def a1_stageA(xt, n):
                xT, hnT = xT2[n % 2], hnT2[n % 2]
                for c in range(8):
                    a = nacc()
                    for s in range(4):
                        TR(a[:, s * 128:(s + 1) * 128], xt[:, s, c * 128:(c + 1) * 128], ident[:], [xt, ident], [a])
                    EVAC(xT[:, c, :], a[:], [a], [xT])
                norm_mod(xT, 8, TB, float(D), gsc[:, 0:8], SH(0, 1), hnT, scr)

            def a1_stageB1(n, kind, idx):
                xT, hnT = xT2[n % 2], hnT2[n % 2]
                a = nacc()
                for c in range(8):
                    MM(a[:], Win0[:, c, 512:640], hnT[:, c, :], c == 0, c == 7, [Win0, hnT], [a])
                EVAC(KTc[:], a[:], [a], [KTc])
                a = nacc()
                for s in range(4):
                    for c in range(8):
                        MM(a[:, s * 128:(s + 1) * 128], hnT[:, c, s * 128:(s + 1) * 128], Win0[:, c, 640:768],
                           c == 0, c == 7, [Win0, hnT], [a])
                EVAC(Vc[:].rearrange("p s d -> p (s d)"), a[:], [a], [Vc])
                if kind == "h2":
                    CP("pool", KTh2[:, idx * 4:(idx + 1) * 4, :].rearrange("p s d -> p (s d)"), KTc[:], [KTc], [KTh2])
                    CP("pool", Vh2[:, idx * 4:(idx + 1) * 4, :], Vc[:], [Vc], [Vh2])
                    return
                if kind == "halo":
                    CP("pool", KTh[:, idx * 4:(idx + 1) * 4, :].rearrange("p s d -> p (s d)"), KTc[:], [KTc], [KTh])
                    CP("pool", Vh[:, idx * 4:(idx + 1) * 4, :], Vc[:], [Vc], [Vh])
                for s in range(4):
                    a = nacc()
                    for c in range(8):
                        MM(a[:], hnT[:, c, s * 128:(s + 1) * 128], Win0[:, c, 1280:1792], c == 0, c == 7,
                           [Win0, hnT], [a])
                    g_, s8, v_ = gg4[s], st84[s], vn4[s]
                    MEMSET("pool", s8[:, 0:2], 0.0, [s8])
                    ACT(g_[:], a[:], AF.Gelu, [a], [g_, s8], accum_out=s8[:, 0:1])
                    ACT(den1[:], g_[:], AF.Square, [g_], [den1, dnb[0], dnb[1], s8], accum_out=s8[:, 1:2])
                R4 = range(4)
                for s in R4:
                    TS("dve", st84[s][:, 2:3], st84[s][:, 0:1], 1.0 / 512, ALU.mult, [st84[s]], [st84[s]])
                for s in R4:
                    TT("dve", st84[s][:, 3:4], st84[s][:, 2:3], st84[s][:, 2:3], ALU.mult, [st84[s]], [st84[s]])
                for s in R4:
                    STT("dve", st84[s][:, 4:5], st84[s][:, 1:2], 1.0 / 512, st84[s][:, 3:4], ALU.mult, ALU.subtract,
                        [st84[s]], [st84[s]])
                for s in R4:
                    ACT(st84[s][:, 5:6], st84[s][:, 4:5], AF.Sqrt, [st84[s]], [st84[s]], bias=EPS_AP[:, 0:1], scale=1.0)
                for s in R4:
                    RCP(st84[s][:, 5:6], st84[s][:, 5:6], [st84[s]], [st84[s]])
                for s in R4:
                    STT("dve", st84[s][:, 6:7], st84[s][:, 2:3], -1.0, st84[s][:, 5:6], ALU.mult, ALU.mult,
                        [st84[s]], [st84[s]])
                for s in R4:
                    ACT(gg4[s][:], gg4[s][:], AF.Identity, [gg4[s], st84[s]], [gg4[s]], scale=st84[s][:, 5:6],
                        bias=st84[s][:, 6:7])
                for s in R4:
                    TT("dve", gg4[s][:], gg4[s][:], lngb[:], ALU.mult, [gg4[s], lngb], [gg4[s]])
                for s in R4:
                    TT("pool", vn4[s][:], gg4[s][:], lnbb[:], ALU.add, [gg4[s], lnbb], [vn4[s]])
                for j in range(4):
                    a = nacc()
                    for c in range(8):
                        MM(a[:], Win0[:, c, j * 128:(j + 1) * 128], hnT[:, c, :], c == 0, c == 7, [Win0, hnT], [a])
                    ACT(QT[:, j, :], a[:], AF.Copy, [a], [QT], scale=0.125)
                for uc in range(4):
                    a = nacc()
                    for c in range(8):
                        MM(a[:], Win0[:, c, 768 + uc * 128:768 + (uc + 1) * 128], hnT[:, c, :], c == 0, c == 7,
                           [Win0, hnT], [a])
                    ACT(uT[:, uc, :], a[:], AF.Gelu, [a], [uT])

            def a1_stageB1b(n, kind, idx):
                if kind == "h2":
                    return
                xT, hnT = xT2[n % 2], hnT2[n % 2]

                def kv_prev(s):
                    if kind == "halo":
                        return KTh2[:, idx * 4 + s, :], Vh2[:, idx * 4 + s, :], [KTh2, Vh2]
                    if s > 0:
                        return KTc[:, (s - 1) * 128:s * 128], Vc[:, s - 1, :], [KTc, Vc]
                    return KTh[:, idx, :], Vh[:, idx, :], [KTh, Vh]

                def swa1(s):
                    ktp, vp, rp = kv_prev(s)
                    ktc = KTc[:, s * 128:(s + 1) * 128]
                    P_ = PT2[s]
                    for g in range(2):
                        r0 = g * 64
                        for pc in range(2):
                            un = (s * 4 + g * 2 + pc) % 4
                            sbk = pb[2 + un]
                            kt = ktp if pc == 0 else ktc
                            for j in range(4):
                                MM(sbk[:, j * 128:(j + 1) * 128], kt[r0:r0 + 64, :], QT[r0:r0 + 64, j, s * 128:(s + 1) * 128],
                                   True, True, [QT, KTc] + rp, [sbk])
                            sc_ = scs4[un]
                            bia = biasT[:, pc, g * 512:(g + 1) * 512]
                            if kind == "main" and s == 0 and pc == 0:
                                STT("dve", sc_[:], sbk[:], flsw[:, idx:idx + 1], bia, ALU.add, ALU.add,
                                    [sbk, flsw, biasT], [sc_])
                            else:
                                TT("dve", sc_[:], sbk[:], bia, ALU.add, [sbk, biasT], [sc_])
                            ACT(P_[g][pc][:], sc_[:], AF.Exp, [sc_], [P_[g][pc]])

                def swa2(s):
                    ktp, vp, rp = kv_prev(s)
                    vc = Vc[:, s, :]
                    P_ = PT2[s]
                    for g in range(2):
                        px, pd = pb[4 + g], pb[6 + g]
                        for j in range(4):
                            js = slice(j * 128, (j + 1) * 128)
                            MM(px[:, js], vp, P_[g][0][:, js], True, False, [P_[g][0], Vc] + rp, [px])
                            MM(px[:, js], vc, P_[g][1][:, js], False, True, [P_[g][1], Vc], [px])
                            MM(pd[:, js], ones_b[:], P_[g][0][:, js], True, False, [P_[g][0], ones_b], [pd])
                            MM(pd[:, js], ones_b[:], P_[g][1][:, js], False, True, [P_[g][1], ones_b], [pd])
                    for g in range(2):
                        r0 = g * 64
                        TT("dve", dnb[g][r0:r0 + 64, :], pb[6 + g][r0:r0 + 64, :],
                           eskf[r0:r0 + 64, :, :].rearrange("p a b -> p (a b)"), ALU.add, [pb[6 + g], eskf], [dnb[g]])
                    for g in range(2):
                        r0 = g * 64
                        ACT(dnb[g][r0:r0 + 64, :], dnb[g][r0:r0 + 64, :], AF.Ln, [dnb[g]], [dnb[g]])
                    for g in range(2):
                        r0 = g * 64
                        ACT(dnb[g][r0:r0 + 64, :], dnb[g][r0:r0 + 64, :], AF.Exp, [dnb[g]], [dnb[g]], scale=-1.0)
                    for g in range(2):
                        r0 = g * 64
                        TT("dve", mixT[r0:r0 + 64, 0:4, s * 128:(s + 1) * 128],
                           pb[4 + g][r0:r0 + 64, :].rearrange("p (a b) -> p a b", a=4),
                           dnb[g][r0:r0 + 64, :].rearrange("p (a b) -> p a b", a=4), ALU.mult, [pb[4 + g], dnb[g]], [mixT])

                for s in range(4):
                    swa1(s)
                    v_ = vn4[s]
                    for pr in range(4):
                        for hf in range(2):
                            MM(pb[hf][:, pr * 128:(pr + 1) * 128], v_[:, pr * 128:(pr + 1) * 128],
                               WsT[:, 2 * pr + hf, :], True, True, [v_, WsT], [pb[hf]])
                    for hf in range(2):
                        r0 = hf * 64
                        TT("dve", sstage[r0:r0 + 64, :, s * 128:(s + 1) * 128],
                           pb[hf][r0:r0 + 64, :].rearrange("p (a b) -> p a b", a=4), bsb[r0:r0 + 64, :, :],
                           ALU.add, [pb[hf], bsb], [sstage])
                for s in range(4):
                    swa2(s)
                TT("pool", mixT[:, 4:8, :], uT[:], sstage[:], ALU.mult, [uT, sstage], [mixT])

            def a1_stageB2(n, kind, idx):
                xT = xT2[n % 2]
                if kind == "h2":
                    return
                for dc in range(8):
                    a = nacc()
                    for mc in range(8):
                        MM(a[:], Wo0[:, mc, dc * 128:(dc + 1) * 128], mixT[:, mc, :], mc == 0, mc == 7, [Wo0, mixT], [a])
                    STT("dve", xT[:, dc, :], a[:], GATE(0, 1)[:, dc:dc + 1], xT[:, dc, :], ALU.mult, ALU.add,
                        [a, modT[0], xT], [xT])
                if kind == "main":
                    store_hT(xT, idx)
                else:
                    for s in range(4):
                        c0 = (idx * 4 + s) * 2
                        CP("pool", hcomp[:, :, c0:c0 + 2], xT[:, :, s * 128 + 126:s * 128 + 128], [xT], [hcomp])

            work = [(xh2[gq * 512:(gq + 1) * 512, :], "h2", gq) for gq in range(4)]
            work += [(xh[gq * 512:(gq + 1) * 512, :], "halo", gq) for gq in range(4)]
            work += [(xm[pos * 512:(pos + 1) * 512, :], "main", pos) for pos in range(NPOS)]

            def x_fetch(n):
                DMA("sp", xts[n % 2][:], work[n][0].rearrange("(s p) d -> p s d", p=128), [], [xts[n % 2]])

            x_fetch(0)
            a1_stageA(xt, 0)
            for n in range(len(work)):
                if n + 1 < len(work):
                    x_fetch(n + 1)
                a1_stageB1(n, work[n][1], work[n][2])
                if n + 1 < len(work):
                    a1_stageA(xt, n + 1)
                a1_stageB1b(n, work[n][1], work[n][2])
                a1_stageB2(n, work[n][1], work[n][2])
                emit_conversions(2)
        p.barrier()

        with contextlib.ExitStack() as st:
            Wg = sbt(st, "Wg", [128, 8, FF0], BF16)
            Wu = sbt(st, "Wu", [128, 8, FF0], BF16)
            Wdp = [sbt(st, "Wdp%d" % i, [128, 22, 128], BF16) for i in range(3)]
            hb = [sbt(st, "hb%d" % i, [128, 8, TB], F32) for i in range(2)]
            hn = sbt(st, "hn2", [128, 8, TB], BF16)
            actT = sbt(st, "actT", [128, 22, TB], BF16)
            sq = [sbt(st, "sq%d" % i, [128, TB], F32) for i in range(2)]
            tmpn = [sbt(st, "tmpn%d" % i, [128, TB], F32) for i in range(2)]
            rr = sbt(st, "rr", [128, TB], F32)
            sg = [sbt(st, "sg%d" % i, [128, TB], F32) for i in range(2)]
            for c in range(8):
                DMA("pool", Wg[:, c, :], wg0[c * 128:(c + 1) * 128, :], [], [Wg])
                DMA("pool", Wu[:, c, :], wu0[c * 128:(c + 1) * 128, :], [], [Wu])
            hnc = sbt(st, "hnc", [128, 8, 32], BF16)
            actc = sbt(st, "actc", [128, 22, 32], BF16)
            wdn = [0]

            def a2_norm(h, hn_t, N):
                norm_mod(h, 8, N, float(D), gsc[:, 8:16], SH(0, 2), hn_t, (sq, tmpn, rr))

            def a2_body(h, hn_t, act_t, N, mid=None):
                for fc in range(22):
                    ag, au = pb[2 + 2 * (fc % 2)], pb[3 + 2 * (fc % 2)]
                    for c in range(8):
                        MM(ag[:, :N], Wg[:, c, fc * 128:(fc + 1) * 128], hn_t[:, c, :], c == 0, c == 7, [Wg, hn_t], [ag])
                    for c in range(8):
                        MM(au[:, :N], Wu[:, c, fc * 128:(fc + 1) * 128], hn_t[:, c, :], c == 0, c == 7, [Wu, hn_t], [au])
                    s_ = sg[fc % 2]
                    ACT(s_[:, :N], ag[:, :N], AF.Silu, [ag], [s_])
                    TT("dve", act_t[:, fc, :], s_[:, :N], au[:, :N], ALU.mult, [s_, au], [act_t])
                if mid is not None:
                    mid()
                for dc in range(8):
                    w = Wdp[wdn[0] % 3]
                    wdn[0] += 1
                    DMA("pool", w[:], wd0[:, dc * 128:(dc + 1) * 128].rearrange("(f p) m -> p f m", p=128), [], [w])
                    a = nacc()
                    for fc in range(22):
                        MM(a[:, :N], w[:, fc, :], act_t[:, fc, :], fc == 0, fc == 21, [w, act_t], [a])
                    STT("dve", h[:, dc, :], a[:, :N], GATE(0, 2)[:, dc:dc + 1], h[:, dc, :], ALU.mult, ALU.add,
                        [a, modT[0], h], [h])

            hnB = sbt(st, "hn2b", [128, 8, TB], BF16)
            hns = [hn, hnB]
            load_hT(hb[0], 0)
            a2_norm(hcomp, hnc, 32)
            a2_body(hcomp, hnc, actc, 32, mid=lambda: a2_norm(hb[0], hns[0], TB))
            for blk in range(NPOS):
                h = hb[blk % 2]
                if blk + 1 < NPOS:
                    load_hT(hb[(blk + 1) % 2], blk + 1)
                    nxt = (lambda b=blk: a2_norm(hb[(b + 1) % 2], hns[(b + 1) % 2], TB))
                else:
                    nxt = None
                a2_body(h, hns[blk % 2], actT, TB, mid=nxt)
                store_hT(h, blk)
                if dbg:
                    DMA("sp", dbg_out["dbg_h"][:, blk * TB:(blk + 1) * TB].rearrange("(c p) n -> p c n", p=128), h[:],
                        [h], [])
        p.barrier()

        with contextlib.ExitStack() as st:
            Win1 = sbt(st, "Win1", [128, 8, 2336], BF16)
            Wkpe = sbt(st, "Wkpe", [128, 8, 96], BF16)
            Wkper = sbt(st, "Wkper", [128, 8, 96], BF16)
            Wqb = sbt(st, "Wqb", [128, 4, 768], BF16)
            Wqbr = sbt(st, "Wqbr", [128, 4, 768], BF16)
            Wkn = sbt(st, "Wkn", [128, 2, 512], BF16)
            Wv = sbt(st, "Wv", [128, 2, 512], BF16)
            hb = [sbt(st, "hb%d" % i, [128, 8, TB], F32) for i in range(2)]
            hn2 = [sbt(st, "hn1", [128, 8, TB], BF16), sbt(st, "hn1b", [128, 8, TB], BF16)]
            hncur = [hn2[0]]
            sq = [sbt(st, "sq%d" % i, [128, TB], F32) for i in range(2)]
            tmpn = [sbt(st, "tmpn%d" % i, [128, TB], F32) for i in range(2)]
            rr = sbt(st, "rr", [128, TB], F32)
            scr = (sq, tmpn, rr)
            gbT = sbt(st, "gbT", [128, 4, TB], F32)
            gcT = sbt(st, "gcT", [128, 4, TB], F32)
            z = sbt(st, "z", [128, 4, TB + 2], F32)
            zh = sbt(st, "zh", [128, 4, NPOS, 2], F32)
            yc = [sbt(st, "yc%d" % i, [128, TB], F32) for i in range(2)]
            cTt = sbt(st, "cTt", [128, 4, TB], BF16)
            qlT = sbt(st, "qlT", [128, 4, TB], F32)
            qnT = sbt(st, "qnT", [128, 4, TB], BF16)
            kvlT = sbt(st, "kvlT", [128, 2, TB], F32)
            kvnT = sbt(st, "kvnT", [128, 2, TB], BF16)
            QTh = [sbt(st, "QTh%d" % i, [128, TB], BF16) for i in range(2)]
            rt = [sbt(st, "rt%d" % i, [128, TB], F32) for i in range(2)]
            knt = [sbt(st, "knt%d" % i, [128, TB], BF16) for i in range(2)]
            vt = sbt(st, "vt", [128, 4, 512], BF16)
            kpe = sbt(st, "kpe", [128, TB], BF16)
            rk = sbt(st, "rk", [128, 2, TB], F32)
            rq = sbt(st, "rq", [128, 2, TB], F32)

            DMA("pool", Win1[:], w_in1.rearrange("(c p) f -> p c f", p=128), [], [Win1])
            MEMSET("pool", Wkpe[:], 0.0, [Wkpe])
            MEMSET("pool", Wkper[:], 0.0, [Wkper])
            MEMSET("pool", Wqbr[:], 0.0, [Wqbr])
            CP("pool", Wkpe[:, :, 64:96], Win1[:, :, 2304:2336], [Win1], [Wkpe])
            TS("pool", Wkper[:, :, 64:80], Win1[:, :, 2320:2336], -1.0, ALU.mult, [Win1], [Wkper])
            CP("pool", Wkper[:, :, 80:96], Win1[:, :, 2304:2320], [Win1], [Wkper])
            DMA("pool", Wqb[:], w_qb.rearrange("(c p) f -> p c f", p=128), [], [Wqb])
            Wqb_v = Wqb[:].rearrange("p c (h e) -> p c h e", e=96)
            Wqbr_v = Wqbr[:].rearrange("p c (h e) -> p c h e", e=96)
            for c in range(4):
                TS("pool", Wqbr_v[:, c, :, 64:80], Wqb_v[:, c, :, 80:96], -1.0, ALU.mult, [Wqb], [Wqbr])
                CP("pool", Wqbr_v[:, c, :, 80:96], Wqb_v[:, c, :, 64:80], [Wqb], [Wqbr])
            kv_v = w_kvb.rearrange("(c p) (h t d) -> p c h t d", p=128, t=2, d=64)
            for c in range(2):
                DMA("pool", Wkn[:, c, :].rearrange("p (h d) -> p h d", d=64), kv_v[:, c, :, 0, :], [], [Wkn])
                DMA("pool", Wv[:, c, :].rearrange("p (h d) -> p h d", d=64), kv_v[:, c, :, 1, :], [], [Wv])
            nqh = [0]

            def proj_fm(col0, nchunk, dst_fn):
                for k in range(nchunk):
                    a = nacc()
                    for c in range(8):
                        MM(a[:], Win1[:, c, col0 + k * 128:col0 + (k + 1) * 128], hncur[0][:, c, :], c == 0, c == 7,
                           [Win1, hncur[0]], [a])
                    dst_fn(k, a)

            hnc1 = sbt(st, "hnc1", [128, 8, 32], BF16)
            gcc = sbt(st, "gcc", [128, 4, 32], F32)
            zh_v = zh[:].rearrange("p c n k -> p c (n k)")
            norm_mod(hcomp, 8, 32, float(D), gsc[:, 16:24], SH(1, 1), hnc1, scr)
            for k in range(4):
                a = nacc()
                for c in range(8):
                    MM(a[:, :32], Win1[:, c, 512 + k * 128:512 + (k + 1) * 128], hnc1[:, c, :], c == 0, c == 7, [Win1, hnc1], [a])
                EVAC(gcc[:, k, :], a[:, :32], [a], [gcc])
            for k in range(4):
                a = nacc()
                for c in range(8):
                    MM(a[:, :32], Win1[:, c, 1024 + k * 128:1024 + (k + 1) * 128], hnc1[:, c, :], c == 0, c == 7, [Win1, hnc1], [a])
                TT("dve", zh_v[:, k, :], gcc[:, k, :], a[:, :32], ALU.mult, [gcc, a], [zh])
            load_hT(hb[0], 0)
            norm_mod(hb[0], 8, TB, float(D), gsc[:, 16:24], SH(1, 1), hn2[0], scr)
            for pos in range(NPOS):
                blk = pos
                h = hb[pos % 2]
                hncur[0] = hn2[pos % 2]
                if pos + 1 < NPOS:
                    load_hT(hb[(pos + 1) % 2], pos + 1)
                proj_fm(512, 4, lambda k, a: EVAC(gcT[:, k, :], a[:], [a], [gcT]))
                proj_fm(1024, 4, lambda k, a: TT("dve", z[:, k, 2:TB + 2], gcT[:, k, :], a[:], ALU.mult, [gcT, a], [z]))
                TS("pool", z[:, :, 0:2], zh[:, :, pos, :], flcv[:, pos:pos + 1], ALU.mult, [zh, flcv], [z])
                proj_fm(0, 4, lambda k, a: EVAC(gbT[:, k, :], a[:], [a], [gbT]))
                for cc in range(4):
                    y_ = yc[cc % 2]
                    TS("dve", y_[:], z[:, cc, 0:TB], vec[:, V_CW + cc:V_CW + cc + 1], ALU.mult, [z, vec], [y_])
                    STT("dve", y_[:], z[:, cc, 1:TB + 1], vec[:, V_CW + 4 + cc:V_CW + 5 + cc], y_[:], ALU.mult, ALU.add,
                        [z, vec, y_], [y_])
                    STT("dve", y_[:], z[:, cc, 2:TB + 2], vec[:, V_CW + 8 + cc:V_CW + 9 + cc], y_[:], ALU.mult, ALU.add,
                        [z, vec, y_], [y_])
                    TT("pool", cTt[:, cc, :], gbT[:, cc, :], y_[:], ALU.mult, [gbT, y_], [cTt])
                DMA("sp", cT_d[:, pos * TB:(pos + 1) * TB].rearrange("(c p) n -> p c n", p=128), cTt[:], [cTt], [b_cT[pos]])
                if pos + 1 < NPOS:
                    norm_mod(hb[(pos + 1) % 2], 8, TB, float(D), gsc[:, 16:24], SH(1, 1), hn2[(pos + 1) % 2], scr)
                DMA("sp", rk[64:96, :, :], ropek_d[:, :, pos * TB:(pos + 1) * TB].rearrange("a r n -> r a n"), [], [rk])
                if pos < NOWN:
                    DMA("sp", rq[64:96, :, :], ropeq_d[:, :, pos * TB:(pos + 1) * TB].rearrange("a r n -> r a n"), [], [rq])
                    proj_fm(1536, 4, lambda k, a: EVAC(qlT[:, k, :], a[:], [a], [qlT]))
                    norm_mod(qlT, 4, TB, 512.0, vec[:, V_QG:V_QG + 4], None, qnT, scr)
                    for hh in range(8):
                        aa, ab = pb[2 + 2 * (hh % 2)], pb[3 + 2 * (hh % 2)]
                        for c in range(4):
                            MM(aa[0:96, :], Wqb[:, c, hh * 96:(hh + 1) * 96], qnT[:, c, :], c == 0, c == 3, [Wqb, qnT], [aa])
                        for c in range(4):
                            MM(ab[0:96, :], Wqbr[:, c, hh * 96:(hh + 1) * 96], qnT[:, c, :], c == 0, c == 3, [Wqbr, qnT], [ab])
                        qt = QTh[nqh[0] % 2]
                        r1, r2 = rt[0], rt[1]
                        nqh[0] += 1
                        ACT(qt[0:64, :], aa[0:64, :], AF.Copy, [aa], [qt], scale=float(96 ** -0.5))
                        TT("dve", r1[64:96, :], aa[64:96, :], rq[64:96, 0, :], ALU.mult, [aa, rq], [r1])
                        TT("dve", r2[64:96, :], ab[64:96, :], rq[64:96, 1, :], ALU.mult, [ab, rq], [r2])
                        TT("pool", qt[64:96, :], r1[64:96, :], r2[64:96, :], ALU.add, [r1, r2], [qt])
                        DMA("sp", qT_d[hh, :, pos * TB:(pos + 1) * TB], qt[0:96, :], [qt], [b_q])
                proj_fm(2048, 2, lambda k, a: EVAC(kvlT[:, k, :], a[:], [a], [kvlT]))
                norm_mod(kvlT, 2, TB, 256.0, vec[:, V_KG:V_KG + 2], None, kvnT, scr)
                for hc in range(4):
                    a = nacc()
                    for c in range(2):
                        MM(a[:], Wkn[:, c, hc * 128:(hc + 1) * 128], kvnT[:, c, :], c == 0, c == 1, [Wkn, kvnT], [a])
                    kt_ = knt[hc % 2]
                    EVAC(kt_[:], a[:], [a], [kt_])
                    DMA("sp", knT_d[hc * 128:(hc + 1) * 128, pos * TB:(pos + 1) * TB], kt_[:], [kt_], [b_kn])
                for s in range(4):
                    a = nacc()
                    for c in range(2):
                        MM(a[:], kvnT[:, c, s * 128:(s + 1) * 128], Wv[:, c, :], c == 0, c == 1, [Wv, kvnT], [a])
                    EVAC(vt[:, s, :], a[:], [a], [vt])
                DMA("sp", v_d[pos * TB:(pos + 1) * TB, :].rearrange("(s p) f -> p s f", p=128), vt[:], [vt], [b_v])
                aa, ab = pb[2], pb[3]
                for c in range(8):
                    MM(aa[0:96, :], Wkpe[:, c, :], hncur[0][:, c, :], c == 0, c == 7, [Wkpe, hncur[0]], [aa])
                for c in range(8):
                    MM(ab[0:96, :], Wkper[:, c, :], hncur[0][:, c, :], c == 0, c == 7, [Wkper, hncur[0]], [ab])
                r1, r2 = rt[0], rt[1]
                TT("dve", r1[64:96, :], aa[64:96, :], rk[64:96, 0, :], ALU.mult, [aa, rk], [r1])
                TT("dve", r2[64:96, :], ab[64:96, :], rk[64:96, 1, :], ALU.mult, [ab, rk], [r2])
                TT("pool", kpe[64:96, :], r1[64:96, :], r2[64:96, :], ALU.add, [r1, r2], [kpe])
                DMA("sp", kpeT_d[:, pos * TB:(pos + 1) * TB], kpe[64:96, :], [kpe], [b_kpe])
        p.barrier()

        with contextlib.ExitStack() as st:
            KT = [sbt(st, "KT%d" % i, [128, S], BF16) for i in range(2)]
            VA = [sbt(st, "VA%d" % i, [128, 64, 128], BF16) for i in range(2)]
            QA = [sbt(st, "QA%d" % i, [128, NOWN * TB], BF16) for i in range(2)]
            PTm = [sbt(st, "PTm%d" % i, [128, TB], BF16) for i in range(4)]
            msk = sbt(st, "msk", [128, 4, TB], BF16)
            ODs = [sbt(st, "ODs%d" % i, [128, TB], F32) for i in range(2)]
            rden = [sbt(st, "rden%d" % i, [128, TB], F32) for i in range(2)]
            dout = [sbt(st, "dout%d" % i, [128, TB], BF16) for i in range(2)]
            SEL = [sbt(st, "SEL%d" % i, [128, 128], F32) for i in range(2)]
            DMA("pool", msk[:], mlam_d.rearrange("p (a b) -> p a b", a=4), [], [msk])
            flob = sbt(st, "flob", [128, NOWN], F32)
            TS("dve", flob[:], flot[:], -NEG, ALU.mult, [flot], [flob], s2=NEG, op1=ALU.add)
            for i_, off in ((0, -64), (1, 64)):
                MEMSET("pool", SEL[i_][:], 0.0, [SEL[i_]])
                p.op("pool", lambda e, t=SEL[i_], off=off: e.affine_select(
                    out=t[:], in_=t[:], pattern=[[-1, 128]], compare_op=ALU.not_equal, fill=1.0, base=off,
                    channel_multiplier=1), (), [SEL[i_].b])
            MEMSET("pool", VA[0][:, :, 64:128], 1.0, [VA[0]])
            MEMSET("pool", VA[1][:, :, 0:64], 1.0, [VA[1]])
            def b2_load(hh):
                par = hh % 2
                kt, va, qa = KT[par], VA[par], QA[par]
                voff = 0 if par == 0 else 64
                DMA("sp", kt[0:64, :], knT_d[hh * 64:(hh + 1) * 64, :], [b_kn], [kt])
                DMA("sp", kt[64:96, :], kpeT_d[:, :], [b_kpe], [kt])
                for q4 in range(4):
                    DMA("sp", va[:, q4 * 16:(q4 + 1) * 16, voff:voff + 64],
                        v_d[q4 * 2048:(q4 + 1) * 2048, hh * 64:(hh + 1) * 64].rearrange("(kb p) d -> p kb d", p=128),
                        [b_v], [va])
                DMA("sp", qa[0:96, :], qT_d[hh, :, :], [b_q], [qa])

            items = []
            for hh in range(8):
                for i in range(NOWN):
                    klist = []
                    for i2 in range(i):
                        for sub in range(4):
                            klist.append((i2, sub, None))
                            klist.append((8 + i2, sub, None))
                    for sub in range(4):
                        klist.append((8 + i, sub, "oth"))
                    for sub in range(4):
                        klist.append((i, sub, "diag"))
                    for n, (pos, sub, kind) in enumerate(klist):
                        items.append((hh, i, n, len(klist), pos, sub, kind))
            sb3 = [pb[1], pb[2], pb[3]]

            def emit_S(t):
                hh, i, n, nk, pos, sub, kind = items[t]
                par = hh % 2
                kt, qa = KT[par], QA[par]
                kc = pos * TB + sub * 128
                sbk = sb3[t % 3]
                pt = PTm[t % 4]
                MM(sbk[:], kt[0:96, kc:kc + 128], qa[0:96, i * TB:(i + 1) * TB], True, True, [kt, qa], [sbk])
                if kind == "oth":
                    ACT(pt[:], sbk[:], AF.Exp, [sbk, flob], [pt], bias=flob[:, i:i + 1], scale=1.0)
                else:
                    ACT(pt[:], sbk[:], AF.Exp, [sbk], [pt])
                if kind == "diag":
                    TT("dve", pt[:], pt[:], msk[:, sub, :], ALU.mult, [pt, msk], [pt])

            b2_load(0)
            SKEW = 2
            for t in range(min(SKEW, len(items))):
                emit_S(t)
            nod = 0
            for t in range(len(items)):
                hh, i, n, nk, pos, sub, kind = items[t]
                par = hh % 2
                va = VA[par]
                if i == 0 and n == 0 and hh + 1 < 8:
                    b2_load(hh + 1)
                if t + SKEW < len(items):
                    emit_S(t + SKEW)
                od = pb[4 + nod % 2]
                MM(od[:], va[:, pos * 4 + sub, :], PTm[t % 4][:], n == 0, n == nk - 1, [va, PTm[t % 4]], [od])
                if n == nk - 1:
                    os_ = ODs[nod % 2]
                    rd = rden[nod % 2]
                    do = dout[nod % 2]
                    nod += 1
                    CP("act", os_[:], od[:], [od], [os_])
                    db = pb[6]
                    MM(db[:], SEL[par][:], os_[:], True, True, [SEL[par], os_], [db])
                    r0 = par * 64
                    RCP(rd[r0:r0 + 64, :], db[r0:r0 + 64, :], [db], [rd])
                    TT("dve", do[r0:r0 + 64, :], os_[r0:r0 + 64, :], rd[r0:r0 + 64, :], ALU.mult, [os_, rd], [do])
                    DMA("sp", dT_d[hh * 64:(hh + 1) * 64, i * TB:(i + 1) * TB], do[r0:r0 + 64, :], [do], [b_dT[i]])
        p.barrier()

        Mall = sbt(top, "Mall", [128, 32, 8], F32)
        Gall = sbt(top, "Gall", [128, 32, 8], F32)
        with contextlib.ExitStack() as st:
            Wo1 = sbt(st, "Wo1", [128, 8, D], BF16)
            wr = sbt(st, "wr", [128, 8, 8], F32)
            brb = sbt(st, "brb", [128, 8], F32)
            mix = [sbt(st, "mix%d" % i, [128, 8, TB], BF16) for i in range(2)]
            hb = [sbt(st, "hb%d" % i, [128, 8, TB], F32) for i in range(2)]
            hnf = sbt(st, "hnf", [128, 8, TB], F32)
            sq = [sbt(st, "sq%d" % i, [128, TB], F32) for i in range(2)]
            tmpn = [sbt(st, "tmpn%d" % i, [128, TB], F32) for i in range(2)]
            rr = sbt(st, "rr", [128, TB], F32)
            lg = [sbt(st, "lg%d" % i, [128, 8], F32) for i in range(2)]
            e8 = [sbt(st, "e8_%d" % i, [128, 8], F32) for i in range(2)]
            s4 = [sbt(st, "s4_%d" % i, [128, 4], F32) for i in range(2)]
            tk = [sbt(st, "tk%d" % i, [128, D], F32) for i in range(4)]
            ntk = [0]
            DMA("pool", Wo1[:], w_o1.rearrange("(c p) m -> p c m", p=128), [], [Wo1])
            DMA("sp", wr[:], w_rt.rearrange("(c p) e -> p c e", p=128), [], [wr])
            DMA("sp", brb[:], brt_d.partition_broadcast(128), [], [brb])
            DMA("sp", g2row_d.rearrange("(c p) -> p c", p=128), modT[1][:, 40:48], [modT[1]], [b_g2row],
                allow_slow_non_contiguous=True)

            def to_rows(src, dst_d, bufs, i):
                for s in range(4):
                    t_ = tk[ntk[0] % 4]
                    ntk[0] += 1
                    for half in range(2):
                        a = nacc()
                        for c4 in range(4):
                            TR(a[:, c4 * 128:(c4 + 1) * 128], src[:, half * 4 + c4, s * 128:(s + 1) * 128], ident[:],
                               [src, ident], [a])
                        EVAC(t_[:, half * 512:(half + 1) * 512], a[:], [a], [t_])
                    ch = i * 4 + s
                    DMA("sp", dst_d[ch * 128:(ch + 1) * 128, :], t_[:], [t_], [bufs[ch]])

            for i in range(NOWN):
                m_ = mix[i % 2]
                h = hb[i % 2]
                DMA("sp", m_[:, 0:4, :], cT_d[:, i * TB:(i + 1) * TB].rearrange("(c p) n -> p c n", p=128), [b_cT[i]], [m_])
                DMA("sp", m_[:, 4:8, :], dT_d[:, i * TB:(i + 1) * TB].rearrange("(c p) n -> p c n", p=128), [b_dT[i]], [m_])
                load_hT(h, i)
                for dc in range(8):
                    a = nacc()
                    for mc in range(8):
                        MM(a[:], Wo1[:, mc, dc * 128:(dc + 1) * 128], m_[:, mc, :], mc == 0, mc == 7, [Wo1, m_], [a])
                    STT("dve", h[:, dc, :], a[:], GATE(1, 1)[:, dc:dc + 1], h[:, dc, :], ALU.mult, ALU.add,
                        [a, modT[1], h], [h])
                to_rows(h, h3tok_d, b_h3t, i)
                norm_mod(h, 8, TB, float(D), gsc[:, 24:32], SH(1, 2), hnf, (sq, tmpn, rr))
                to_rows(hnf, hn3tok_d, b_hn3t, i)
                for s in range(4):
                    ch = i * 4 + s
                    a = nacc()
                    for c in range(8):
                        MM(a[:, 0:8], hnf[:, c, s * 128:(s + 1) * 128], wr[:, c, :], c == 0, c == 7, [hnf, wr], [a])
                    L, E8, S4 = lg[s % 2], e8[s % 2], s4[s % 2]
                    W8 = Mall[:, ch, :]
                    TT("dve", L[:], a[:, 0:8], brb[:], ALU.add, [a, brb], [L])
                    RED(S4[:, 0:1], L[:], ALU.max, [L], [S4])
                    TS("dve", W8, L[:], S4[:, 0:1], ALU.is_equal, [L, S4], [Mall])
                    STT("dve", W8, W8, -1e30, L[:], ALU.mult, ALU.add, [Mall, L], [Mall])
                    RED(S4[:, 1:2], W8, ALU.max, [Mall], [S4])
                    TS("dve", W8, L[:], S4[:, 1:2], ALU.is_ge, [L, S4], [Mall])
                    TS("dve", S4[:, 2:3], S4[:, 0:1], -1.0, ALU.mult, [S4], [S4])
                    ACT(E8[:], L[:], AF.Exp, [L, S4], [E8], bias=S4[:, 2:3], scale=1.0)
                    TT("dve", E8[:], E8[:], W8, ALU.mult, [E8, Mall], [E8])
                    RED(S4[:, 3:4], E8[:], ALU.add, [E8], [S4])
                    RCP(S4[:, 3:4], S4[:, 3:4], [S4], [S4])
                    TS("dve", Gall[:, ch, :], E8[:], S4[:, 3:4], ALU.mult, [E8, S4], [Gall])
        p.barrier()

        I32 = mybir.dt.int32
        idx_hi = sbt(top, "idx_hi", [128, 32], I32)
        idx_lo = sbt(top, "idx_lo", [128, 32], I32)
        g_hi = sbt(top, "g_hi", [128, 32], F32)
        g_lo = sbt(top, "g_lo", [128, 32], F32)
        widx = sbt(top, "widx", [128, NBLK, 7], I32)
        with contextlib.ExitStack() as st:
            cst = sbt(st, "cst", [128, 32], F32)
            Uex = sbt(st, "Uex", [128, 128], F32)
            tot = sbt(st, "tot", [128, 32, 8], F32)
            offs = sbt(st, "offs", [128, 32, 8], F32)
            slot = sbt(st, "slot", [128, 32, 8], F32)
            vidx = sbt(st, "vidx", [128, 32, 8], F32)
            t256 = sbt(st, "t256", [128, 32, 8], F32)
            eqh = sbt(st, "eqh", [128, 32, 8], F32)
            ne = sbt(st, "ne", [128, 8], F32)
            nb8 = sbt(st, "nb8", [128, 8], F32)
            c8 = sbt(st, "c8", [128, 8], F32)
            pend = sbt(st, "pend", [128, 8], F32)
            pstart = sbt(st, "pstart", [128, 8], F32)
            hi_f = sbt(st, "hi_f", [128, 32], F32)
            lo_f = sbt(st, "lo_f", [128, 32], F32)
            gs = sbt(st, "gs", [128, 32], F32)
            be = sbt(st, "be", [128, NBLK], F32)
            t24 = sbt(st, "t24", [128, NBLK], F32)
            wf = sbt(st, "wf", [128, NBLK, 7], F32)
            DMA("sp", cst[:], cst_d, [], [cst])
            MEMSET("pool", Uex[:], 1.0, [Uex])
            p.op("pool", lambda e: e.affine_select(out=Uex[:], in_=Uex[:], pattern=[[1, 128]],
                                                   compare_op=ALU.is_gt, fill=0.0, base=0, channel_multiplier=-1),
                 (), [Uex.b])
            Mf = Mall[:].rearrange("p a b -> p (a b)")
            MM(pb[2][:, 0:256], Uex[:], Mf, True, True, [Uex, Mall], [pb[2]])
            MM(pb[3][:, 0:256], ones_f[:], Mf, True, True, [ones_f, Mall], [pb[3]])
            CP("dve", tot[:].rearrange("p a b -> p (a b)"), pb[3][:, 0:256], [pb[3]], [tot])
            MEMSET("pool", offs[:, 0, :], 0.0, [offs])
            for ch in range(1, 32):
                TT("dve", offs[:, ch, :], offs[:, ch - 1, :], tot[:, ch - 1, :], ALU.add, [offs, tot], [offs])
            TT("dve", ne[:], offs[:, 31, :], tot[:, 31, :], ALU.add, [offs, tot], [ne])
            TS("dve", nb8[:], ne[:], 0.0, ALU.is_gt, [ne], [nb8])
            for k in range(1, 8):
                TS("dve", c8[:], ne[:], 512.0 * k, ALU.is_gt, [ne], [c8])
                TT("dve", nb8[:], nb8[:], c8[:], ALU.add, [nb8, c8], [nb8])
            CP("dve", pend[:, 0:1], nb8[:, 0:1], [nb8], [pend])
            for e in range(1, 8):
                TT("dve", pend[:, e:e + 1], pend[:, e - 1:e], nb8[:, e:e + 1], ALU.add, [pend, nb8], [pend])
            TT("dve", pstart[:], pend[:], nb8[:], ALU.subtract, [pend, nb8], [pstart])
            TS("dve", pstart[:], pstart[:], 512.0, ALU.mult, [pstart], [pstart])
            TT("dve", slot[:].rearrange("p a b -> p (a b)"), pb[2][:, 0:256], offs[:].rearrange("p a b -> p (a b)"),
               ALU.add, [pb[2], offs], [slot])
            for e in range(8):
                TS("dve", slot[:, :, e], slot[:, :, e], pstart[:, e:e + 1], ALU.add, [slot, pstart], [slot])
            TT("dve", vidx[:], slot[:], Mall[:], ALU.mult, [slot, Mall], [vidx])
            RED(hi_f[:], vidx[:], ALU.max, [vidx], [hi_f])
            BIG = 1.0e6
            TS("dve", t256[:], Mall[:], -BIG, ALU.mult, [Mall], [t256], s2=BIG, op1=ALU.add)
            TT("dve", t256[:], t256[:], slot[:], ALU.add, [t256, slot], [t256])
            RED(lo_f[:], t256[:], ALU.min, [t256], [lo_f])
            TS("dve", lo_f[:], lo_f[:], float(LSLOT - 1), ALU.min, [lo_f], [lo_f])
            for e in range(8):
                TT("dve", eqh[:, :, e], vidx[:, :, e], hi_f[:], ALU.is_equal, [vidx, hi_f], [eqh])
            TT("dve", eqh[:], eqh[:], Gall[:], ALU.mult, [eqh, Gall], [eqh])
            RED(g_hi[:], eqh[:], ALU.add, [eqh], [g_hi])
            RED(gs[:], Gall[:], ALU.add, [Gall], [gs])
            TT("dve", g_lo[:], gs[:], g_hi[:], ALU.subtract, [gs, g_hi], [g_lo])
            CP("dve", idx_hi[:], hi_f[:], [hi_f], [idx_hi])
            CP("dve", idx_lo[:], lo_f[:], [lo_f], [idx_lo])
            MEMSET("pool", be[:], 0.0, [be])
            for e in range(8):
                TS("dve", t24[:], cst[:, 0:NBLK], pend[:, e:e + 1], ALU.is_ge, [cst, pend], [t24])
                TT("dve", be[:], be[:], t24[:], ALU.add, [be, t24], [be])
            TS("dve", be[:], be[:], 7.0, ALU.min, [be], [be])
            TS("dve", be[:], be[:], 896.0, ALU.mult, [be], [be], s2=cst[:, 24:25], op1=ALU.add)
            for fg in range(7):
                TS("dve", wf[:, :, fg], be[:], 128.0 * fg, ALU.add, [be], [wf])
            CP("dve", widx[:], wf[:], [wf], [widx])
            xr = [sbt(st, "xr%d" % i, [128, D], F32) for i in range(3)]
            for ch in range(32):
                x_ = xr[ch % 3]
                DMA("sp", x_[:], hn3tok_d[ch * 128:(ch + 1) * 128, :], [b_hn3t[ch]], [x_])
                for it in (idx_hi, idx_lo):
                    p.idma(lambda e, x_=x_, it=it, ch=ch: e.indirect_dma_start(
                        out=xs_h[:, :], out_offset=bass.IndirectOffsetOnAxis(ap=it[:, ch:ch + 1], axis=0),
                        in_=x_[:, :], in_offset=None),
                        [x_.b, it.b], [b_xs])
        p.barrier()

        with contextlib.ExitStack() as st:
            WG = [sbt(st, "WG%d" % i, [128, 8, 512], BF16) for i in range(2)]
            WU = [sbt(st, "WU%d" % i, [128, 8, 512], BF16) for i in range(2)]
            WD = [sbt(st, "WD%d" % i, [128, 4, D], BF16) for i in range(2)]
            XT = [sbt(st, "XT%d" % i, [128, 8, TB], BF16) for i in range(2)]
            Yb = [sbt(st, "Yb%d" % i, [128, 4, D], F32) for i in range(2)]
            xrow = [sbt(st, "xrow%d" % i, [128, D], F32) for i in range(8)]
            actm = [sbt(st, "actm%d" % i, [128, 4, TB], BF16) for i in range(2)]
            sg = [sbt(st, "sgm%d" % i, [128, TB], F32) for i in range(2)]
            nsg = [0]
            nxr = [0]

            def w_load(n):
                b_, fg_ = n // 7, n % 7
                k_ = n % 2
                for (tiles, srcs, v) in ((WG, ew_bf["g"], "g"), (WU, ew_bf["u"], "u"), (WD, ew_bf["d"], "d")):
                    t_ = tiles[k_]
                    for h in range(2):
                        if v == "d":
                            o_ = t_[:, 2 * h:2 * h + 2, :].rearrange("p a b -> p (a b)")
                        else:
                            o_ = t_[:, 4 * h:4 * h + 4, :].rearrange("p a b -> p (a b)")
                        p.idma(lambda e, o_=o_, src=srcs[h], b_=b_, fg_=fg_: e.indirect_dma_start(
                            out=o_, out_offset=None, in_=src[:, :],
                            in_offset=bass.IndirectOffsetOnAxis(ap=widx[:, b_, fg_:fg_ + 1], axis=0)),
                            [widx.b] + b_cv, [t_.b])

            def x_issue(b_):
                for s in range(4):
                    xr_ = xrow[(b_ % 2) * 4 + s]
                    DMA("sp", xr_[:], xs_d[b_ * TB + s * 128:b_ * TB + (s + 1) * 128, :], [b_xs], [xr_])

            def x_trans(b_):
                xt_ = XT[b_ % 2]
                for s in range(4):
                    xr_ = xrow[(b_ % 2) * 4 + s]
                    for half in range(2):
                        a = nacc()
                        for c4 in range(4):
                            TR(a[:, c4 * 128:(c4 + 1) * 128], xr_[:, (half * 4 + c4) * 128:(half * 4 + c4 + 1) * 128], ident[:],
                               [xr_, ident], [a])
                        EVAC(xt_[:, half * 4:(half + 1) * 4, s * 128:(s + 1) * 128],
                             a[:].rearrange("p (a b) -> p a b", a=4), [a], [xt_])

            w_load(0)
            x_issue(0)
            x_trans(0)
            for b_ in range(NBLK):
                xt_ = XT[b_ % 2]
                yb = Yb[b_ % 2]
                if b_ + 1 < NBLK:
                    x_issue(b_ + 1)
                for fg in range(7):
                    n_ = b_ * 7 + fg
                    if fg == 3 and b_ + 1 < NBLK:
                        x_trans(b_ + 1)
                    k_ = n_ % 2
                    wgt, wut, wdt = WG[k_], WU[k_], WD[k_]
                    if n_ + 1 < NBLK * 7:
                        w_load(n_ + 1)
                    am = actm[n_ % 2]
                    for f4 in range(4):
                        ag, au = pb[2 + 2 * (f4 % 2)], pb[3 + 2 * (f4 % 2)]
                        for c in range(8):
                            MM(ag[:], wgt[:, c, f4 * 128:(f4 + 1) * 128], xt_[:, c, :], c == 0, c == 7, [wgt, xt_], [ag])
                        for c in range(8):
                            MM(au[:], wut[:, c, f4 * 128:(f4 + 1) * 128], xt_[:, c, :], c == 0, c == 7, [wut, xt_], [au])
                        s_ = sg[nsg[0] % 2]
                        nsg[0] += 1
                        ACT(s_[:], ag[:], AF.Silu, [ag], [s_])
                        TT("dve", am[:, f4, :], s_[:], au[:], ALU.mult, [s_, au], [am])
                    for s4_ in range(4):
                        for half in range(2):
                            a = nacc()
                            for f4 in range(4):
                                MM(a[:], am[:, f4, s4_ * 128:(s4_ + 1) * 128], wdt[:, f4, half * 512:(half + 1) * 512],
                                   f4 == 0, f4 == 3, [wdt, am], [a])
                            dst = yb[:, s4_, half * 512:(half + 1) * 512]
                            if fg == 0:
                                EVAC(dst, a[:], [a], [yb])
                            else:
                                TT("dve", dst, dst, a[:], ALU.add, [yb, a], [yb])
                DMA("sp", ys_d[b_ * TB:(b_ + 1) * TB, :].rearrange("(s p) m -> p s m", p=128), yb[:], [yb], [b_ys[b_]])
        p.barrier()

        fin = []
        with contextlib.ExitStack() as st:
            g2b = sbt(st, "g2b", [128, D], F32)
            fgb = sbt(st, "fgb", [128, D], F32)
            Yh = [sbt(st, "Yh%d" % i, [128, D], F32) for i in range(2)]
            Yl = [sbt(st, "Yl%d" % i, [128, D], F32) for i in range(2)]
            h3r = [sbt(st, "h3r%d" % i, [128, D], F32) for i in range(2)]
            ot = [sbt(st, "ot%d" % i, [128, D], F32) for i in range(2)]
            junk = sbt(st, "junk", [128, D], F32)
            ss = [sbt(st, "ss%d" % i, [128, 2], F32) for i in range(2)]
            DMA("sp", g2b[:], g2row_d.rearrange("(o n) -> o n", o=1).partition_broadcast(128), [b_g2row], [g2b])
            DMA("sp", fgb[:], fgrow_d.partition_broadcast(128), [], [fgb])
            for ch in range(32):
                yh, yl, hr, o, s2 = Yh[ch % 2], Yl[ch % 2], h3r[ch % 2], ot[ch % 2], ss[ch % 2]
                for (yt, it) in ((yh, idx_hi), (yl, idx_lo)):
                    p.idma(lambda e, yt=yt, it=it, ch=ch: e.indirect_dma_start(
                        out=yt[:, :], out_offset=None, in_=ys_h[:, :],
                        in_offset=bass.IndirectOffsetOnAxis(ap=it[:, ch:ch + 1], axis=0)),
                        [it.b] + b_ys, [yt.b])
                DMA("sp", hr[:], h3tok_d[ch * 128:(ch + 1) * 128, :], [b_h3t[ch]], [hr])
                ACT(yl[:], yl[:], AF.Copy, [yl, g_lo], [yl], scale=g_lo[:, ch:ch + 1])
                STT("dve", yh[:], yh[:], g_hi[:, ch:ch + 1], yl[:], ALU.mult, ALU.add, [yh, g_hi, yl], [yh])
                TT("dve", yh[:], yh[:], g2b[:], ALU.mult, [yh, g2b], [yh])
                TT("dve", hr[:], hr[:], yh[:], ALU.add, [hr, yh], [hr])
                MEMSET("pool", s2[:], 0.0, [s2])
                ACT(junk[:], hr[:], AF.Square, [hr], [junk, s2], accum_out=s2[:, 0:1])
                ACT(s2[:, 1:2], s2[:, 0:1], AF.Sqrt, [s2], [s2], bias=EPS_AP[:, 0:1], scale=1.0 / D)
                RCP(s2[:, 1:2], s2[:, 1:2], [s2], [s2])
                STT("dve", o[:], hr[:], s2[:, 1:2], fgb[:], ALU.mult, ALU.mult, [hr, s2, fgb], [o])
                fin.append(DMA("sp", y_out[ch * 128:(ch + 1) * 128, :], o[:], [o], []))
        for t in fin:
            p.wait_tok("sp", t)
        for t in list(p.last_dma.values()):
            p.wait_tok("sp", t)
        p.emit()
    return nc


def _pc(v, k):
    return np.ascontiguousarray(np.asarray(v, np.float32).reshape(k, 128).T)


def _host_inputs(inp, dbg_cores=None):
    f = lambda k: np.asarray(inp[k], np.float32)
    x = f("x")
    B = x.shape[0]
    slopes = (2.0 ** (-8.0 * np.arange(1, 9, dtype=np.float32) / 8)).astype(np.float32)
    k_i = np.arange(128)[:, None]
    q_i = np.arange(128)[None, :]
    swab = np.zeros((128, 2, 2, 4, 128), np.float32)
    for g in range(2):
        for j in range(4):
            sl = slopes[g * 4 + j]
            dist_prev = q_i + 128 - k_i
            dist_cur = q_i - k_i
            bp = np.where((dist_prev >= 0) & (dist_prev < 128), -sl * dist_prev, NEG)
            bc = np.where((dist_cur >= 0) & (dist_cur < 128), -sl * dist_cur, NEG)
            swab[:, 0, g, j, :] = bp
            swab[:, 1, g, j, :] = bc
    swab = swab.reshape(128, 2 * 8 * 128)
    mlam = np.zeros((128, 4, TB), np.float32)
    for d in range(4):
        mlam[:, d, :] = ((d * 128 + np.arange(128))[:, None] <= np.arange(TB)[None, :]).astype(np.float32)
    mlam = mlam.reshape(128, 4 * TB)
    inv = (10000.0 ** (-np.arange(0, 32, 2, dtype=np.float32) / 32)).astype(np.float32)
    ang = np.arange(S, dtype=np.float32)[:, None] * inv[None, :]
    cosf = np.cos(ang).astype(np.float32).T
    sinf = np.sin(ang).astype(np.float32).T
    cos32 = np.concatenate([cosf, cosf], 0)
    sin32 = np.concatenate([sinf, sinf], 0)
    qs = np.float32(96 ** -0.5)

    sinks = f("even_sinks")[0]
    esk = np.zeros((128, 4), np.float32)
    for g in range(2):
        for j in range(4):
            esk[g * 64:(g + 1) * 64, j] = sinks[g * 4 + j]
    bs = f("even_b_s")[0]
    bsb = np.zeros((128, 4, 128), np.float32)
    for pr in range(4):
        bsb[0:64, pr, :] = bs[2 * pr][None, :]
        bsb[64:128, pr, :] = bs[2 * pr + 1][None, :]
    bsb = bsb.reshape(128, 512)
    vecs_common = np.zeros((128, 160), np.float32)
    vecs_common[:, 0:8] = _pc(f("even_norm_mix_g")[0], 8)
    vecs_common[:, 8:16] = _pc(f("even_norm_ffn_g")[0], 8)
    vecs_common[:, 16:24] = _pc(f("odd_norm_mix_g")[0], 8)
    vecs_common[:, 24:32] = _pc(f("odd_norm_ffn_g")[0], 8)
    vecs_common[:, 32:40] = _pc(f("final_norm_g"), 8)
    vecs_common[:, 40:88] = _pc(f("even_ada_b")[0], 48)
    vecs_common[:, 88:136] = _pc(f("odd_ada_b")[0], 48)
    vecs_common[:, 136:140] = _pc(f("odd_q_norm_g")[0], 4)
    vecs_common[:, 140:142] = _pc(f("odd_kv_norm_g")[0], 2)
    cw = f("odd_conv_w")[0]
    for j in range(3):
        vecs_common[:, 142 + j * 4:142 + (j + 1) * 4] = _pc(cw[j], 4)

    shared = {
        "vecs": vecs_common, "swab": swab, "mlam": mlam, "esk_sink": esk,
        "lng": f("even_gmlp_ln_g")[0][None, :].copy(), "lnb": f("even_gmlp_ln_b")[0][None, :].copy(),
        "bsb": bsb, "brt": f("odd_router_b")[0][None, :].copy(),
        "ada_w0": f("even_ada_w")[0], "ada_w1": f("odd_ada_w")[0],
        "w_in0": f("even_w_in")[0], "w_s": f("even_w_s")[0], "w_o0": f("even_w_o")[0],
        "wg0": f("even_ffn_w_gate")[0], "wu0": f("even_ffn_w_up")[0], "wd0": f("even_ffn_w_down")[0],
        "w_in1": f("odd_w_in")[0], "w_qb": f("odd_w_q_b")[0], "w_kvb": f("odd_w_kv_b")[0], "w_o1": f("odd_w_o")[0],
        "w_rt": f("odd_router_w")[0], "fgrow": f("final_norm_g")[None, :].copy(),
    }
    cst = np.zeros((128, 32), np.float32)
    cst[:, 0:24] = np.arange(24, dtype=np.float32)[None, :]
    cst[:, 24] = np.arange(128, dtype=np.float32)
    shared["cst"] = cst
    for nm, key in (("ewg", "odd_exp_w_gate"), ("ewu", "odd_exp_w_up")):
        w = f(key)[0].reshape(NEXP, 2, 4, 128, 7, 512)
        w = w.transpose(1, 0, 4, 3, 2, 5)
        for h in range(2):
            shared["%s_h%d" % (nm, h)] = w[h].reshape(NEXP * 7 * 128, 2048)
    w = f("odd_exp_w_down")[0].reshape(NEXP, 7, 2, 2, 128, D)
    w = w.transpose(2, 0, 1, 4, 3, 5)
    for h in range(2):
        shared["ewd_h%d" % h] = w[h].reshape(NEXP * 7 * 128, 2048)
    shared = {k: np.ascontiguousarray(v, dtype=np.float32) for k, v in shared.items()}
    cvec = f("c")
    maps = []
    orders = []
    cores = range(2 * B) if dbg_cores is None else dbg_cores
    for core in cores:
        b, hf = core // 2, core % 2
        order = list(OWN[hf]) + list(OWN[1 - hf])
        orders.append((b, order))
        xb = x[b]
        xpad = np.concatenate([np.zeros((256, D), np.float32), xb], 0)
        xm_ = np.concatenate([xb[j * TB:(j + 1) * TB] for j in order], 0)
        xh_ = np.concatenate([xpad[j * TB + 128:j * TB + 256] for j in order], 0)
        xh2_ = np.concatenate([xpad[j * TB:j * TB + 128] for j in order], 0)
        fl_swa = np.zeros((128, NPOS), np.float32)
        fl_conv = np.ones((128, NPOS), np.float32)
        for pos, j in enumerate(order):
            if j == 0:
                fl_swa[:, pos] = NEG
                fl_conv[:, pos] = 0.0
        fl_oth = np.zeros((128, NOWN), np.float32)
        for i in range(NOWN):
            fl_oth[:, i] = 1.0 if order[8 + i] < order[i] else 0.0
        tok = np.concatenate([np.arange(j * TB, (j + 1) * TB) for j in order])
        ropek = np.stack([cos32[:, tok], sin32[:, tok]], 0)
        ropeq = np.stack([cos32[:, tok[:NOWN * TB]] * qs, sin32[:, tok[:NOWN * TB]] * qs], 0)
        m = dict(shared)
        m.update({
            "xm": np.ascontiguousarray(xm_), "xh": np.ascontiguousarray(xh_), "xh2": np.ascontiguousarray(xh2_),
            "c_pc": _pc(cvec[b], 8), "fl_swa": fl_swa, "fl_conv": fl_conv, "fl_oth": fl_oth,
            "ropek": np.ascontiguousarray(ropek, dtype=np.float32),
            "ropeq": np.ascontiguousarray(ropeq, dtype=np.float32),
        })
        maps.append(m)
    return maps, orders


def kernel(**inputs):
    x = np.asarray(inputs["x"])
    B = x.shape[0]
    maps, orders = _host_inputs(inputs)
    nc = build(False)
    res = run_bass_kernel_spmd(nc, maps, core_ids=list(range(len(maps))))
    out = np.zeros((B, S, D), np.float32)
    for core, (b, order) in enumerate(orders):
        y = res.results[core]["y"]
        for i in range(NOWN):
            j = order[i]
            out[b, j * TB:(j + 1) * TB, :] = y[i * TB:(i + 1) * TB, :]
    return out
```

```python
import contextlib
import numpy as np
import concourse.bass as bass
import concourse.mybir as mybir
from concourse.bass_utils import run_bass_kernel_spmd

F32 = mybir.dt.float32
BF16 = mybir.dt.bfloat16
AF = mybir.ActivationFunctionType
ALU = mybir.AluOpType
AX = mybir.AxisListType

NDMA_SLOTS = 8
D = 1024
S = 8192
TB = 512
NPOS = 16
NOWN = 8
EPS = 1e-6
OWN = ([0, 3, 4, 7, 8, 11, 12, 15], [1, 2, 5, 6, 9, 10, 13, 14])
FF0 = 2816
FFE = 3584
NEXP = 8
NEG = -30000.0


class Buf:
    __slots__ = ("lw", "rd")

    def __init__(self):
        self.lw = None
        self.rd = {}


class Prog:
    ENG = ("pe", "act", "dve", "pool", "sp")

    def __init__(self, nc):
        self.nc = nc
        self.ops = {e: [] for e in self.ENG}
        self.cnt = {e: 0 for e in self.ENG}
        self.waited = {e: {} for e in self.ENG}
        self.dma_n = {e: 0 for e in self.ENG}
        self.last_dma = {}

    def _need(self, eng, tok, waits):
        if tok is None:
            return
        key, val, src = tok
        if src == eng and key[0] == "c" and eng == "pe":
            return
        w = self.waited[eng]
        if w.get(key, 0) >= val:
            return
        w[key] = val
        waits.append((key, val))

    def _deps(self, eng, reads, writes):
        waits = []
        for b in reads:
            self._need(eng, b.lw, waits)
        for b in writes:
            self._need(eng, b.lw, waits)
            for t in b.rd.values():
                self._need(eng, t, waits)
        m = {}
        for k, v in waits:
            m[k] = max(m.get(k, 0), v)
        return list(m.items())

    def _mark(self, tok, reads, writes):
        for b in reads:
            o = b.rd.get(tok[0])
            if o is None or o[1] < tok[1]:
                b.rd[tok[0]] = tok
        for b in writes:
            b.lw = tok
            b.rd = {}

    def op(self, eng, fn, reads=(), writes=()):
        waits = self._deps(eng, reads, writes)
        self.cnt[eng] += 1
        key = "c:" + eng
        tok = (key, self.cnt[eng], eng)
        self.ops[eng].append((waits, fn, (key, 1)))
        self._mark(tok, reads, writes)
        return tok

    def dma(self, eng, out, in_, reads=(), writes=(), **kw):
        n = self.dma_n[eng]
        self.dma_n[eng] += 1
        slot = n % NDMA_SLOTS
        key = "d:%s:%d" % (eng, slot)
        val = 16 * (n // NDMA_SLOTS + 1)
        waits = self._deps(eng, reads, writes)
        if n >= NDMA_SLOTS:
            pv = val - 16
            if self.waited[eng].get(key, 0) < pv:
                self.waited[eng][key] = pv
                waits.append((key, pv))
        tok = (key, val, eng)
        self.last_dma[key] = tok

        def fn(e, out=out, in_=in_, kw=kw):
            return e.dma_start(out=out, in_=in_, **kw)
        self.ops[eng].append((waits, fn, (key, 16)))
        self._mark(tok, reads, writes)
        return tok

    def idma(self, fn, reads=(), writes=()):
        eng = "pool"
        n = self.dma_n[eng]
        self.dma_n[eng] += 1
        slot = n % NDMA_SLOTS
        key = "d:%s:%d" % (eng, slot)
        val = 16 * (n // NDMA_SLOTS + 1)
        waits = self._deps(eng, reads, writes)
        if n >= NDMA_SLOTS:
            pv = val - 16
            if self.waited[eng].get(key, 0) < pv:
                self.waited[eng][key] = pv
                waits.append((key, pv))
        tok = (key, val, eng)
        self.last_dma[key] = tok
        self.ops[eng].append((waits, fn, (key, 16)))
        self._mark(tok, reads, writes)
        return tok

    def wait_tok(self, eng, tok):
        waits = []
        self._need(eng, tok, waits)
        if waits:
            self.ops[eng].append((waits, None, None))

    def barrier(self):
        toks = [("c:" + e, self.cnt[e], e) for e in self.ENG if self.cnt[e] > 0]
        toks += list(self.last_dma.values())
        for e in self.ENG:
            for t in toks:
                self.wait_tok(e, t)

    def emit(self):
        nc = self.nc
        keys = set()
        for e in self.ENG:
            for waits, fn, inc in self.ops[e]:
                for k, _ in waits:
                    keys.add(k)
                if inc is not None:
                    keys.add(inc[0])
        sems = {}
        with contextlib.ExitStack() as st:
            for k in sorted(keys):
                sems[k] = st.enter_context(nc.semaphore(k.replace(":", "_")))
            block = st.enter_context(nc.Block())

            def run(e, lst):
                for waits, fn, inc in lst:
                    for k, v in waits:
                        e.wait_ge(sems[k], v)
                    if fn is not None:
                        ins = fn(e)
                        ins.then_inc(sems[inc[0]], inc[1])

            @block.tensor
            def _(e):
                run(e, self.ops["pe"])

            @block.scalar
            def _(e):
                run(e, self.ops["act"])

            @block.vector
            def _(e):
                run(e, self.ops["dve"])

            @block.gpsimd
            def _(e):
                run(e, self.ops["pool"])

            @block.sync
            def _(e):
                run(e, self.ops["sp"])


class Tl:
    __slots__ = ("t", "b")

    def __init__(self, t):
        self.t = t
        self.b = Buf()

    def __getitem__(self, k):
        return self.t[k]


def build(dbg=False):
    nc = bass.Bass("TRN2", target_bir_lowering=False)
    p = Prog(nc)

    def din(name, shape):
        return nc.dram_tensor(name, list(shape), F32, kind="ExternalInput").ap()

    def dscr(name, shape, dt):
        return nc.dram_tensor(name, list(shape), dt).ap()

    xm = din("xm", [NPOS * TB, D])
    xh = din("xh", [NPOS * 128, D])
    xh2 = din("xh2", [NPOS * 128, D])
    c_pc = din("c_pc", [128, 8])
    vecs = din("vecs", [128, 160])
    fl_swa_d = din("fl_swa", [128, NPOS])
    fl_conv_d = din("fl_conv", [128, NPOS])
    fl_oth_d = din("fl_oth", [128, NOWN])
    ropek_d = din("ropek", [2, 32, S])
    ropeq_d = din("ropeq", [2, 32, NOWN * TB])
    swab_d = din("swab", [128, 2 * 8 * 128])
    mlam_d = din("mlam", [128, 4 * TB])
    esk_d = din("esk_sink", [128, 4])
    lng_d = din("lng", [1, 512])
    lnb_d = din("lnb", [1, 512])
    bsb_d = din("bsb", [128, 4 * 128])
    brt_d = din("brt", [1, 8])
    ada_w = [din("ada_w0", [D, 6 * D]), din("ada_w1", [D, 6 * D])]
    w_in0 = din("w_in0", [D, 1792])
    w_s = din("w_s", [8, 128, 128])
    w_o0 = din("w_o0", [D, D])
    wg0 = din("wg0", [D, FF0])
    wu0 = din("wu0", [D, FF0])
    wd0 = din("wd0", [FF0, D])
    w_in1 = din("w_in1", [D, 2336])
    w_qb = din("w_qb", [512, 768])
    w_kvb = din("w_kvb", [256, 1024])
    w_o1 = din("w_o1", [D, D])
    w_rt = din("w_rt", [D, 8])
    NWR = NEXP * 7 * 128
    ewg_h = [nc.dram_tensor("ewg_h%d" % h, [NWR, 2048], F32, kind="ExternalInput") for h in range(2)]
    ewu_h = [nc.dram_tensor("ewu_h%d" % h, [NWR, 2048], F32, kind="ExternalInput") for h in range(2)]
    ewd_h = [nc.dram_tensor("ewd_h%d" % h, [NWR, 2048], F32, kind="ExternalInput") for h in range(2)]
    cst_d = din("cst", [128, 32])
    fgrow_d = din("fgrow", [1, D])
    y_out = nc.dram_tensor("y", [NOWN * TB, D], F32, kind="ExternalOutput").ap()

    NCOL = NPOS * TB + NPOS * 128
    hT_d = dscr("hT_d", [D, NCOL], F32)
    cT_d = dscr("cT_d", [512, NPOS * TB], BF16)
    qT_d = dscr("qT_d", [8, 96, NOWN * TB], BF16)
    knT_d = dscr("knT_d", [512, S], BF16)
    kpeT_d = dscr("kpeT_d", [32, S], BF16)
    v_d = dscr("v_d", [S, 512], BF16)
    dT_d = dscr("dT_d", [512, NOWN * TB], BF16)
    NBLK = 23
    LSLOT = NBLK * TB
    ew_src = {"g": ewg_h, "u": ewu_h, "d": ewd_h}
    ew_bf = {k: [nc.dram_tensor("ew%s_b%d" % (k, h), [NWR, 2048], BF16) for h in range(2)] for k in "gud"}
    cv_jobs = [(k, h, r) for r in range(7) for k in "gud" for h in range(2)]
    b_cv = [Buf() for _ in cv_jobs]
    cv_next = [0]

    def emit_conversions(n):
        for _ in range(n):
            if cv_next[0] >= len(cv_jobs):
                return
            j = cv_next[0]
            cv_next[0] += 1
            k, h, r = cv_jobs[j]
            p.dma("pool", ew_bf[k][h].ap()[r * 1024:(r + 1) * 1024, :], ew_src[k][h].ap()[r * 1024:(r + 1) * 1024, :],
                  [], [b_cv[j]])
    h3tok_d = dscr("h3tok_d", [NOWN * TB, D], F32)
    hn3tok_d = dscr("hn3tok_d", [NOWN * TB, D], F32)
    xs_h = nc.dram_tensor("xs_d", [LSLOT, D], F32)
    ys_h = nc.dram_tensor("ys_d", [LSLOT, D], F32)
    xs_d = xs_h.ap()
    ys_d = ys_h.ap()
    g2row_d = dscr("g2row_d", [D], F32)
    b_xs = Buf(); b_ys = [Buf() for _ in range(NBLK)]; b_g2row = Buf()
    b_h3t = [Buf() for _ in range(32)]; b_hn3t = [Buf() for _ in range(32)]
    b_hT = [Buf() for _ in range(NPOS + 4)]
    b_cT = [Buf() for _ in range(NPOS)]
    b_q = Buf(); b_kn = Buf(); b_kpe = Buf(); b_v = Buf()
    b_dT = [Buf() for _ in range(NOWN)]
    dbg_out = {}
    if dbg:
        dbg_out["dbg_h"] = nc.dram_tensor("dbg_h", [D, NCOL], F32, kind="ExternalOutput").ap()

    def bl(xs):
        return [x.b if isinstance(x, Tl) else x for x in xs]

    def MM(out, lhsT, rhs, start, stop, R, W):
        p.op("pe", lambda e: e.matmul(out, lhsT=lhsT, rhs=rhs, start=start, stop=stop), bl(R), bl(W))

    def TR(out, in_, ident, R, W):
        p.op("pe", lambda e: e.transpose(out, in_, ident), bl(R), bl(W))

    def ACT(out, in_, func, R, W, **kw):
        p.op("act", lambda e: e.activation(out=out, in_=in_, func=func, **kw), bl(R), bl(W))

    def TT(eng, out, in0, in1, op, R, W):
        p.op(eng, lambda e: e.tensor_tensor(out=out, in0=in0, in1=in1, op=op), bl(R), bl(W))

    def TS(eng, out, in0, s1, op0, R, W, s2=None, op1=None):
        if op1 is None:
            p.op(eng, lambda e: e.tensor_scalar(out=out, in0=in0, scalar1=s1, scalar2=None, op0=op0), bl(R), bl(W))
        else:
            p.op(eng, lambda e: e.tensor_scalar(out=out, in0=in0, scalar1=s1, scalar2=s2, op0=op0, op1=op1), bl(R), bl(W))

    def STT(eng, out, in0, scalar, in1, op0, op1, R, W):
        p.op(eng, lambda e: e.scalar_tensor_tensor(out=out, in0=in0, scalar=scalar, in1=in1, op0=op0, op1=op1),
             bl(R), bl(W))

    def CP(eng, out, in_, R, W):
        if eng == "act":
            ACT(out, in_, AF.Copy, R, W)
        else:
            p.op(eng, lambda e: e.tensor_copy(out=out, in_=in_), bl(R), bl(W))

    def RCP(out, in_, R, W):
        p.op("dve", lambda e: e.reciprocal(out=out, in_=in_), bl(R), bl(W))

    def RED(out, in_, op, R, W):
        p.op("dve", lambda e: e.tensor_reduce(out=out, in_=in_, axis=AX.X, op=op), bl(R), bl(W))

    def MEMSET(eng, ap, val, W):
        p.op(eng, lambda e: e.memset(ap, val), (), bl(W))

    def DMA(q, out, in_, R, W, **kw):
        return p.dma(q, out, in_, bl(R), bl(W), **kw)

    evac_rr = [0]

    def EVAC(out, in_, R, W):
        evac_rr[0] ^= 1
        CP("act" if evac_rr[0] else "dve", out, in_, R, W)

    with contextlib.ExitStack() as top:
        uid = [0]

        def sbt(st, name, shape, dt):
            uid[0] += 1
            return Tl(st.enter_context(nc.sbuf_tensor("s%d_%s" % (uid[0], name), list(shape), dt)))

        pb = [Tl(top.enter_context(nc.psum_tensor("pb%d" % i, [128, 512], F32))) for i in range(8)]
        acc_rr = [0]

        def nacc():
            acc_rr[0] ^= 1
            return pb[acc_rr[0]]

        ident = sbt(top, "ident", [128, 128], F32)
        ones_f = sbt(top, "ones_f", [128, 128], F32)
        ones_b = sbt(top, "ones_b", [128, 128], BF16)
        vec = sbt(top, "vec", [128, 160], F32)
        modT = [sbt(top, "modT%d" % l, [128, 48], F32) for l in range(2)]
        gsc = sbt(top, "gsc", [128, 32], F32)
        flsw = sbt(top, "flsw", [128, NPOS], F32)
        flcv = sbt(top, "flcv", [128, NPOS], F32)
        flot = sbt(top, "flot", [128, NOWN], F32)
        hcomp = sbt(top, "hcomp", [128, 8, 32], F32)
        MEMSET("pool", ident[:], 0.0, [ident])
        p.op("pool", lambda e: e.affine_select(out=ident[:], in_=ident[:], pattern=[[-1, 128]],
                                               compare_op=ALU.not_equal, fill=1.0, base=0, channel_multiplier=1),
             (), [ident.b])
        MEMSET("pool", ones_f[:], 1.0, [ones_f])
        MEMSET("pool", ones_b[:], 1.0, [ones_b])
        DMA("sp", vec[:], vecs, [], [vec])
        DMA("sp", flsw[:], fl_swa_d, [], [flsw])
        DMA("sp", flcv[:], fl_conv_d, [], [flcv])
        DMA("sp", flot[:], fl_oth_d, [], [flot])
        V_G = [0, 8, 16, 24]
        V_FG = 32
        V_AB = [40, 88]
        V_QG = 136
        V_KG = 140
        V_CW = 142

        with contextlib.ExitStack() as st:
            cT = sbt(st, "cT", [128, 8], F32)
            condT = sbt(st, "condT", [128, 8], F32)
            awp = [sbt(st, "awp%d" % i, [128, 8, 512], F32) for i in range(2)]
            DMA("sp", cT[:], c_pc, [], [cT])
            ACT(condT[:], cT[:], AF.Silu, [cT], [condT])
            for l in range(2):
                mp = pb[2 + l]
                for nb in range(12):
                    a = awp[nb % 2]
                    DMA("sp", a[:], ada_w[l][:, nb * 512:(nb + 1) * 512].rearrange("(c p) f -> p c f", p=128), [], [a])
                    for k4 in range(4):
                        k = nb * 4 + k4
                        for c in range(8):
                            MM(mp[:, k:k + 1], a[:, c, k4 * 128:(k4 + 1) * 128], condT[:, c:c + 1], c == 0, c == 7,
                               [a, condT], [mp])
                TT("dve", modT[l][:], mp[:, 0:48], vec[:, V_AB[l]:V_AB[l] + 48], ALU.add, [mp, vec], [modT[l]])
            for n, (l, so) in enumerate([(0, 8), (0, 32), (1, 8), (1, 32)]):
                TS("dve", gsc[:, n * 8:(n + 1) * 8], modT[l][:, so:so + 8], 1.0, ALU.add, [modT[l]], [gsc])
                TT("dve", gsc[:, n * 8:(n + 1) * 8], gsc[:, n * 8:(n + 1) * 8], vec[:, V_G[n]:V_G[n] + 8], ALU.mult,
                   [vec], [gsc])
        p.barrier()

        def SH(l, which):
            o = 0 if which == 1 else 24
            return modT[l][:, o:o + 8]

        def GATE(l, which):
            o = 16 if which == 1 else 40
            return modT[l][:, o:o + 8]

        def norm_mod(src, KC, N, dfeat, gs_ap, sh_ap, dst, scr):
            sq, tmp, rr = scr
            stat = pb[7]
            for c in range(KC):
                s = sq[c % 2]
                ACT(s[:, :N], src[:, c, :], AF.Square, [src], [s])
                MM(stat[:, :N], ones_f[:], s[:, :N], c == 0, c == KC - 1, [ones_f, s], [stat])
            ACT(rr[:, :N], stat[:, :N], AF.Ln, [stat], [rr], bias=EPS_AP[:, 0:1], scale=1.0 / dfeat)
            ACT(rr[:, :N], rr[:, :N], AF.Exp, [rr], [rr], scale=-0.5)
            for c in range(KC):
                t = tmp[c % 2]
                TT("dve", t[:, :N], src[:, c, :], rr[:, :N], ALU.mult, [src, rr], [t])
                if sh_ap is None:
                    ACT(dst[:, c, :], t[:, :N], AF.Identity, [t, gsc, vec], [dst], scale=gs_ap[:, c:c + 1],
                        bias=ZERO_AP[:, 0:1])
                else:
                    ACT(dst[:, c, :], t[:, :N], AF.Identity, [t, gsc, vec, modT[0], modT[1]], [dst],
                        scale=gs_ap[:, c:c + 1], bias=sh_ap[:, c:c + 1])

        epsT = sbt(top, "epsT", [128, 1], F32)
        MEMSET("pool", epsT[:], EPS, [epsT])
        EPS_AP = epsT.t
        zeroT = sbt(top, "zeroT", [128, 1], F32)
        MEMSET("pool", zeroT[:], 0.0, [zeroT])
        ZERO_AP = zeroT.t

        def load_hT(tile, blk, q="sp"):
            DMA(q, tile[:], hT_d[:, blk * TB:(blk + 1) * TB].rearrange("(c p) n -> p c n", p=128), [b_hT[blk]], [tile])

        def store_hT(tile, blk, q="sp"):
            DMA(q, hT_d[:, blk * TB:(blk + 1) * TB].rearrange("(c p) n -> p c n", p=128), tile[:], [tile], [b_hT[blk]])

        with contextlib.ExitStack() as st:
            Win0 = sbt(st, "Win0", [128, 8, 1792], BF16)
            Wo0 = sbt(st, "Wo0", [128, 8, D], BF16)
            WsT = sbt(st, "WsT", [128, 8, 128], BF16)
            biasT = sbt(st, "biasT", [128, 2, 8 * 128], F32)
            eskf = sbt(st, "eskf", [128, 4, 128], F32)
            esk = sbt(st, "esk", [128, 4], F32)
            lngb = sbt(st, "lngb", [128, 512], F32)
            lnbb = sbt(st, "lnbb", [128, 512], F32)
            bsb = sbt(st, "bsb", [128, 4, 128], F32)
            xt = sbt(st, "xt", [128, 4, D], F32)
            xT2 = None
            hnT2 = None
            sq = [sbt(st, "sq%d" % i, [128, TB], F32) for i in range(2)]
            tmpn = [sbt(st, "tmpn%d" % i, [128, TB], F32) for i in range(2)]
            rr = sbt(st, "rr", [128, TB], F32)
            scr = (sq, tmpn, rr)
            QT = sbt(st, "QT", [128, 4, TB], BF16)
            KTc = sbt(st, "KTc", [128, TB], BF16)
            Vc = sbt(st, "Vc", [128, 4, 128], BF16)
            KTh = sbt(st, "KTh", [128, NPOS, 128], BF16)
            Vh = sbt(st, "Vh", [128, NPOS, 128], BF16)
            KTh2 = sbt(st, "KTh2", [128, NPOS, 128], BF16)
            Vh2 = sbt(st, "Vh2", [128, NPOS, 128], BF16)
            uT = sbt(st, "uT", [128, 4, TB], BF16)
            gg = [sbt(st, "gg%d" % i, [128, 512], F32) for i in range(2)]
            vn = [sbt(st, "vn%d" % i, [128, 512], BF16) for i in range(2)]
            st8 = [sbt(st, "st8_%d" % i, [128, 8], F32) for i in range(2)]
            sstage = sbt(st, "sstage", [128, 4, TB], F32)
            scs = [sbt(st, "scs%d" % i, [128, 512], F32) for i in range(2)]
            PT = [[sbt(st, "PT%d%d" % (g, pc), [128, 512], BF16) for pc in range(2)] for g in range(2)]
            den1 = sbt(st, "den", [128, 512], F32)
            den = [den1, den1]

            dnb = [Tl(den1.t), Tl(den1.t)]
            mixT = sbt(st, "mixT", [128, 8, TB], BF16)

            Wq_v = Win0[:, :, 0:512].rearrange("p c (j g d) -> p c j g d", g=2, d=64)
            for g in range(2):
                for c in range(8):
                    DMA("pool", Wq_v[:, c, :, g, :],
                        w_in0[c * 128:(c + 1) * 128, g * 256:(g + 1) * 256].rearrange("p (j d) -> p j d", d=64),
                        [], [Win0])
            DMA("pool", Win0[:, :, 512:1792], w_in0[:, 512:1792].rearrange("(c p) f -> p c f", p=128), [], [Win0])
            for g in range(2):
                DMA("pool", Wo0[g * 64:(g + 1) * 64, 0:4, :],
                    w_o0[g * 256:(g + 1) * 256, :].rearrange("(j d) m -> d j m", d=64), [], [Wo0])
            DMA("pool", Wo0[:, 4:8, :], w_o0[512:1024, :].rearrange("(c p) m -> p c m", p=128), [], [Wo0])
            DMA("sp", biasT[:], swab_d.rearrange("p (a b) -> p a b", a=2), [], [biasT])
            DMA("sp", esk[:], esk_d, [], [esk])
            DMA("sp", lngb[:], lng_d.partition_broadcast(128), [], [lngb])
            DMA("sp", lnbb[:], lnb_d.partition_broadcast(128), [], [lnbb])
            DMA("sp", bsb[:], bsb_d.rearrange("p (a b) -> p a b", a=4), [], [bsb])
            st_setup = contextlib.ExitStack()
            tri = sbt(st_setup, "tri", [128, 128], F32)
            wsn = sbt(st_setup, "wsn", [128, 8, 128], F32)
            DMA("sp", wsn[:], w_s.rearrange("g t s -> t g s"), [], [wsn])
            MEMSET("pool", tri[:], 1.0, [tri])
            p.op("pool", lambda e: e.affine_select(out=tri[:], in_=tri[:], pattern=[[1, 128]],
                                                   compare_op=ALU.is_ge, fill=0.0, base=0, channel_multiplier=-1),
                 (), [tri.b])
            for g8 in range(8):
                a = nacc()
                TR(a[:, 0:128], wsn[:, g8, :], ident[:], [wsn, ident], [a])
                TT("dve", WsT[:, g8, :], a[:, 0:128], tri[:], ALU.mult, [a, tri], [WsT])
            ACT(esk[:], esk[:], AF.Exp, [esk], [esk])
            for j in range(4):
                TS("dve", eskf[:, j, :], ones_f[:], esk[:, j:j + 1], ALU.mult, [ones_f, esk], [eskf])

            p.barrier()
            st_setup.close()
            xts = [xt, xt]
            xT2 = [sbt(st, "xTa", [128, 8, TB], F32), sbt(st, "xTb", [128, 8, TB], F32)]
            hnT1 = sbt(st, "hnTa", [128, 8, TB], BF16)
            hnT2 = [hnT1, hnT1]
            gg4 = gg + [sbt(st, "gg%d" % i, [128, 512], F32) for i in (2, 3)]
            vn4 = vn + [sbt(st, "vn%d" % i, [128, 512], BF16) for i in (2, 3)]
            st84 = st8 + [sbt(st, "st8_%d" % i, [128, 8], F32) for i in (2, 3)]
            PT2 = [PT] + [[[sbt(st, "PT%s%d%d" % (k, g, pc), [128, 512], BF16) for pc in range(2)] for g in range(2)]
                          for k in "bcd"]
            scs4 = gg4

            def a1_stageA(xt, n):
                xT, hnT = xT2[n % 2], hnT2[n % 2]
                for c in range(8):
                    a = nacc()
                    for s in range(4):
                        TR(a[:, s * 128:(s + 1) * 128], xt[:, s, c * 128:(c + 1) * 128], ident[:], [xt, ident], [a])
                    EVAC(xT[:, c, :], a[:], [a], [xT])
                norm_mod(xT, 8, TB, float(D), gsc[:, 0:8], SH(0, 1), hnT, scr)

            def a1_stageB1(n, kind, idx):
                xT, hnT = xT2[n % 2], hnT2[n % 2]
                a = nacc()
                for c in range(8):
                    MM(a[:], Win0[:, c, 512:640], hnT[:, c, :], c == 0, c == 7, [Win0, hnT], [a])
                EVAC(KTc[:], a[:], [a], [KTc])
                a = nacc()
                for s in range(4):
                    for c in range(8):
                        MM(a[:, s * 128:(s + 1) * 128], hnT[:, c, s * 128:(s + 1) * 128], Win0[:, c, 640:768],
                           c == 0, c == 7, [Win0, hnT], [a])
                EVAC(Vc[:].rearrange("p s d -> p (s d)"), a[:], [a], [Vc])
                if kind == "h2":
                    CP("pool", KTh2[:, idx * 4:(idx + 1) * 4, :].rearrange("p s d -> p (s d)"), KTc[:], [KTc], [KTh2])
                    CP("pool", Vh2[:, idx * 4:(idx + 1) * 4, :], Vc[:], [Vc], [Vh2])
                    return
                if kind == "halo":
                    CP("pool", KTh[:, idx * 4:(idx + 1) * 4, :].rearrange("p s d -> p (s d)"), KTc[:], [KTc], [KTh])
                    CP("pool", Vh[:, idx * 4:(idx + 1) * 4, :], Vc[:], [Vc], [Vh])
                for s in range(4):
                    a = nacc()
                    for c in range(8):
                        MM(a[:], hnT[:, c, s * 128:(s + 1) * 128], Win0[:, c, 1280:1792], c == 0, c == 7,
                           [Win0, hnT], [a])
                    g_, s8, v_ = gg4[s], st84[s], vn4[s]
                    MEMSET("pool", s8[:, 0:2], 0.0, [s8])
                    ACT(g_[:], a[:], AF.Gelu, [a], [g_, s8], accum_out=s8[:, 0:1])
                    ACT(den1[:], g_[:], AF.Square, [g_], [den1, dnb[0], dnb[1], s8], accum_out=s8[:, 1:2])
                R4 = range(4)
                for s in R4:
                    TS("dve", st84[s][:, 2:3], st84[s][:, 0:1], 1.0 / 512, ALU.mult, [st84[s]], [st84[s]])
                for s in R4:
                    TT("dve", st84[s][:, 3:4], st84[s][:, 2:3], st84[s][:, 2:3], ALU.mult, [st84[s]], [st84[s]])
                for s in R4:
                    STT("dve", st84[s][:, 4:5], st84[s][:, 1:2], 1.0 / 512, st84[s][:, 3:4], ALU.mult, ALU.subtract,
                        [st84[s]], [st84[s]])
                for s in R4:
                    ACT(st84[s][:, 5:6], st84[s][:, 4:5], AF.Sqrt, [st84[s]], [st84[s]], bias=EPS_AP[:, 0:1], scale=1.0)
                for s in R4:
                    RCP(st84[s][:, 5:6], st84[s][:, 5:6], [st84[s]], [st84[s]])
                for s in R4:
                    STT("dve", st84[s][:, 6:7], st84[s][:, 2:3], -1.0, st84[s][:, 5:6], ALU.mult, ALU.mult,
                        [st84[s]], [st84[s]])
                for s in R4:
                    ACT(gg4[s][:], gg4[s][:], AF.Identity, [gg4[s], st84[s]], [gg4[s]], scale=st84[s][:, 5:6],
                        bias=st84[s][:, 6:7])
                for s in R4:
                    TT("dve", gg4[s][:], gg4[s][:], lngb[:], ALU.mult, [gg4[s], lngb], [gg4[s]])
                for s in R4:
                    TT("pool", vn4[s][:], gg4[s][:], lnbb[:], ALU.add, [gg4[s], lnbb], [vn4[s]])
                for j in range(4):
                    a = nacc()
                    for c in range(8):
                        MM(a[:], Win0[:, c, j * 128:(j + 1) * 128], hnT[:, c, :], c == 0, c == 7, [Win0, hnT], [a])
                    ACT(QT[:, j, :], a[:], AF.Copy, [a], [QT], scale=0.125)
                for uc in range(4):
                    a = nacc()
                    for c in range(8):
                        MM(a[:], Win0[:, c, 768 + uc * 128:768 + (uc + 1) * 128], hnT[:, c, :], c == 0, c == 7,
                           [Win0, hnT], [a])
                    ACT(uT[:, uc, :], a[:], AF.Gelu, [a], [uT])

            def a1_stageB1b(n, kind, idx):
                if kind == "h2":
                    return
                xT, hnT = xT2[n % 2], hnT2[n % 2]

                def kv_prev(s):
                    if kind == "halo":
                        return KTh2[:, idx * 4 + s, :], Vh2[:, idx * 4 + s, :], [KTh2, Vh2]
                    if s > 0:
                        return KTc[:, (s - 1) * 128:s * 128], Vc[:, s - 1, :], [KTc, Vc]
                    return KTh[:, idx, :], Vh[:, idx, :], [KTh, Vh]

                def swa1(s):
                    ktp, vp, rp = kv_prev(s)
                    ktc = KTc[:, s * 128:(s + 1) * 128]
                    P_ = PT2[s]
                    for g in range(2):
                        r0 = g * 64
                        for pc in range(2):
                            un = (s * 4 + g * 2 + pc) % 4
                            sbk = pb[2 + un]
                            kt = ktp if pc == 0 else ktc
                            for j in range(4):
                                MM(sbk[:, j * 128:(j + 1) * 128], kt[r0:r0 + 64, :], QT[r0:r0 + 64, j, s * 128:(s + 1) * 128],
                                   True, True, [QT, KTc] + rp, [sbk])
                            sc_ = scs4[un]
                            bia = biasT[:, pc, g * 512:(g + 1) * 512]
                            if kind == "main" and s == 0 and pc == 0:
                                STT("dve", sc_[:], sbk[:], flsw[:, idx:idx + 1], bia, ALU.add, ALU.add,
                                    [sbk, flsw, biasT], [sc_])
                            else:
                                TT("dve", sc_[:], sbk[:], bia, ALU.add, [sbk, biasT], [sc_])
                            ACT(P_[g][pc][:], sc_[:], AF.Exp, [sc_], [P_[g][pc]])

                def swa2(s):
                    ktp, vp, rp = kv_prev(s)
                    vc = Vc[:, s, :]
                    P_ = PT2[s]
                    for g in range(2):
                        px, pd = pb[4 + g], pb[6 + g]
                        for j in range(4):
                            js = slice(j * 128, (j + 1) * 128)
                            MM(px[:, js], vp, P_[g][0][:, js], True, False, [P_[g][0], Vc] + rp, [px])
                            MM(px[:, js], vc, P_[g][1][:, js], False, True, [P_[g][1], Vc], [px])
                            MM(pd[:, js], ones_b[:], P_[g][0][:, js], True, False, [P_[g][0], ones_b], [pd])
                            MM(pd[:, js], ones_b[:], P_[g][1][:, js], False, True, [P_[g][1], ones_b], [pd])
                    for g in range(2):
                        r0 = g * 64
                        TT("dve", dnb[g][r0:r0 + 64, :], pb[6 + g][r0:r0 + 64, :],
                           eskf[r0:r0 + 64, :, :].rearrange("p a b -> p (a b)"), ALU.add, [pb[6 + g], eskf], [dnb[g]])
                    for g in range(2):
                        r0 = g * 64
                        ACT(dnb[g][r0:r0 + 64, :], dnb[g][r0:r0 + 64, :], AF.Ln, [dnb[g]], [dnb[g]])
                    for g in range(2):
                        r0 = g * 64
                        ACT(dnb[g][r0:r0 + 64, :], dnb[g][r0:r0 + 64, :], AF.Exp, [dnb[g]], [dnb[g]], scale=-1.0)
                    for g in range(2):
                        r0 = g * 64
                        TT("dve", mixT[r0:r0 + 64, 0:4, s * 128:(s + 1) * 128],
                           pb[4 + g][r0:r0 + 64, :].rearrange("p (a b) -> p a b", a=4),
                           dnb[g][r0:r0 + 64, :].rearrange("p (a b) -> p a b", a=4), ALU.mult, [pb[4 + g], dnb[g]], [mixT])

                for s in range(4):
                    swa1(s)
                    v_ = vn4[s]
                    for pr in range(4):
                        for hf in range(2):
                            MM(pb[hf][:, pr * 128:(pr + 1) * 128], v_[:, pr * 128:(pr + 1) * 128],
                               WsT[:, 2 * pr + hf, :], True, True, [v_, WsT], [pb[hf]])
                    for hf in range(2):
                        r0 = hf * 64
                        TT("dve", sstage[r0:r0 + 64, :, s * 128:(s + 1) * 128],
                           pb[hf][r0:r0 + 64, :].rearrange("p (a b) -> p a b", a=4), bsb[r0:r0 + 64, :, :],
                           ALU.add, [pb[hf], bsb], [sstage])
                for s in range(4):
                    swa2(s)
                TT("pool", mixT[:, 4:8, :], uT[:], sstage[:], ALU.mult, [uT, sstage], [mixT])

            def a1_stageB2(n, kind, idx):
                xT = xT2[n % 2]
                if kind == "h2":
                    return
                for dc in range(8):
                    a = nacc()
                    for mc in range(8):
                        MM(a[:], Wo0[:, mc, dc * 128:(dc + 1) * 128], mixT[:, mc, :], mc == 0, mc == 7, [Wo0, mixT], [a])
                    STT("dve", xT[:, dc, :], a[:], GATE(0, 1)[:, dc:dc + 1], xT[:, dc, :], ALU.mult, ALU.add,
                        [a, modT[0], xT], [xT])
                if kind == "main":
                    store_hT(xT, idx)
                else:
                    for s in range(4):
                        c0 = (idx * 4 + s) * 2
                        CP("pool", hcomp[:, :, c0:c0 + 2], xT[:, :, s * 128 + 126:s * 128 + 128], [xT], [hcomp])

            work = [(xh2[gq * 512:(gq + 1) * 512, :], "h2", gq) for gq in range(4)]
            work += [(xh[gq * 512:(gq + 1) * 512, :], "halo", gq) for gq in range(4)]
            work += [(xm[pos * 512:(pos + 1) * 512, :], "main", pos) for pos in range(NPOS)]

            def x_fetch(n):
                DMA("sp", xts[n % 2][:], work[n][0].rearrange("(s p) d -> p s d", p=128), [], [xts[n % 2]])

            x_fetch(0)
            a1_stageA(xt, 0)
            x_fetch(1)
            for n in range(len(work)):
                a1_stageB1(n, work[n][1], work[n][2])
                if n + 1 < len(work):
                    a1_stageA(xt, n + 1)
                if n + 2 < len(work):
                    x_fetch(n + 2)
                a1_stageB1b(n, work[n][1], work[n][2])
                a1_stageB2(n, work[n][1], work[n][2])
                emit_conversions(2)
        p.barrier()

        with contextlib.ExitStack() as st:
            Wg = sbt(st, "Wg", [128, 8, FF0], BF16)
            Wu = sbt(st, "Wu", [128, 8, FF0], BF16)
            Wdp = [sbt(st, "Wdp%d" % i, [128, 22, 128], BF16) for i in range(3)]
            hb = [sbt(st, "hb%d" % i, [128, 8, TB], F32) for i in range(2)]
            hn = sbt(st, "hn2", [128, 8, TB], BF16)
            actT = sbt(st, "actT", [128, 22, TB], BF16)
            sq = [sbt(st, "sq%d" % i, [128, TB], F32) for i in range(2)]
            tmpn = [sbt(st, "tmpn%d" % i, [128, TB], F32) for i in range(2)]
            rr = sbt(st, "rr", [128, TB], F32)
            sg = [sbt(st, "sg%d" % i, [128, TB], F32) for i in range(2)]
            for c in range(8):
                DMA("pool", Wg[:, c, :], wg0[c * 128:(c + 1) * 128, :], [], [Wg])
                DMA("pool", Wu[:, c, :], wu0[c * 128:(c + 1) * 128, :], [], [Wu])
            hnc = sbt(st, "hnc", [128, 8, 32], BF16)
            actc = sbt(st, "actc", [128, 22, 32], BF16)
            wdn = [0]

            def a2_norm(h, hn_t, N):
                norm_mod(h, 8, N, float(D), gsc[:, 8:16], SH(0, 2), hn_t, (sq, tmpn, rr))

            def a2_body(h, hn_t, act_t, N, mid=None):
                for fc in range(22):
                    ag, au = pb[2 + 2 * (fc % 2)], pb[3 + 2 * (fc % 2)]
                    for c in range(8):
                        MM(ag[:, :N], Wg[:, c, fc * 128:(fc + 1) * 128], hn_t[:, c, :], c == 0, c == 7, [Wg, hn_t], [ag])
                    for c in range(8):
                        MM(au[:, :N], Wu[:, c, fc * 128:(fc + 1) * 128], hn_t[:, c, :], c == 0, c == 7, [Wu, hn_t], [au])
                    s_ = sg[fc % 2]
                    ACT(s_[:, :N], ag[:, :N], AF.Silu, [ag], [s_])
                    TT("dve", act_t[:, fc, :], s_[:, :N], au[:, :N], ALU.mult, [s_, au], [act_t])
                if mid is not None:
                    mid()
                for dc in range(8):
                    w = Wdp[wdn[0] % 3]
                    wdn[0] += 1
                    DMA("pool", w[:], wd0[:, dc * 128:(dc + 1) * 128].rearrange("(f p) m -> p f m", p=128), [], [w])
                    a = nacc()
                    for fc in range(22):
                        MM(a[:, :N], w[:, fc, :], act_t[:, fc, :], fc == 0, fc == 21, [w, act_t], [a])
                    STT("dve", h[:, dc, :], a[:, :N], GATE(0, 2)[:, dc:dc + 1], h[:, dc, :], ALU.mult, ALU.add,
                        [a, modT[0], h], [h])

            hnB = sbt(st, "hn2b", [128, 8, TB], BF16)
            hns = [hn, hnB]
            load_hT(hb[0], 0)
            a2_norm(hcomp, hnc, 32)
            a2_body(hcomp, hnc, actc, 32, mid=lambda: a2_norm(hb[0], hns[0], TB))
            for blk in range(NPOS):
                h = hb[blk % 2]
                if blk + 1 < NPOS:
                    load_hT(hb[(blk + 1) % 2], blk + 1)
                    nxt = (lambda b=blk: a2_norm(hb[(b + 1) % 2], hns[(b + 1) % 2], TB))
                else:
                    nxt = None
                a2_body(h, hns[blk % 2], actT, TB, mid=nxt)
                store_hT(h, blk)
                if dbg:
                    DMA("sp", dbg_out["dbg_h"][:, blk * TB:(blk + 1) * TB].rearrange("(c p) n -> p c n", p=128), h[:],
                        [h], [])
        p.barrier()

        with contextlib.ExitStack() as st:
            Win1 = sbt(st, "Win1", [128, 8, 2336], BF16)
            Wkpe = sbt(st, "Wkpe", [128, 8, 96], BF16)
            Wkper = sbt(st, "Wkper", [128, 8, 96], BF16)
            Wqb = sbt(st, "Wqb", [128, 4, 768], BF16)
            Wqbr = sbt(st, "Wqbr", [128, 4, 768], BF16)
            Wkn = sbt(st, "Wkn", [128, 2, 512], BF16)
            Wv = sbt(st, "Wv", [128, 2, 512], BF16)
            hb = [sbt(st, "hb%d" % i, [128, 8, TB], F32) for i in range(2)]
            hn2 = [sbt(st, "hn1", [128, 8, TB], BF16), sbt(st, "hn1b", [128, 8, TB], BF16)]
            hncur = [hn2[0]]
            sq = [sbt(st, "sq%d" % i, [128, TB], F32) for i in range(2)]
            tmpn = [sbt(st, "tmpn%d" % i, [128, TB], F32) for i in range(2)]
            rr = sbt(st, "rr", [128, TB], F32)
            scr = (sq, tmpn, rr)
            gbT = sbt(st, "gbT", [128, 4, TB], F32)
            gcT = sbt(st, "gcT", [128, 4, TB], F32)
            z = sbt(st, "z", [128, 4, TB + 2], F32)
            zh = sbt(st, "zh", [128, 4, NPOS, 2], F32)
            yc = [sbt(st, "yc%d" % i, [128, TB], F32) for i in range(2)]
            cTt = sbt(st, "cTt", [128, 4, TB], BF16)
            qlT = sbt(st, "qlT", [128, 4, TB], F32)
            qnT = sbt(st, "qnT", [128, 4, TB], BF16)
            kvlT = sbt(st, "kvlT", [128, 2, TB], F32)
            kvnT = sbt(st, "kvnT", [128, 2, TB], BF16)
            QTh = [sbt(st, "QTh%d" % i, [128, TB], BF16) for i in range(2)]
            rt = [sbt(st, "rt%d" % i, [128, TB], F32) for i in range(2)]
            knt = [sbt(st, "knt%d" % i, [128, TB], BF16) for i in range(2)]
            vt = sbt(st, "vt", [128, 4, 512], BF16)
            kpe = sbt(st, "kpe", [128, TB], BF16)
            rk = sbt(st, "rk", [128, 2, TB], F32)
            rq = sbt(st, "rq", [128, 2, TB], F32)

            DMA("pool", Win1[:], w_in1.rearrange("(c p) f -> p c f", p=128), [], [Win1])
            MEMSET("pool", Wkpe[:], 0.0, [Wkpe])
            MEMSET("pool", Wkper[:], 0.0, [Wkper])
            MEMSET("pool", Wqbr[:], 0.0, [Wqbr])
            CP("pool", Wkpe[:, :, 64:96], Win1[:, :, 2304:2336], [Win1], [Wkpe])
            TS("pool", Wkper[:, :, 64:80], Win1[:, :, 2320:2336], -1.0, ALU.mult, [Win1], [Wkper])
            CP("pool", Wkper[:, :, 80:96], Win1[:, :, 2304:2320], [Win1], [Wkper])
            DMA("pool", Wqb[:], w_qb.rearrange("(c p) f -> p c f", p=128), [], [Wqb])
            Wqb_v = Wqb[:].rearrange("p c (h e) -> p c h e", e=96)
            Wqbr_v = Wqbr[:].rearrange("p c (h e) -> p c h e", e=96)
            for c in range(4):
                TS("pool", Wqbr_v[:, c, :, 64:80], Wqb_v[:, c, :, 80:96], -1.0, ALU.mult, [Wqb], [Wqbr])
                CP("pool", Wqbr_v[:, c, :, 80:96], Wqb_v[:, c, :, 64:80], [Wqb], [Wqbr])
            kv_v = w_kvb.rearrange("(c p) (h t d) -> p c h t d", p=128, t=2, d=64)
            for c in range(2):
                DMA("pool", Wkn[:, c, :].rearrange("p (h d) -> p h d", d=64), kv_v[:, c, :, 0, :], [], [Wkn])
                DMA("pool", Wv[:, c, :].rearrange("p (h d) -> p h d", d=64), kv_v[:, c, :, 1, :], [], [Wv])
            nqh = [0]

            def proj_fm(col0, nchunk, dst_fn):
                for k in range(nchunk):
                    a = nacc()
                    for c in range(8):
                        MM(a[:], Win1[:, c, col0 + k * 128:col0 + (k + 1) * 128], hncur[0][:, c, :], c == 0, c == 7,
                           [Win1, hncur[0]], [a])
                    dst_fn(k, a)

            hnc1 = sbt(st, "hnc1", [128, 8, 32], BF16)
            gcc = sbt(st, "gcc", [128, 4, 32], F32)
            zh_v = zh[:].rearrange("p c n k -> p c (n k)")
            norm_mod(hcomp, 8, 32, float(D), gsc[:, 16:24], SH(1, 1), hnc1, scr)
            for k in range(4):
                a = nacc()
                for c in range(8):
                    MM(a[:, :32], Win1[:, c, 512 + k * 128:512 + (k + 1) * 128], hnc1[:, c, :], c == 0, c == 7, [Win1, hnc1], [a])
                EVAC(gcc[:, k, :], a[:, :32], [a], [gcc])
            for k in range(4):
                a = nacc()
                for c in range(8):
                    MM(a[:, :32], Win1[:, c, 1024 + k * 128:1024 + (k + 1) * 128], hnc1[:, c, :], c == 0, c == 7, [Win1, hnc1], [a])
                TT("dve", zh_v[:, k, :], gcc[:, k, :], a[:, :32], ALU.mult, [gcc, a], [zh])
            load_hT(hb[0], 0)
            norm_mod(hb[0], 8, TB, float(D), gsc[:, 16:24], SH(1, 1), hn2[0], scr)
            for pos in range(NPOS):
                blk = pos
                h = hb[pos % 2]
                hncur[0] = hn2[pos % 2]
                if pos + 1 < NPOS:
                    load_hT(hb[(pos + 1) % 2], pos + 1)
                proj_fm(512, 4, lambda k, a: EVAC(gcT[:, k, :], a[:], [a], [gcT]))
                proj_fm(1024, 4, lambda k, a: TT("dve", z[:, k, 2:TB + 2], gcT[:, k, :], a[:], ALU.mult, [gcT, a], [z]))
                TS("pool", z[:, :, 0:2], zh[:, :, pos, :], flcv[:, pos:pos + 1], ALU.mult, [zh, flcv], [z])
                proj_fm(0, 4, lambda k, a: EVAC(gbT[:, k, :], a[:], [a], [gbT]))
                for cc in range(4):
                    y_ = yc[cc % 2]
                    TS("dve", y_[:], z[:, cc, 0:TB], vec[:, V_CW + cc:V_CW + cc + 1], ALU.mult, [z, vec], [y_])
                    STT("dve", y_[:], z[:, cc, 1:TB + 1], vec[:, V_CW + 4 + cc:V_CW + 5 + cc], y_[:], ALU.mult, ALU.add,
                        [z, vec, y_], [y_])
                    STT("dve", y_[:], z[:, cc, 2:TB + 2], vec[:, V_CW + 8 + cc:V_CW + 9 + cc], y_[:], ALU.mult, ALU.add,
                        [z, vec, y_], [y_])
                    TT("pool", cTt[:, cc, :], gbT[:, cc, :], y_[:], ALU.mult, [gbT, y_], [cTt])
                DMA("sp", cT_d[:, pos * TB:(pos + 1) * TB].rearrange("(c p) n -> p c n", p=128), cTt[:], [cTt], [b_cT[pos]])
                if pos + 1 < NPOS:
                    norm_mod(hb[(pos + 1) % 2], 8, TB, float(D), gsc[:, 16:24], SH(1, 1), hn2[(pos + 1) % 2], scr)
                DMA("sp", rk[64:96, :, :], ropek_d[:, :, pos * TB:(pos + 1) * TB].rearrange("a r n -> r a n"), [], [rk])
                if pos < NOWN:
                    DMA("sp", rq[64:96, :, :], ropeq_d[:, :, pos * TB:(pos + 1) * TB].rearrange("a r n -> r a n"), [], [rq])
                    proj_fm(1536, 4, lambda k, a: EVAC(qlT[:, k, :], a[:], [a], [qlT]))
                    norm_mod(qlT, 4, TB, 512.0, vec[:, V_QG:V_QG + 4], None, qnT, scr)
                    for hh in range(8):
                        aa, ab = pb[2 + 2 * (hh % 2)], pb[3 + 2 * (hh % 2)]
                        for c in range(4):
                            MM(aa[0:96, :], Wqb[:, c, hh * 96:(hh + 1) * 96], qnT[:, c, :], c == 0, c == 3, [Wqb, qnT], [aa])
                        for c in range(4):
                            MM(ab[0:96, :], Wqbr[:, c, hh * 96:(hh + 1) * 96], qnT[:, c, :], c == 0, c == 3, [Wqbr, qnT], [ab])
                        qt = QTh[nqh[0] % 2]
                        r1, r2 = rt[0], rt[1]
                        nqh[0] += 1
                        ACT(qt[0:64, :], aa[0:64, :], AF.Copy, [aa], [qt], scale=float(96 ** -0.5))
                        TT("dve", r1[64:96, :], aa[64:96, :], rq[64:96, 0, :], ALU.mult, [aa, rq], [r1])
                        TT("dve", r2[64:96, :], ab[64:96, :], rq[64:96, 1, :], ALU.mult, [ab, rq], [r2])
                        TT("pool", qt[64:96, :], r1[64:96, :], r2[64:96, :], ALU.add, [r1, r2], [qt])
                        DMA("sp", qT_d[hh, :, pos * TB:(pos + 1) * TB], qt[0:96, :], [qt], [b_q])
                proj_fm(2048, 2, lambda k, a: EVAC(kvlT[:, k, :], a[:], [a], [kvlT]))
                norm_mod(kvlT, 2, TB, 256.0, vec[:, V_KG:V_KG + 2], None, kvnT, scr)
                for hc in range(4):
                    a = nacc()
                    for c in range(2):
                        MM(a[:], Wkn[:, c, hc * 128:(hc + 1) * 128], kvnT[:, c, :], c == 0, c == 1, [Wkn, kvnT], [a])
                    kt_ = knt[hc % 2]
                    EVAC(kt_[:], a[:], [a], [kt_])
                    DMA("sp", knT_d[hc * 128:(hc + 1) * 128, pos * TB:(pos + 1) * TB], kt_[:], [kt_], [b_kn])
                for s in range(4):
                    a = nacc()
                    for c in range(2):
                        MM(a[:], kvnT[:, c, s * 128:(s + 1) * 128], Wv[:, c, :], c == 0, c == 1, [Wv, kvnT], [a])
                    EVAC(vt[:, s, :], a[:], [a], [vt])
                DMA("sp", v_d[pos * TB:(pos + 1) * TB, :].rearrange("(s p) f -> p s f", p=128), vt[:], [vt], [b_v])
                aa, ab = pb[2], pb[3]
                for c in range(8):
                    MM(aa[0:96, :], Wkpe[:, c, :], hncur[0][:, c, :], c == 0, c == 7, [Wkpe, hncur[0]], [aa])
                for c in range(8):
                    MM(ab[0:96, :], Wkper[:, c, :], hncur[0][:, c, :], c == 0, c == 7, [Wkper, hncur[0]], [ab])
                r1, r2 = rt[0], rt[1]
                TT("dve", r1[64:96, :], aa[64:96, :], rk[64:96, 0, :], ALU.mult, [aa, rk], [r1])
                TT("dve", r2[64:96, :], ab[64:96, :], rk[64:96, 1, :], ALU.mult, [ab, rk], [r2])
                TT("pool", kpe[64:96, :], r1[64:96, :], r2[64:96, :], ALU.add, [r1, r2], [kpe])
                DMA("sp", kpeT_d[:, pos * TB:(pos + 1) * TB], kpe[64:96, :], [kpe], [b_kpe])
        p.barrier()

        with contextlib.ExitStack() as st:
            KT = [sbt(st, "KT%d" % i, [128, S], BF16) for i in range(2)]
            VA = [sbt(st, "VA%d" % i, [128, 64, 128], BF16) for i in range(2)]
            QA = [sbt(st, "QA%d" % i, [128, NOWN * TB], BF16) for i in range(2)]
            PTm = [sbt(st, "PTm%d" % i, [128, TB], BF16) for i in range(4)]
            msk = sbt(st, "msk", [128, 4, TB], BF16)
            ODs = [sbt(st, "ODs%d" % i, [128, TB], F32) for i in range(2)]
            rden = [sbt(st, "rden%d" % i, [128, TB], F32) for i in range(2)]
            dout = [sbt(st, "dout%d" % i, [128, TB], BF16) for i in range(2)]
            SEL = [sbt(st, "SEL%d" % i, [128, 128], F32) for i in range(2)]
            DMA("pool", msk[:], mlam_d.rearrange("p (a b) -> p a b", a=4), [], [msk])
            flob = sbt(st, "flob", [128, NOWN], F32)
            TS("dve", flob[:], flot[:], -NEG, ALU.mult, [flot], [flob], s2=NEG, op1=ALU.add)
            for i_, off in ((0, -64), (1, 64)):
                MEMSET("pool", SEL[i_][:], 0.0, [SEL[i_]])
                p.op("pool", lambda e, t=SEL[i_], off=off: e.affine_select(
                    out=t[:], in_=t[:], pattern=[[-1, 128]], compare_op=ALU.not_equal, fill=1.0, base=off,
                    channel_multiplier=1), (), [SEL[i_].b])
            MEMSET("pool", VA[0][:, :, 64:128], 1.0, [VA[0]])
            MEMSET("pool", VA[1][:, :, 0:64], 1.0, [VA[1]])
            def b2_load(hh):
                par = hh % 2
                kt, va, qa = KT[par], VA[par], QA[par]
                voff = 0 if par == 0 else 64
                DMA("sp", kt[0:64, :], knT_d[hh * 64:(hh + 1) * 64, :], [b_kn], [kt])
                DMA("sp", kt[64:96, :], kpeT_d[:, :], [b_kpe], [kt])
                for q4 in range(4):
                    DMA("sp", va[:, q4 * 16:(q4 + 1) * 16, voff:voff + 64],
                        v_d[q4 * 2048:(q4 + 1) * 2048, hh * 64:(hh + 1) * 64].rearrange("(kb p) d -> p kb d", p=128),
                        [b_v], [va])
                DMA("sp", qa[0:96, :], qT_d[hh, :, :], [b_q], [qa])

            items = []
            for hh in range(8):
                for i in range(NOWN):
                    klist = []
                    for i2 in range(i):
                        for sub in range(4):
                            klist.append((i2, sub, None))
                            klist.append((8 + i2, sub, None))
                    for sub in range(4):
                        klist.append((8 + i, sub, "oth"))
                    for sub in range(4):
                        klist.append((i, sub, "diag"))
                    for n, (pos, sub, kind) in enumerate(klist):
                        items.append((hh, i, n, len(klist), pos, sub, kind))
            sb3 = [pb[1], pb[2], pb[3]]

            def emit_S(t):
                hh, i, n, nk, pos, sub, kind = items[t]
                par = hh % 2
                kt, qa = KT[par], QA[par]
                kc = pos * TB + sub * 128
                sbk = sb3[t % 3]
                pt = PTm[t % 4]
                MM(sbk[:], kt[0:96, kc:kc + 128], qa[0:96, i * TB:(i + 1) * TB], True, True, [kt, qa], [sbk])
                if kind == "oth":
                    ACT(pt[:], sbk[:], AF.Exp, [sbk, flob], [pt], bias=flob[:, i:i + 1], scale=1.0)
                else:
                    ACT(pt[:], sbk[:], AF.Exp, [sbk], [pt])
                if kind == "diag":
                    TT("dve", pt[:], pt[:], msk[:, sub, :], ALU.mult, [pt, msk], [pt])

            b2_load(0)
            SKEW = 2
            for t in range(min(SKEW, len(items))):
                emit_S(t)
            nod = 0
            for t in range(len(items)):
                hh, i, n, nk, pos, sub, kind = items[t]
                par = hh % 2
                va = VA[par]
                if i == 0 and n == 0 and hh + 1 < 8:
                    b2_load(hh + 1)
                if t + SKEW < len(items):
                    emit_S(t + SKEW)
                od = pb[4 + nod % 2]
                MM(od[:], va[:, pos * 4 + sub, :], PTm[t % 4][:], n == 0, n == nk - 1, [va, PTm[t % 4]], [od])
                if n == nk - 1:
                    os_ = ODs[nod % 2]
                    rd = rden[nod % 2]
                    do = dout[nod % 2]
                    nod += 1
                    CP("act", os_[:], od[:], [od], [os_])
                    db = pb[6]
                    MM(db[:], SEL[par][:], os_[:], True, True, [SEL[par], os_], [db])
                    r0 = par * 64
                    RCP(rd[r0:r0 + 64, :], db[r0:r0 + 64, :], [db], [rd])
                    TT("dve", do[r0:r0 + 64, :], os_[r0:r0 + 64, :], rd[r0:r0 + 64, :], ALU.mult, [os_, rd], [do])
                    DMA("sp", dT_d[hh * 64:(hh + 1) * 64, i * TB:(i + 1) * TB], do[r0:r0 + 64, :], [do], [b_dT[i]])
        p.barrier()

        Mall = sbt(top, "Mall", [128, 32, 8], F32)
        Gall = sbt(top, "Gall", [128, 32, 8], F32)
        with contextlib.ExitStack() as st:
            Wo1 = sbt(st, "Wo1", [128, 8, D], BF16)
            wr = sbt(st, "wr", [128, 8, 8], F32)
            brb = sbt(st, "brb", [128, 8], F32)
            mix = [sbt(st, "mix%d" % i, [128, 8, TB], BF16) for i in range(2)]
            hb = [sbt(st, "hb%d" % i, [128, 8, TB], F32) for i in range(2)]
            hnf = sbt(st, "hnf", [128, 8, TB], F32)
            sq = [sbt(st, "sq%d" % i, [128, TB], F32) for i in range(2)]
            tmpn = [sbt(st, "tmpn%d" % i, [128, TB], F32) for i in range(2)]
            rr = sbt(st, "rr", [128, TB], F32)
            lg = [sbt(st, "lg%d" % i, [128, 8], F32) for i in range(2)]
            e8 = [sbt(st, "e8_%d" % i, [128, 8], F32) for i in range(2)]
            s4 = [sbt(st, "s4_%d" % i, [128, 4], F32) for i in range(2)]
            tk = [sbt(st, "tk%d" % i, [128, D], F32) for i in range(4)]
            ntk = [0]
            DMA("pool", Wo1[:], w_o1.rearrange("(c p) m -> p c m", p=128), [], [Wo1])
            DMA("sp", wr[:], w_rt.rearrange("(c p) e -> p c e", p=128), [], [wr])
            DMA("sp", brb[:], brt_d.partition_broadcast(128), [], [brb])
            DMA("sp", g2row_d.rearrange("(c p) -> p c", p=128), modT[1][:, 40:48], [modT[1]], [b_g2row],
                allow_slow_non_contiguous=True)

            def to_rows(src, dst_d, bufs, i):
                for s in range(4):
                    t_ = tk[ntk[0] % 4]
                    ntk[0] += 1
                    for half in range(2):
                        a = nacc()
                        for c4 in range(4):
                            TR(a[:, c4 * 128:(c4 + 1) * 128], src[:, half * 4 + c4, s * 128:(s + 1) * 128], ident[:],
                               [src, ident], [a])
                        EVAC(t_[:, half * 512:(half + 1) * 512], a[:], [a], [t_])
                    ch = i * 4 + s
                    DMA("sp", dst_d[ch * 128:(ch + 1) * 128, :], t_[:], [t_], [bufs[ch]])

            for i in range(NOWN):
                m_ = mix[i % 2]
                h = hb[i % 2]
                DMA("sp", m_[:, 0:4, :], cT_d[:, i * TB:(i + 1) * TB].rearrange("(c p) n -> p c n", p=128), [b_cT[i]], [m_])
                DMA("sp", m_[:, 4:8, :], dT_d[:, i * TB:(i + 1) * TB].rearrange("(c p) n -> p c n", p=128), [b_dT[i]], [m_])
                load_hT(h, i)
                for dc in range(8):
                    a = nacc()
                    for mc in range(8):
                        MM(a[:], Wo1[:, mc, dc * 128:(dc + 1) * 128], m_[:, mc, :], mc == 0, mc == 7, [Wo1, m_], [a])
                    STT("dve", h[:, dc, :], a[:], GATE(1, 1)[:, dc:dc + 1], h[:, dc, :], ALU.mult, ALU.add,
                        [a, modT[1], h], [h])
                to_rows(h, h3tok_d, b_h3t, i)
                norm_mod(h, 8, TB, float(D), gsc[:, 24:32], SH(1, 2), hnf, (sq, tmpn, rr))
                to_rows(hnf, hn3tok_d, b_hn3t, i)
                for s in range(4):
                    ch = i * 4 + s
                    a = nacc()
                    for c in range(8):
                        MM(a[:, 0:8], hnf[:, c, s * 128:(s + 1) * 128], wr[:, c, :], c == 0, c == 7, [hnf, wr], [a])
                    L, E8, S4 = lg[s % 2], e8[s % 2], s4[s % 2]
                    W8 = Mall[:, ch, :]
                    TT("dve", L[:], a[:, 0:8], brb[:], ALU.add, [a, brb], [L])
                    RED(S4[:, 0:1], L[:], ALU.max, [L], [S4])
                    TS("dve", W8, L[:], S4[:, 0:1], ALU.is_equal, [L, S4], [Mall])
                    STT("dve", W8, W8, -1e30, L[:], ALU.mult, ALU.add, [Mall, L], [Mall])
                    RED(S4[:, 1:2], W8, ALU.max, [Mall], [S4])
                    TS("dve", W8, L[:], S4[:, 1:2], ALU.is_ge, [L, S4], [Mall])
                    TS("dve", S4[:, 2:3], S4[:, 0:1], -1.0, ALU.mult, [S4], [S4])
                    ACT(E8[:], L[:], AF.Exp, [L, S4], [E8], bias=S4[:, 2:3], scale=1.0)
                    TT("dve", E8[:], E8[:], W8, ALU.mult, [E8, Mall], [E8])
                    RED(S4[:, 3:4], E8[:], ALU.add, [E8], [S4])
                    RCP(S4[:, 3:4], S4[:, 3:4], [S4], [S4])
                    TS("dve", Gall[:, ch, :], E8[:], S4[:, 3:4], ALU.mult, [E8, S4], [Gall])
        p.barrier()

        I32 = mybir.dt.int32
        idx_hi = sbt(top, "idx_hi", [128, 32], I32)
        idx_lo = sbt(top, "idx_lo", [128, 32], I32)
        g_hi = sbt(top, "g_hi", [128, 32], F32)
        g_lo = sbt(top, "g_lo", [128, 32], F32)
        widx = sbt(top, "widx", [128, NBLK, 7], I32)
        with contextlib.ExitStack() as st:
            cst = sbt(st, "cst", [128, 32], F32)
            Uex = sbt(st, "Uex", [128, 128], F32)
            tot = sbt(st, "tot", [128, 32, 8], F32)
            offs = sbt(st, "offs", [128, 32, 8], F32)
            slot = sbt(st, "slot", [128, 32, 8], F32)
            vidx = sbt(st, "vidx", [128, 32, 8], F32)
            t256 = sbt(st, "t256", [128, 32, 8], F32)
            eqh = sbt(st, "eqh", [128, 32, 8], F32)
            ne = sbt(st, "ne", [128, 8], F32)
            nb8 = sbt(st, "nb8", [128, 8], F32)
            c8 = sbt(st, "c8", [128, 8], F32)
            pend = sbt(st, "pend", [128, 8], F32)
            pstart = sbt(st, "pstart", [128, 8], F32)
            hi_f = sbt(st, "hi_f", [128, 32], F32)
            lo_f = sbt(st, "lo_f", [128, 32], F32)
            gs = sbt(st, "gs", [128, 32], F32)
            be = sbt(st, "be", [128, NBLK], F32)
            t24 = sbt(st, "t24", [128, NBLK], F32)
            wf = sbt(st, "wf", [128, NBLK, 7], F32)
            DMA("sp", cst[:], cst_d, [], [cst])
            MEMSET("pool", Uex[:], 1.0, [Uex])
            p.op("pool", lambda e: e.affine_select(out=Uex[:], in_=Uex[:], pattern=[[1, 128]],
                                                   compare_op=ALU.is_gt, fill=0.0, base=0, channel_multiplier=-1),
                 (), [Uex.b])
            Mf = Mall[:].rearrange("p a b -> p (a b)")
            MM(pb[2][:, 0:256], Uex[:], Mf, True, True, [Uex, Mall], [pb[2]])
            MM(pb[3][:, 0:256], ones_f[:], Mf, True, True, [ones_f, Mall], [pb[3]])
            CP("dve", tot[:].rearrange("p a b -> p (a b)"), pb[3][:, 0:256], [pb[3]], [tot])
            MEMSET("pool", offs[:, 0, :], 0.0, [offs])
            for ch in range(1, 32):
                TT("dve", offs[:, ch, :], offs[:, ch - 1, :], tot[:, ch - 1, :], ALU.add, [offs, tot], [offs])
            TT("dve", ne[:], offs[:, 31, :], tot[:, 31, :], ALU.add, [offs, tot], [ne])
            TS("dve", nb8[:], ne[:], 0.0, ALU.is_gt, [ne], [nb8])
            for k in range(1, 8):
                TS("dve", c8[:], ne[:], 512.0 * k, ALU.is_gt, [ne], [c8])
                TT("dve", nb8[:], nb8[:], c8[:], ALU.add, [nb8, c8], [nb8])
            CP("dve", pend[:, 0:1], nb8[:, 0:1], [nb8], [pend])
            for e in range(1, 8):
                TT("dve", pend[:, e:e + 1], pend[:, e - 1:e], nb8[:, e:e + 1], ALU.add, [pend, nb8], [pend])
            TT("dve", pstart[:], pend[:], nb8[:], ALU.subtract, [pend, nb8], [pstart])
            TS("dve", pstart[:], pstart[:], 512.0, ALU.mult, [pstart], [pstart])
            TT("dve", slot[:].rearrange("p a b -> p (a b)"), pb[2][:, 0:256], offs[:].rearrange("p a b -> p (a b)"),
               ALU.add, [pb[2], offs], [slot])
            for e in range(8):
                TS("dve", slot[:, :, e], slot[:, :, e], pstart[:, e:e + 1], ALU.add, [slot, pstart], [slot])
            TT("dve", vidx[:], slot[:], Mall[:], ALU.mult, [slot, Mall], [vidx])
            RED(hi_f[:], vidx[:], ALU.max, [vidx], [hi_f])
            BIG = 1.0e6
            TS("dve", t256[:], Mall[:], -BIG, ALU.mult, [Mall], [t256], s2=BIG, op1=ALU.add)
            TT("dve", t256[:], t256[:], slot[:], ALU.add, [t256, slot], [t256])
            RED(lo_f[:], t256[:], ALU.min, [t256], [lo_f])
            TS("dve", lo_f[:], lo_f[:], float(LSLOT - 1), ALU.min, [lo_f], [lo_f])
            for e in range(8):
                TT("dve", eqh[:, :, e], vidx[:, :, e], hi_f[:], ALU.is_equal, [vidx, hi_f], [eqh])
            TT("dve", eqh[:], eqh[:], Gall[:], ALU.mult, [eqh, Gall], [eqh])
            RED(g_hi[:], eqh[:], ALU.add, [eqh], [g_hi])
            RED(gs[:], Gall[:], ALU.add, [Gall], [gs])
            TT("dve", g_lo[:], gs[:], g_hi[:], ALU.subtract, [gs, g_hi], [g_lo])
            CP("dve", idx_hi[:], hi_f[:], [hi_f], [idx_hi])
            CP("dve", idx_lo[:], lo_f[:], [lo_f], [idx_lo])
            MEMSET("pool", be[:], 0.0, [be])
            for e in range(8):
                TS("dve", t24[:], cst[:, 0:NBLK], pend[:, e:e + 1], ALU.is_ge, [cst, pend], [t24])
                TT("dve", be[:], be[:], t24[:], ALU.add, [be, t24], [be])
            TS("dve", be[:], be[:], 7.0, ALU.min, [be], [be])
            TS("dve", be[:], be[:], 896.0, ALU.mult, [be], [be], s2=cst[:, 24:25], op1=ALU.add)
            for fg in range(7):
                TS("dve", wf[:, :, fg], be[:], 128.0 * fg, ALU.add, [be], [wf])
            CP("dve", widx[:], wf[:], [wf], [widx])
            xr = [sbt(st, "xr%d" % i, [128, D], F32) for i in range(3)]
            for ch in range(32):
                x_ = xr[ch % 3]
                DMA("sp", x_[:], hn3tok_d[ch * 128:(ch + 1) * 128, :], [b_hn3t[ch]], [x_])
                for it in (idx_hi, idx_lo):
                    p.idma(lambda e, x_=x_, it=it, ch=ch: e.indirect_dma_start(
                        out=xs_h[:, :], out_offset=bass.IndirectOffsetOnAxis(ap=it[:, ch:ch + 1], axis=0),
                        in_=x_[:, :], in_offset=None),
                        [x_.b, it.b], [b_xs])
        p.barrier()

        with contextlib.ExitStack() as st:
            WG = [sbt(st, "WG%d" % i, [128, 8, 512], BF16) for i in range(2)]
            WU = [sbt(st, "WU%d" % i, [128, 8, 512], BF16) for i in range(2)]
            WD = [sbt(st, "WD%d" % i, [128, 4, D], BF16) for i in range(2)]
            XT = [sbt(st, "XT%d" % i, [128, 8, TB], BF16) for i in range(2)]
            Yb = [sbt(st, "Yb%d" % i, [128, 4, D], F32) for i in range(2)]
            xrow = [sbt(st, "xrow%d" % i, [128, D], F32) for i in range(8)]
            actm = [sbt(st, "actm%d" % i, [128, 4, TB], BF16) for i in range(2)]
            sg = [sbt(st, "sgm%d" % i, [128, TB], F32) for i in range(2)]
            nsg = [0]
            nxr = [0]

            def w_load(n):
                b_, fg_ = n // 7, n % 7
                k_ = n % 2
                for (tiles, srcs, v) in ((WG, ew_bf["g"], "g"), (WU, ew_bf["u"], "u"), (WD, ew_bf["d"], "d")):
                    t_ = tiles[k_]
                    for h in range(2):
                        if v == "d":
                            o_ = t_[:, 2 * h:2 * h + 2, :].rearrange("p a b -> p (a b)")
                        else:
                            o_ = t_[:, 4 * h:4 * h + 4, :].rearrange("p a b -> p (a b)")
                        p.idma(lambda e, o_=o_, src=srcs[h], b_=b_, fg_=fg_: e.indirect_dma_start(
                            out=o_, out_offset=None, in_=src[:, :],
                            in_offset=bass.IndirectOffsetOnAxis(ap=widx[:, b_, fg_:fg_ + 1], axis=0)),
                            [widx.b] + b_cv, [t_.b])

            def x_issue(b_):
                for s in range(4):
                    xr_ = xrow[(b_ % 2) * 4 + s]
                    DMA("sp", xr_[:], xs_d[b_ * TB + s * 128:b_ * TB + (s + 1) * 128, :], [b_xs], [xr_])

            def x_trans(b_):
                xt_ = XT[b_ % 2]
                for s in range(4):
                    xr_ = xrow[(b_ % 2) * 4 + s]
                    for half in range(2):
                        a = nacc()
                        for c4 in range(4):
                            TR(a[:, c4 * 128:(c4 + 1) * 128], xr_[:, (half * 4 + c4) * 128:(half * 4 + c4 + 1) * 128], ident[:],
                               [xr_, ident], [a])
                        EVAC(xt_[:, half * 4:(half + 1) * 4, s * 128:(s + 1) * 128],
                             a[:].rearrange("p (a b) -> p a b", a=4), [a], [xt_])

            w_load(0)
            x_issue(0)
            x_trans(0)
            for b_ in range(NBLK):
                xt_ = XT[b_ % 2]
                yb = Yb[b_ % 2]
                if b_ + 1 < NBLK:
                    x_issue(b_ + 1)
                for fg in range(7):
                    n_ = b_ * 7 + fg
                    if fg == 3 and b_ + 1 < NBLK:
                        x_trans(b_ + 1)
                    k_ = n_ % 2
                    wgt, wut, wdt = WG[k_], WU[k_], WD[k_]
                    if n_ + 1 < NBLK * 7:
                        w_load(n_ + 1)
                    am = actm[n_ % 2]
                    for f4 in range(4):
                        ag, au = pb[2 + 2 * (f4 % 2)], pb[3 + 2 * (f4 % 2)]
                        for c in range(8):
                            MM(ag[:], wgt[:, c, f4 * 128:(f4 + 1) * 128], xt_[:, c, :], c == 0, c == 7, [wgt, xt_], [ag])
                        for c in range(8):
                            MM(au[:], wut[:, c, f4 * 128:(f4 + 1) * 128], xt_[:, c, :], c == 0, c == 7, [wut, xt_], [au])
                        s_ = sg[nsg[0] % 2]
                        nsg[0] += 1
                        ACT(s_[:], ag[:], AF.Silu, [ag], [s_])
                        TT("dve", am[:, f4, :], s_[:], au[:], ALU.mult, [s_, au], [am])
                    for s4_ in range(4):
                        for half in range(2):
                            a = nacc()
                            for f4 in range(4):
                                MM(a[:], am[:, f4, s4_ * 128:(s4_ + 1) * 128], wdt[:, f4, half * 512:(half + 1) * 512],
                                   f4 == 0, f4 == 3, [wdt, am], [a])
                            dst = yb[:, s4_, half * 512:(half + 1) * 512]
                            if fg == 0:
                                EVAC(dst, a[:], [a], [yb])
                            else:
                                TT("dve", dst, dst, a[:], ALU.add, [yb, a], [yb])
                DMA("sp", ys_d[b_ * TB:(b_ + 1) * TB, :].rearrange("(s p) m -> p s m", p=128), yb[:], [yb], [b_ys[b_]])
        p.barrier()

        fin = []
        with contextlib.ExitStack() as st:
            g2b = sbt(st, "g2b", [128, D], F32)
            fgb = sbt(st, "fgb", [128, D], F32)
            Yh = [sbt(st, "Yh%d" % i, [128, D], F32) for i in range(2)]
            Yl = [sbt(st, "Yl%d" % i, [128, D], F32) for i in range(2)]
            h3r = [sbt(st, "h3r%d" % i, [128, D], F32) for i in range(2)]
            ot = [sbt(st, "ot%d" % i, [128, D], F32) for i in range(2)]
            junk = sbt(st, "junk", [128, D], F32)
            ss = [sbt(st, "ss%d" % i, [128, 2], F32) for i in range(2)]
            DMA("sp", g2b[:], g2row_d.rearrange("(o n) -> o n", o=1).partition_broadcast(128), [b_g2row], [g2b])
            DMA("sp", fgb[:], fgrow_d.partition_broadcast(128), [], [fgb])
            for ch in range(32):
                yh, yl, hr, o, s2 = Yh[ch % 2], Yl[ch % 2], h3r[ch % 2], ot[ch % 2], ss[ch % 2]
                for (yt, it) in ((yh, idx_hi), (yl, idx_lo)):
                    p.idma(lambda e, yt=yt, it=it, ch=ch: e.indirect_dma_start(
                        out=yt[:, :], out_offset=None, in_=ys_h[:, :],
                        in_offset=bass.IndirectOffsetOnAxis(ap=it[:, ch:ch + 1], axis=0)),
                        [it.b] + b_ys, [yt.b])
                DMA("sp", hr[:], h3tok_d[ch * 128:(ch + 1) * 128, :], [b_h3t[ch]], [hr])
                ACT(yl[:], yl[:], AF.Copy, [yl, g_lo], [yl], scale=g_lo[:, ch:ch + 1])
                STT("dve", yh[:], yh[:], g_hi[:, ch:ch + 1], yl[:], ALU.mult, ALU.add, [yh, g_hi, yl], [yh])
                TT("dve", yh[:], yh[:], g2b[:], ALU.mult, [yh, g2b], [yh])
                TT("dve", hr[:], hr[:], yh[:], ALU.add, [hr, yh], [hr])
                MEMSET("pool", s2[:], 0.0, [s2])
                ACT(junk[:], hr[:], AF.Square, [hr], [junk, s2], accum_out=s2[:, 0:1])
                ACT(s2[:, 1:2], s2[:, 0:1], AF.Sqrt, [s2], [s2], bias=EPS_AP[:, 0:1], scale=1.0 / D)
                RCP(s2[:, 1:2], s2[:, 1:2], [s2], [s2])
                STT("dve", o[:], hr[:], s2[:, 1:2], fgb[:], ALU.mult, ALU.mult, [hr, s2, fgb], [o])
                fin.append(DMA("sp", y_out[ch * 128:(ch + 1) * 128, :], o[:], [o], []))
        for t in fin:
            p.wait_tok("sp", t)
        for t in list(p.last_dma.values()):
            p.wait_tok("sp", t)
        p.emit()
    return nc


def _pc(v, k):
    return np.ascontiguousarray(np.asarray(v, np.float32).reshape(k, 128).T)


def _host_inputs(inp, dbg_cores=None):
    f = lambda k: np.asarray(inp[k], np.float32)
    x = f("x")
    B = x.shape[0]
    slopes = (2.0 ** (-8.0 * np.arange(1, 9, dtype=np.float32) / 8)).astype(np.float32)
    k_i = np.arange(128)[:, None]
    q_i = np.arange(128)[None, :]
    swab = np.zeros((128, 2, 2, 4, 128), np.float32)
    for g in range(2):
        for j in range(4):
            sl = slopes[g * 4 + j]
            dist_prev = q_i + 128 - k_i
            dist_cur = q_i - k_i
            bp = np.where((dist_prev >= 0) & (dist_prev < 128), -sl * dist_prev, NEG)
            bc = np.where((dist_cur >= 0) & (dist_cur < 128), -sl * dist_cur, NEG)
            swab[:, 0, g, j, :] = bp
            swab[:, 1, g, j, :] = bc
    swab = swab.reshape(128, 2 * 8 * 128)
    mlam = np.zeros((128, 4, TB), np.float32)
    for d in range(4):
        mlam[:, d, :] = ((d * 128 + np.arange(128))[:, None] <= np.arange(TB)[None, :]).astype(np.float32)
    mlam = mlam.reshape(128, 4 * TB)
    inv = (10000.0 ** (-np.arange(0, 32, 2, dtype=np.float32) / 32)).astype(np.float32)
    ang = np.arange(S, dtype=np.float32)[:, None] * inv[None, :]
    cosf = np.cos(ang).astype(np.float32).T
    sinf = np.sin(ang).astype(np.float32).T
    cos32 = np.concatenate([cosf, cosf], 0)
    sin32 = np.concatenate([sinf, sinf], 0)
    qs = np.float32(96 ** -0.5)

    sinks = f("even_sinks")[0]
    esk = np.zeros((128, 4), np.float32)
    for g in range(2):
        for j in range(4):
            esk[g * 64:(g + 1) * 64, j] = sinks[g * 4 + j]
    bs = f("even_b_s")[0]
    bsb = np.zeros((128, 4, 128), np.float32)
    for pr in range(4):
        bsb[0:64, pr, :] = bs[2 * pr][None, :]
        bsb[64:128, pr, :] = bs[2 * pr + 1][None, :]
    bsb = bsb.reshape(128, 512)
    vecs_common = np.zeros((128, 160), np.float32)
    vecs_common[:, 0:8] = _pc(f("even_norm_mix_g")[0], 8)
    vecs_common[:, 8:16] = _pc(f("even_norm_ffn_g")[0], 8)
    vecs_common[:, 16:24] = _pc(f("odd_norm_mix_g")[0], 8)
    vecs_common[:, 24:32] = _pc(f("odd_norm_ffn_g")[0], 8)
    vecs_common[:, 32:40] = _pc(f("final_norm_g"), 8)
    vecs_common[:, 40:88] = _pc(f("even_ada_b")[0], 48)
    vecs_common[:, 88:136] = _pc(f("odd_ada_b")[0], 48)
    vecs_common[:, 136:140] = _pc(f("odd_q_norm_g")[0], 4)
    vecs_common[:, 140:142] = _pc(f("odd_kv_norm_g")[0], 2)
    cw = f("odd_conv_w")[0]
    for j in range(3):
        vecs_common[:, 142 + j * 4:142 + (j + 1) * 4] = _pc(cw[j], 4)

    shared = {
        "vecs": vecs_common, "swab": swab, "mlam": mlam, "esk_sink": esk,
        "lng": f("even_gmlp_ln_g")[0][None, :].copy(), "lnb": f("even_gmlp_ln_b")[0][None, :].copy(),
        "bsb": bsb, "brt": f("odd_router_b")[0][None, :].copy(),
        "ada_w0": f("even_ada_w")[0], "ada_w1": f("odd_ada_w")[0],
        "w_in0": f("even_w_in")[0], "w_s": f("even_w_s")[0], "w_o0": f("even_w_o")[0],
        "wg0": f("even_ffn_w_gate")[0], "wu0": f("even_ffn_w_up")[0], "wd0": f("even_ffn_w_down")[0],
        "w_in1": f("odd_w_in")[0], "w_qb": f("odd_w_q_b")[0], "w_kvb": f("odd_w_kv_b")[0], "w_o1": f("odd_w_o")[0],
        "w_rt": f("odd_router_w")[0], "fgrow": f("final_norm_g")[None, :].copy(),
    }
    cst = np.zeros((128, 32), np.float32)
    cst[:, 0:24] = np.arange(24, dtype=np.float32)[None, :]
    cst[:, 24] = np.arange(128, dtype=np.float32)
    shared["cst"] = cst
    for nm, key in (("ewg", "odd_exp_w_gate"), ("ewu", "odd_exp_w_up")):
        w = f(key)[0].reshape(NEXP, 2, 4, 128, 7, 512)
        w = w.transpose(1, 0, 4, 3, 2, 5)
        for h in range(2):
            shared["%s_h%d" % (nm, h)] = w[h].reshape(NEXP * 7 * 128, 2048)
    w = f("odd_exp_w_down")[0].reshape(NEXP, 7, 2, 2, 128, D)
    w = w.transpose(2, 0, 1, 4, 3, 5)
    for h in range(2):
        shared["ewd_h%d" % h] = w[h].reshape(NEXP * 7 * 128, 2048)
    shared = {k: np.ascontiguousarray(v, dtype=np.float32) for k, v in shared.items()}
    cvec = f("c")
    maps = []
    orders = []
    cores = range(2 * B) if dbg_cores is None else dbg_cores
    for core in cores:
        b, hf = core // 2, core % 2
        order = list(OWN[hf]) + list(OWN[1 - hf])
        orders.append((b, order))
        xb = x[b]
        xpad = np.concatenate([np.zeros((256, D), np.float32), xb], 0)
        xm_ = np.concatenate([xb[j * TB:(j + 1) * TB] for j in order], 0)
        xh_ = np.concatenate([xpad[j * TB + 128:j * TB + 256] for j in order], 0)
        xh2_ = np.concatenate([xpad[j * TB:j * TB + 128] for j in order], 0)
        fl_swa = np.zeros((128, NPOS), np.float32)
        fl_conv = np.ones((128, NPOS), np.float32)
        for pos, j in enumerate(order):
            if j == 0:
                fl_swa[:, pos] = NEG
                fl_conv[:, pos] = 0.0
        fl_oth = np.zeros((128, NOWN), np.float32)
        for i in range(NOWN):
            fl_oth[:, i] = 1.0 if order[8 + i] < order[i] else 0.0
        tok = np.concatenate([np.arange(j * TB, (j + 1) * TB) for j in order])
        ropek = np.stack([cos32[:, tok], sin32[:, tok]], 0)
        ropeq = np.stack([cos32[:, tok[:NOWN * TB]] * qs, sin32[:, tok[:NOWN * TB]] * qs], 0)
        m = dict(shared)
        m.update({
            "xm": np.ascontiguousarray(xm_), "xh": np.ascontiguousarray(xh_), "xh2": np.ascontiguousarray(xh2_),
            "c_pc": _pc(cvec[b], 8), "fl_swa": fl_swa, "fl_conv": fl_conv, "fl_oth": fl_oth,
            "ropek": np.ascontiguousarray(ropek, dtype=np.float32),
            "ropeq": np.ascontiguousarray(ropeq, dtype=np.float32),
        })
        maps.append(m)
    return maps, orders


def kernel(**inputs):
    x = np.asarray(inputs["x"])
    B = x.shape[0]
    maps, orders = _host_inputs(inputs)
    nc = build(False)
    res = run_bass_kernel_spmd(nc, maps, core_ids=list(range(len(maps))))
    out = np.zeros((B, S, D), np.float32)
    for core, (b, order) in enumerate(orders):
        y = res.results[core]["y"]
        for i in range(NOWN):
            j = order[i]
            out[b, j * TB:(j + 1) * TB, :] = y[i * TB:(i + 1) * TB, :]
    return out
```

```python
import contextlib
import numpy as np
import concourse.bass as bass
import concourse.mybir as mybir
from concourse.bass_utils import run_bass_kernel_spmd

F32 = mybir.dt.float32
BF16 = mybir.dt.bfloat16
AF = mybir.ActivationFunctionType
ALU = mybir.AluOpType
AX = mybir.AxisListType

NDMA_SLOTS = 8
D = 1024
S = 8192
TB = 512
NPOS = 16
NOWN = 8
EPS = 1e-6
OWN = ([0, 3, 4, 7, 8, 11, 12, 15], [1, 2, 5, 6, 9, 10, 13, 14])
FF0 = 2816
FFE = 3584
NEXP = 8
NEG = -30000.0


class Buf:
    __slots__ = ("lw", "rd")

    def __init__(self):
        self.lw = None
        self.rd = {}


class Prog:
    ENG = ("pe", "act", "dve", "pool", "sp")

    def __init__(self, nc):
        self.nc = nc
        self.ops = {e: [] for e in self.ENG}
        self.cnt = {e: 0 for e in self.ENG}
        self.waited = {e: {} for e in self.ENG}
        self.dma_n = {e: 0 for e in self.ENG}
        self.last_dma = {}

    def _need(self, eng, tok, waits):
        if tok is None:
            return
        key, val, src = tok
        if src == eng and key[0] == "c" and eng == "pe":
            return
        w = self.waited[eng]
        if w.get(key, 0) >= val:
            return
        w[key] = val
        waits.append((key, val))

    def _deps(self, eng, reads, writes):
        waits = []
        for b in reads:
            self._need(eng, b.lw, waits)
        for b in writes:
            self._need(eng, b.lw, waits)
            for t in b.rd.values():
                self._need(eng, t, waits)
        m = {}
        for k, v in waits:
            m[k] = max(m.get(k, 0), v)
        return list(m.items())

    def _mark(self, tok, reads, writes):
        for b in reads:
            o = b.rd.get(tok[0])
            if o is None or o[1] < tok[1]:
                b.rd[tok[0]] = tok
        for b in writes:
            b.lw = tok
            b.rd = {}

    def op(self, eng, fn, reads=(), writes=()):
        waits = self._deps(eng, reads, writes)
        self.cnt[eng] += 1
        key = "c:" + eng
        tok = (key, self.cnt[eng], eng)
        self.ops[eng].append((waits, fn, (key, 1)))
        self._mark(tok, reads, writes)
        return tok

    def dma(self, eng, out, in_, reads=(), writes=(), **kw):
        n = self.dma_n[eng]
        self.dma_n[eng] += 1
        slot = n % NDMA_SLOTS
        key = "d:%s:%d" % (eng, slot)
        val = 16 * (n // NDMA_SLOTS + 1)
        waits = self._deps(eng, reads, writes)
        if n >= NDMA_SLOTS:
            pv = val - 16
            if self.waited[eng].get(key, 0) < pv:
                self.waited[eng][key] = pv
                waits.append((key, pv))
        tok = (key, val, eng)
        self.last_dma[key] = tok

        def fn(e, out=out, in_=in_, kw=kw):
            return e.dma_start(out=out, in_=in_, **kw)
        self.ops[eng].append((waits, fn, (key, 16)))
        self._mark(tok, reads, writes)
        return tok

    def idma(self, fn, reads=(), writes=()):
        eng = "pool"
        n = self.dma_n[eng]
        self.dma_n[eng] += 1
        slot = n % NDMA_SLOTS
        key = "d:%s:%d" % (eng, slot)
        val = 16 * (n // NDMA_SLOTS + 1)
        waits = self._deps(eng, reads, writes)
        if n >= NDMA_SLOTS:
            pv = val - 16
            if self.waited[eng].get(key, 0) < pv:
                self.waited[eng][key] = pv
                waits.append((key, pv))
        tok = (key, val, eng)
        self.last_dma[key] = tok
        self.ops[eng].append((waits, fn, (key, 16)))
        self._mark(tok, reads, writes)
        return tok

    def wait_tok(self, eng, tok):
        waits = []
        self._need(eng, tok, waits)
        if waits:
            self.ops[eng].append((waits, None, None))

    def barrier(self):
        toks = [("c:" + e, self.cnt[e], e) for e in self.ENG if self.cnt[e] > 0]
        toks += list(self.last_dma.values())
        for e in self.ENG:
            for t in toks:
                self.wait_tok(e, t)

    def emit(self):
        nc = self.nc
        keys = set()
        for e in self.ENG:
            for waits, fn, inc in self.ops[e]:
                for k, _ in waits:
                    keys.add(k)
                if inc is not None:
                    keys.add(inc[0])
        sems = {}
        with contextlib.ExitStack() as st:
            for k in sorted(keys):
                sems[k] = st.enter_context(nc.semaphore(k.replace(":", "_")))
            block = st.enter_context(nc.Block())

            def run(e, lst):
                for waits, fn, inc in lst:
                    for k, v in waits:
                        e.wait_ge(sems[k], v)
                    if fn is not None:
                        ins = fn(e)
                        ins.then_inc(sems[inc[0]], inc[1])

            @block.tensor
            def _(e):
                run(e, self.ops["pe"])

            @block.scalar
            def _(e):
                run(e, self.ops["act"])

            @block.vector
            def _(e):
                run(e, self.ops["dve"])

            @block.gpsimd
            def _(e):
                run(e, self.ops["pool"])

            @block.sync
            def _(e):
                run(e, self.ops["sp"])


class Tl:
    __slots__ = ("t", "b")

    def __init__(self, t):
        self.t = t
        self.b = Buf()

    def __getitem__(self, k):
        return self.t[k]


def build(dbg=False):
    nc = bass.Bass("TRN2", target_bir_lowering=False)
    p = Prog(nc)

    def din(name, shape):
        return nc.dram_tensor(name, list(shape), F32, kind="ExternalInput").ap()

    def dscr(name, shape, dt):
        return nc.dram_tensor(name, list(shape), dt).ap()

    xm = din("xm", [NPOS * TB, D])
    xh = din("xh", [NPOS * 128, D])
    xh2 = din("xh2", [NPOS * 128, D])
    c_pc = din("c_pc", [128, 8])
    vecs = din("vecs", [128, 160])
    fl_swa_d = din("fl_swa", [128, NPOS])
    fl_conv_d = din("fl_conv", [128, NPOS])
    fl_oth_d = din("fl_oth", [128, NOWN])
    ropek_d = din("ropek", [2, 32, S])
    ropeq_d = din("ropeq", [2, 32, NOWN * TB])
    swab_d = din("swab", [128, 2 * 8 * 128])
    mlam_d = din("mlam", [128, 4 * TB])
    esk_d = din("esk_sink", [128, 4])
    lng_d = din("lng", [1, 512])
    lnb_d = din("lnb", [1, 512])
    bsb_d = din("bsb", [128, 4 * 128])
    brt_d = din("brt", [1, 8])
    ada_w = [din("ada_w0", [D, 6 * D]), din("ada_w1", [D, 6 * D])]
    w_in0 = din("w_in0", [D, 1792])
    w_s = din("w_s", [8, 128, 128])
    w_o0 = din("w_o0", [D, D])
    wg0 = din("wg0", [D, FF0])
    wu0 = din("wu0", [D, FF0])
    wd0 = din("wd0", [FF0, D])
    w_in1 = din("w_in1", [D, 2336])
    w_qb = din("w_qb", [512, 768])
    w_kvb = din("w_kvb", [256, 1024])
    w_o1 = din("w_o1", [D, D])
    w_rt = din("w_rt", [D, 8])
    NWR = NEXP * 7 * 128
    ewg_h = [nc.dram_tensor("ewg_h%d" % h, [NWR, 2048], F32, kind="ExternalInput") for h in range(2)]
    ewu_h = [nc.dram_tensor("ewu_h%d" % h, [NWR, 2048], F32, kind="ExternalInput") for h in range(2)]
    ewd_h = [nc.dram_tensor("ewd_h%d" % h, [NWR, 2048], F32, kind="ExternalInput") for h in range(2)]
    cst_d = din("cst", [128, 32])
    fgrow_d = din("fgrow", [1, D])
    y_out = nc.dram_tensor("y", [NOWN * TB, D], F32, kind="ExternalOutput").ap()

    NCOL = NPOS * TB + NPOS * 128
    hT_d = dscr("hT_d", [D, NCOL], F32)
    cT_d = dscr("cT_d", [512, NPOS * TB], BF16)
    qT_d = dscr("qT_d", [8, 96, NOWN * TB], BF16)
    knT_d = dscr("knT_d", [512, S], BF16)
    kpeT_d = dscr("kpeT_d", [32, S], BF16)
    v_d = dscr("v_d", [S, 512], BF16)
    dT_d = dscr("dT_d", [512, NOWN * TB], BF16)
    NBLK = 23
    LSLOT = NBLK * TB
    ew_src = {"g": ewg_h, "u": ewu_h, "d": ewd_h}
    ew_bf = {k: [nc.dram_tensor("ew%s_b%d" % (k, h), [NWR, 2048], BF16) for h in range(2)] for k in "gud"}
    cv_jobs = [(k, h, r) for r in range(7) for k in "gud" for h in range(2)]
    b_cv = [Buf() for _ in cv_jobs]
    cv_next = [0]

    def emit_conversions(n):
        for _ in range(n):
            if cv_next[0] >= len(cv_jobs):
                return
            j = cv_next[0]
            cv_next[0] += 1
            k, h, r = cv_jobs[j]
            p.dma("pool", ew_bf[k][h].ap()[r * 1024:(r + 1) * 1024, :], ew_src[k][h].ap()[r * 1024:(r + 1) * 1024, :],
                  [], [b_cv[j]])
    h3tok_d = dscr("h3tok_d", [NOWN * TB, D], F32)
    hn3tok_d = dscr("hn3tok_d", [NOWN * TB, D], F32)
    xs_h = nc.dram_tensor("xs_d", [LSLOT, D], F32)
    ys_h = nc.dram_tensor("ys_d", [LSLOT, D], F32)
    xs_d = xs_h.ap()
    ys_d = ys_h.ap()
    g2row_d = dscr("g2row_d", [D], F32)
    b_xs = Buf(); b_ys = [Buf() for _ in range(NBLK)]; b_g2row = Buf()
    b_h3t = [Buf() for _ in range(32)]; b_hn3t = [Buf() for _ in range(32)]
    b_hT = [Buf() for _ in range(NPOS + 4)]
    b_cT = [Buf() for _ in range(NPOS)]
    b_q = Buf(); b_kn = Buf(); b_kpe = Buf(); b_v = Buf()
    b_dT = [Buf() for _ in range(NOWN)]
    dbg_out = {}
    if dbg:
        dbg_out["dbg_h"] = nc.dram_tensor("dbg_h", [D, NCOL], F32, kind="ExternalOutput").ap()

    def bl(xs):
        return [x.b if isinstance(x, Tl) else x for x in xs]

    def MM(out, lhsT, rhs, start, stop, R, W):
        p.op("pe", lambda e: e.matmul(out, lhsT=lhsT, rhs=rhs, start=start, stop=stop), bl(R), bl(W))

    def TR(out, in_, ident, R, W):
        p.op("pe", lambda e: e.transpose(out, in_, ident), bl(R), bl(W))

    def ACT(out, in_, func, R, W, **kw):
        p.op("act", lambda e: e.activation(out=out, in_=in_, func=func, **kw), bl(R), bl(W))

    def TT(eng, out, in0, in1, op, R, W):
        p.op(eng, lambda e: e.tensor_tensor(out=out, in0=in0, in1=in1, op=op), bl(R), bl(W))

    def TS(eng, out, in0, s1, op0, R, W, s2=None, op1=None):
        if op1 is None:
            p.op(eng, lambda e: e.tensor_scalar(out=out, in0=in0, scalar1=s1, scalar2=None, op0=op0), bl(R), bl(W))
        else:
            p.op(eng, lambda e: e.tensor_scalar(out=out, in0=in0, scalar1=s1, scalar2=s2, op0=op0, op1=op1), bl(R), bl(W))

    def STT(eng, out, in0, scalar, in1, op0, op1, R, W):
        p.op(eng, lambda e: e.scalar_tensor_tensor(out=out, in0=in0, scalar=scalar, in1=in1, op0=op0, op1=op1),
             bl(R), bl(W))

    def CP(eng, out, in_, R, W):
        if eng == "act":
            ACT(out, in_, AF.Copy, R, W)
        else:
            p.op(eng, lambda e: e.tensor_copy(out=out, in_=in_), bl(R), bl(W))

    def RCP(out, in_, R, W):
        p.op("dve", lambda e: e.reciprocal(out=out, in_=in_), bl(R), bl(W))

    def RED(out, in_, op, R, W):
        p.op("dve", lambda e: e.tensor_reduce(out=out, in_=in_, axis=AX.X, op=op), bl(R), bl(W))

    def MEMSET(eng, ap, val, W):
        p.op(eng, lambda e: e.memset(ap, val), (), bl(W))

    def DMA(q, out, in_, R, W, **kw):
        return p.dma(q, out, in_, bl(R), bl(W), **kw)

    evac_rr = [0]

    def EVAC(out, in_, R, W):
        evac_rr[0] ^= 1
        CP("act" if evac_rr[0] else "dve", out, in_, R, W)

    with contextlib.ExitStack() as top:
        uid = [0]

        def sbt(st, name, shape, dt):
            uid[0] += 1
            return Tl(st.enter_context(nc.sbuf_tensor("s%d_%s" % (uid[0], name), list(shape), dt)))

        pb = [Tl(top.enter_context(nc.psum_tensor("pb%d" % i, [128, 512], F32))) for i in range(8)]
        acc_rr = [0]

        def nacc():
            acc_rr[0] ^= 1
            return pb[acc_rr[0]]

        ident = sbt(top, "ident", [128, 128], F32)
        ones_f = sbt(top, "ones_f", [128, 128], F32)
        ones_b = sbt(top, "ones_b", [128, 128], BF16)
        vec = sbt(top, "vec", [128, 160], F32)
        modT = [sbt(top, "modT%d" % l, [128, 48], F32) for l in range(2)]
        gsc = sbt(top, "gsc", [128, 32], F32)
        flsw = sbt(top, "flsw", [128, NPOS], F32)
        flcv = sbt(top, "flcv", [128, NPOS], F32)
        flot = sbt(top, "flot", [128, NOWN], F32)
        hcomp = sbt(top, "hcomp", [128, 8, 32], F32)
        MEMSET("pool", ident[:], 0.0, [ident])
        p.op("pool", lambda e: e.affine_select(out=ident[:], in_=ident[:], pattern=[[-1, 128]],
                                               compare_op=ALU.not_equal, fill=1.0, base=0, channel_multiplier=1),
             (), [ident.b])
        MEMSET("pool", ones_f[:], 1.0, [ones_f])
        MEMSET("pool", ones_b[:], 1.0, [ones_b])
        DMA("sp", vec[:], vecs, [], [vec])
        DMA("sp", flsw[:], fl_swa_d, [], [flsw])
        DMA("sp", flcv[:], fl_conv_d, [], [flcv])
        DMA("sp", flot[:], fl_oth_d, [], [flot])
        V_G = [0, 8, 16, 24]
        V_FG = 32
        V_AB = [40, 88]
        V_QG = 136
        V_KG = 140
        V_CW = 142

        with contextlib.ExitStack() as st:
            cT = sbt(st, "cT", [128, 8], F32)
            condT = sbt(st, "condT", [128, 8], F32)
            awp = [sbt(st, "awp%d" % i, [128, 8, 512], F32) for i in range(2)]
            DMA("sp", cT[:], c_pc, [], [cT])
            ACT(condT[:], cT[:], AF.Silu, [cT], [condT])
            for l in range(2):
                mp = pb[2 + l]
                for nb in range(12):
                    a = awp[nb % 2]
                    DMA("sp", a[:], ada_w[l][:, nb * 512:(nb + 1) * 512].rearrange("(c p) f -> p c f", p=128), [], [a])
                    for k4 in range(4):
                        k = nb * 4 + k4
                        for c in range(8):
                            MM(mp[:, k:k + 1], a[:, c, k4 * 128:(k4 + 1) * 128], condT[:, c:c + 1], c == 0, c == 7,
                               [a, condT], [mp])
                TT("dve", modT[l][:], mp[:, 0:48], vec[:, V_AB[l]:V_AB[l] + 48], ALU.add, [mp, vec], [modT[l]])
            for n, (l, so) in enumerate([(0, 8), (0, 32), (1, 8), (1, 32)]):
                TS("dve", gsc[:, n * 8:(n + 1) * 8], modT[l][:, so:so + 8], 1.0, ALU.add, [modT[l]], [gsc])
                TT("dve", gsc[:, n * 8:(n + 1) * 8], gsc[:, n * 8:(n + 1) * 8], vec[:, V_G[n]:V_G[n] + 8], ALU.mult,
                   [vec], [gsc])
        p.barrier()

        def SH(l, which):
            o = 0 if which == 1 else 24
            return modT[l][:, o:o + 8]

        def GATE(l, which):
            o = 16 if which == 1 else 40
            return modT[l][:, o:o + 8]

        def norm_mod(src, KC, N, dfeat, gs_ap, sh_ap, dst, scr):
            sq, tmp, rr = scr
            stat = pb[7]
            for c in range(KC):
                s = sq[c % 2]
                ACT(s[:, :N], src[:, c, :], AF.Square, [src], [s])
                MM(stat[:, :N], ones_f[:], s[:, :N], c == 0, c == KC - 1, [ones_f, s], [stat])
            ACT(rr[:, :N], stat[:, :N], AF.Ln, [stat], [rr], bias=EPS_AP[:, 0:1], scale=1.0 / dfeat)
            ACT(rr[:, :N], rr[:, :N], AF.Exp, [rr], [rr], scale=-0.5)
            for c in range(KC):
                t = tmp[c % 2]
                TT("dve", t[:, :N], src[:, c, :], rr[:, :N], ALU.mult, [src, rr], [t])
                if sh_ap is None:
                    ACT(dst[:, c, :], t[:, :N], AF.Identity, [t, gsc, vec], [dst], scale=gs_ap[:, c:c + 1],
                        bias=ZERO_AP[:, 0:1])
                else:
                    ACT(dst[:, c, :], t[:, :N], AF.Identity, [t, gsc, vec, modT[0], modT[1]], [dst],
                        scale=gs_ap[:, c:c + 1], bias=sh_ap[:, c:c + 1])

        epsT = sbt(top, "epsT", [128, 1], F32)
        MEMSET("pool", epsT[:], EPS, [epsT])
        EPS_AP = epsT.t
        zeroT = sbt(top, "zeroT", [128, 1], F32)
        MEMSET("pool", zeroT[:], 0.0, [zeroT])
        ZERO_AP = zeroT.t

        def load_hT(tile, blk, q="sp"):
            DMA(q, tile[:], hT_d[:, blk * TB:(blk + 1) * TB].rearrange("(c p) n -> p c n", p=128), [b_hT[blk]], [tile])

        def store_hT(tile, blk, q="sp"):
            DMA(q, hT_d[:, blk * TB:(blk + 1) * TB].rearrange("(c p) n -> p c n", p=128), tile[:], [tile], [b_hT[blk]])

        with contextlib.ExitStack() as st:
            Win0 = sbt(st, "Win0", [128, 8, 1792], BF16)
            Wo0 = sbt(st, "Wo0", [128, 8, D], BF16)
            WsT = sbt(st, "WsT", [128, 8, 128], BF16)
            biasT = sbt(st, "biasT", [128, 2, 8 * 128], F32)
            eskf = sbt(st, "eskf", [128, 4, 128], F32)
            esk = sbt(st, "esk", [128, 4], F32)
            lngb = sbt(st, "lngb", [128, 512], F32)
            lnbb = sbt(st, "lnbb", [128, 512], F32)
            bsb = sbt(st, "bsb", [128, 4, 128], F32)
            xt = sbt(st, "xt", [128, 4, D], F32)
            xT2 = None
            hnT2 = None
            sq = [sbt(st, "sq%d" % i, [128, TB], F32) for i in range(2)]
            tmpn = [sbt(st, "tmpn%d" % i, [128, TB], F32) for i in range(2)]
            rr = sbt(st, "rr", [128, TB], F32)
            scr = (sq, tmpn, rr)
            QT = sbt(st, "QT", [128, 4, TB], BF16)
            KTc = sbt(st, "KTc", [128, TB], BF16)
            Vc = sbt(st, "Vc", [128, 4, 128], BF16)
            KTh = sbt(st, "KTh", [128, NPOS, 128], BF16)
            Vh = sbt(st, "Vh", [128, NPOS, 128], BF16)
            KTh2 = sbt(st, "KTh2", [128, NPOS, 128], BF16)
            Vh2 = sbt(st, "Vh2", [128, NPOS, 128], BF16)
            uT = sbt(st, "uT", [128, 4, TB], BF16)
            gg = [sbt(st, "gg%d" % i, [128, 512], F32) for i in range(2)]
            vn = [sbt(st, "vn%d" % i, [128, 512], BF16) for i in range(2)]
            st8 = [sbt(st, "st8_%d" % i, [128, 8], F32) for i in range(2)]
            sstage = sbt(st, "sstage", [128, 4, TB], F32)
            scs = [sbt(st, "scs%d" % i, [128, 512], F32) for i in range(2)]
            PT = [[sbt(st, "PT%d%d" % (g, pc), [128, 512], BF16) for pc in range(2)] for g in range(2)]
            den1 = sbt(st, "den", [128, 512], F32)
            den = [den1, den1]

            dnb = [Tl(den1.t), Tl(den1.t)]
            mixT = sbt(st, "mixT", [128, 8, TB], BF16)

            Wq_v = Win0[:, :, 0:512].rearrange("p c (j g d) -> p c j g d", g=2, d=64)
            for g in range(2):
                for c in range(8):
                    DMA("pool", Wq_v[:, c, :, g, :],
                        w_in0[c * 128:(c + 1) * 128, g * 256:(g + 1) * 256].rearrange("p (j d) -> p j d", d=64),
                        [], [Win0])
            DMA("pool", Win0[:, :, 512:1792], w_in0[:, 512:1792].rearrange("(c p) f -> p c f", p=128), [], [Win0])
            for g in range(2):
                DMA("pool", Wo0[g * 64:(g + 1) * 64, 0:4, :],
                    w_o0[g * 256:(g + 1) * 256, :].rearrange("(j d) m -> d j m", d=64), [], [Wo0])
            DMA("pool", Wo0[:, 4:8, :], w_o0[512:1024, :].rearrange("(c p) m -> p c m", p=128), [], [Wo0])
            DMA("sp", biasT[:], swab_d.rearrange("p (a b) -> p a b", a=2), [], [biasT])
            DMA("sp", esk[:], esk_d, [], [esk])
            DMA("sp", lngb[:], lng_d.partition_broadcast(128), [], [lngb])
            DMA("sp", lnbb[:], lnb_d.partition_broadcast(128), [], [lnbb])
            DMA("sp", bsb[:], bsb_d.rearrange("p (a b) -> p a b", a=4), [], [bsb])
            st_setup = contextlib.ExitStack()
            tri = sbt(st_setup, "tri", [128, 128], F32)
            wsn = sbt(st_setup, "wsn", [128, 8, 128], F32)
            DMA("sp", wsn[:], w_s.rearrange("g t s -> t g s"), [], [wsn])
            MEMSET("pool", tri[:], 1.0, [tri])
            p.op("pool", lambda e: e.affine_select(out=tri[:], in_=tri[:], pattern=[[1, 128]],
                                                   compare_op=ALU.is_ge, fill=0.0, base=0, channel_multiplier=-1),
                 (), [tri.b])
            for g8 in range(8):
                a = nacc()
                TR(a[:, 0:128], wsn[:, g8, :], ident[:], [wsn, ident], [a])
                TT("dve", WsT[:, g8, :], a[:, 0:128], tri[:], ALU.mult, [a, tri], [WsT])
            ACT(esk[:], esk[:], AF.Exp, [esk], [esk])
            for j in range(4):
                TS("dve", eskf[:, j, :], ones_f[:], esk[:, j:j + 1], ALU.mult, [ones_f, esk], [eskf])

            p.barrier()
            st_setup.close()
            xts = [xt, xt]
            xT2 = [sbt(st, "xTa", [128, 8, TB], F32), sbt(st, "xTb", [128, 8, TB], F32)]
            hnT1 = sbt(st, "hnTa", [128, 8, TB], BF16)
            hnT2 = [hnT1, hnT1]
            gg4 = gg + [sbt(st, "gg%d" % i, [128, 512], F32) for i in (2, 3)]
            vn4 = vn + [sbt(st, "vn%d" % i, [128, 512], BF16) for i in (2, 3)]
            st84 = st8 + [sbt(st, "st8_%d" % i, [128, 8], F32) for i in (2, 3)]
            PT2 = [PT] + [[[sbt(st, "PT%s%d%d" % (k, g, pc), [128, 512], BF16) for pc in range(2)] for g in range(2)]
                          for k in "bcd"]
            scs4 = gg4

            def a1_stageA(xt, n):
                xT, hnT = xT2[n % 2], hnT2[n % 2]
                for c in range(8):
                    a = nacc()
                    for s in range(4):
                        TR(a[:, s * 128:(s + 1) * 128], xt[:, s, c * 128:(c + 1) * 128], ident[:], [xt, ident], [a])
                    EVAC(xT[:, c, :], a[:], [a], [xT])
                norm_mod(xT, 8, TB, float(D), gsc[:, 0:8], SH(0, 1), hnT, scr)

            def a1_stageB1(n, kind, idx):
                xT, hnT = xT2[n % 2], hnT2[n % 2]
                a = nacc()
                for c in range(8):
                    MM(a[:], Win0[:, c, 512:640], hnT[:, c, :], c == 0, c == 7, [Win0, hnT], [a])
                EVAC(KTc[:], a[:], [a], [KTc])
                a = nacc()
                for s in range(4):
                    for c in range(8):
                        MM(a[:, s * 128:(s + 1) * 128], hnT[:, c, s * 128:(s + 1) * 128], Win0[:, c, 640:768],
                           c == 0, c == 7, [Win0, hnT], [a])
                EVAC(Vc[:].rearrange("p s d -> p (s d)"), a[:], [a], [Vc])
                if kind == "h2":
                    CP("pool", KTh2[:, idx * 4:(idx + 1) * 4, :].rearrange("p s d -> p (s d)"), KTc[:], [KTc], [KTh2])
                    CP("pool", Vh2[:, idx * 4:(idx + 1) * 4, :], Vc[:], [Vc], [Vh2])
                    return
                if kind == "halo":
                    CP("pool", KTh[:, idx * 4:(idx + 1) * 4, :].rearrange("p s d -> p (s d)"), KTc[:], [KTc], [KTh])
                    CP("pool", Vh[:, idx * 4:(idx + 1) * 4, :], Vc[:], [Vc], [Vh])
                for s in range(4):
                    a = nacc()
                    for c in range(8):
                        MM(a[:], hnT[:, c, s * 128:(s + 1) * 128], Win0[:, c, 1280:1792], c == 0, c == 7,
                           [Win0, hnT], [a])
                    g_, s8, v_ = gg4[s], st84[s], vn4[s]
                    MEMSET("pool", s8[:, 0:2], 0.0, [s8])
                    ACT(g_[:], a[:], AF.Gelu, [a], [g_, s8], accum_out=s8[:, 0:1])
                    ACT(den1[:], g_[:], AF.Square, [g_], [den1, dnb[0], dnb[1], s8], accum_out=s8[:, 1:2])
                R4 = range(4)
                for s in R4:
                    TS("dve", st84[s][:, 2:3], st84[s][:, 0:1], 1.0 / 512, ALU.mult, [st84[s]], [st84[s]])
                for s in R4:
                    TT("dve", st84[s][:, 3:4], st84[s][:, 2:3], st84[s][:, 2:3], ALU.mult, [st84[s]], [st84[s]])
                for s in R4:
                    STT("dve", st84[s][:, 4:5], st84[s][:, 1:2], 1.0 / 512, st84[s][:, 3:4], ALU.mult, ALU.subtract,
                        [st84[s]], [st84[s]])
                for s in R4:
                    ACT(st84[s][:, 5:6], st84[s][:, 4:5], AF.Sqrt, [st84[s]], [st84[s]], bias=EPS_AP[:, 0:1], scale=1.0)
                for s in R4:
                    RCP(st84[s][:, 5:6], st84[s][:, 5:6], [st84[s]], [st84[s]])
                for s in R4:
                    STT("dve", st84[s][:, 6:7], st84[s][:, 2:3], -1.0, st84[s][:, 5:6], ALU.mult, ALU.mult,
                        [st84[s]], [st84[s]])
                for s in R4:
                    ACT(gg4[s][:], gg4[s][:], AF.Identity, [gg4[s], st84[s]], [gg4[s]], scale=st84[s][:, 5:6],
                        bias=st84[s][:, 6:7])
                for s in R4:
                    TT("dve", gg4[s][:], gg4[s][:], lngb[:], ALU.mult, [gg4[s], lngb], [gg4[s]])
                for s in R4:
                    TT("pool", vn4[s][:], gg4[s][:], lnbb[:], ALU.add, [gg4[s], lnbb], [vn4[s]])
                for j in range(4):
                    a = nacc()
                    for c in range(8):
                        MM(a[:], Win0[:, c, j * 128:(j + 1) * 128], hnT[:, c, :], c == 0, c == 7, [Win0, hnT], [a])
                    ACT(QT[:, j, :], a[:], AF.Copy, [a], [QT], scale=0.125)
                for uc in range(4):
                    a = nacc()
                    for c in range(8):
                        MM(a[:], Win0[:, c, 768 + uc * 128:768 + (uc + 1) * 128], hnT[:, c, :], c == 0, c == 7,
                           [Win0, hnT], [a])
                    ACT(uT[:, uc, :], a[:], AF.Gelu, [a], [uT])

            def a1_stageB1b(n, kind, idx):
                if kind == "h2":
                    return
                xT, hnT = xT2[n % 2], hnT2[n % 2]

                def kv_prev(s):
                    if kind == "halo":
                        return KTh2[:, idx * 4 + s, :], Vh2[:, idx * 4 + s, :], [KTh2, Vh2]
                    if s > 0:
                        return KTc[:, (s - 1) * 128:s * 128], Vc[:, s - 1, :], [KTc, Vc]
                    return KTh[:, idx, :], Vh[:, idx, :], [KTh, Vh]

                def swa1(s):
                    ktp, vp, rp = kv_prev(s)
                    ktc = KTc[:, s * 128:(s + 1) * 128]
                    P_ = PT2[s]
                    for g in range(2):
                        r0 = g * 64
                        for pc in range(2):
                            un = (s * 4 + g * 2 + pc) % 4
                            sbk = pb[2 + un]
                            kt = ktp if pc == 0 else ktc
                            for j in range(4):
                                MM(sbk[:, j * 128:(j + 1) * 128], kt[r0:r0 + 64, :], QT[r0:r0 + 64, j, s * 128:(s + 1) * 128],
                                   True, True, [QT, KTc] + rp, [sbk])
                            sc_ = scs4[un]
                            bia = biasT[:, pc, g * 512:(g + 1) * 512]
                            if kind == "main" and s == 0 and pc == 0:
                                STT("dve", sc_[:], sbk[:], flsw[:, idx:idx + 1], bia, ALU.add, ALU.add,
                                    [sbk, flsw, biasT], [sc_])
                            else:
                                TT("dve", sc_[:], sbk[:], bia, ALU.add, [sbk, biasT], [sc_])
                            ACT(P_[g][pc][:], sc_[:], AF.Exp, [sc_], [P_[g][pc]])

                def swa2(s):
                    ktp, vp, rp = kv_prev(s)
                    vc = Vc[:, s, :]
                    P_ = PT2[s]
                    for g in range(2):
                        px, pd = pb[4 + g], pb[6 + g]
                        for j in range(4):
                            js = slice(j * 128, (j + 1) * 128)
                            MM(px[:, js], vp, P_[g][0][:, js], True, False, [P_[g][0], Vc] + rp, [px])
                            MM(px[:, js], vc, P_[g][1][:, js], False, True, [P_[g][1], Vc], [px])
                            MM(pd[:, js], ones_b[:], P_[g][0][:, js], True, False, [P_[g][0], ones_b], [pd])
                            MM(pd[:, js], ones_b[:], P_[g][1][:, js], False, True, [P_[g][1], ones_b], [pd])
                    for g in range(2):
                        r0 = g * 64
                        TT("dve", dnb[g][r0:r0 + 64, :], pb[6 + g][r0:r0 + 64, :],
                           eskf[r0:r0 + 64, :, :].rearrange("p a b -> p (a b)"), ALU.add, [pb[6 + g], eskf], [dnb[g]])
                    for g in range(2):
                        r0 = g * 64
                        ACT(dnb[g][r0:r0 + 64, :], dnb[g][r0:r0 + 64, :], AF.Ln, [dnb[g]], [dnb[g]])
                    for g in range(2):
                        r0 = g * 64
                        ACT(dnb[g][r0:r0 + 64, :], dnb[g][r0:r0 + 64, :], AF.Exp, [dnb[g]], [dnb[g]], scale=-1.0)
                    for g in range(2):
                        r0 = g * 64
                        TT("dve", mixT[r0:r0 + 64, 0:4, s * 128:(s + 1) * 128],
                           pb[4 + g][r0:r0 + 64, :].rearrange("p (a b) -> p a b", a=4),
                           dnb[g][r0:r0 + 64, :].rearrange("p (a b) -> p a b", a=4), ALU.mult, [pb[4 + g], dnb[g]], [mixT])

                for s in range(4):
                    swa1(s)
                    v_ = vn4[s]
                    for pr in range(4):
                        for hf in range(2):
                            MM(pb[hf][:, pr * 128:(pr + 1) * 128], v_[:, pr * 128:(pr + 1) * 128],
                               WsT[:, 2 * pr + hf, :], True, True, [v_, WsT], [pb[hf]])
                    for hf in range(2):
                        r0 = hf * 64
                        TT("dve", sstage[r0:r0 + 64, :, s * 128:(s + 1) * 128],
                           pb[hf][r0:r0 + 64, :].rearrange("p (a b) -> p a b", a=4), bsb[r0:r0 + 64, :, :],
                           ALU.add, [pb[hf], bsb], [sstage])
                for s in range(4):
                    swa2(s)
                TT("pool", mixT[:, 4:8, :], uT[:], sstage[:], ALU.mult, [uT, sstage], [mixT])

            def a1_stageB2(n, kind, idx):
                xT = xT2[n % 2]
                if kind == "h2":
                    return
                for dc in range(8):
                    a = nacc()
                    for mc in range(8):
                        MM(a[:], Wo0[:, mc, dc * 128:(dc + 1) * 128], mixT[:, mc, :], mc == 0, mc == 7, [Wo0, mixT], [a])
                    STT("dve", xT[:, dc, :], a[:], GATE(0, 1)[:, dc:dc + 1], xT[:, dc, :], ALU.mult, ALU.add,
                        [a, modT[0], xT], [xT])
                if kind == "main":
                    store_hT(xT, idx)
                else:
                    for s in range(4):
                        c0 = (idx * 4 + s) * 2
                        CP("pool", hcomp[:, :, c0:c0 + 2], xT[:, :, s * 128 + 126:s * 128 + 128], [xT], [hcomp])

            work = [(xh2[gq * 512:(gq + 1) * 512, :], "h2", gq) for gq in range(4)]
            work += [(xh[gq * 512:(gq + 1) * 512, :], "halo", gq) for gq in range(4)]
            work += [(xm[pos * 512:(pos + 1) * 512, :], "main", pos) for pos in range(NPOS)]

            def x_fetch(n):
                DMA("sp", xts[n % 2][:], work[n][0].rearrange("(s p) d -> p s d", p=128), [], [xts[n % 2]])

            x_fetch(0)
            a1_stageA(xt, 0)
            x_fetch(1)
            for n in range(len(work)):
                a1_stageB1(n, work[n][1], work[n][2])
                if n + 1 < len(work):
                    a1_stageA(xt, n + 1)
                if n + 2 < len(work):
                    x_fetch(n + 2)
                a1_stageB1b(n, work[n][1], work[n][2])
                a1_stageB2(n, work[n][1], work[n][2])
                emit_conversions(2)
        p.barrier()

        with contextlib.ExitStack() as st:
            Wg = sbt(st, "Wg", [128, 8, FF0], BF16)
            Wu = sbt(st, "Wu", [128, 8, FF0], BF16)
            Wdp = [sbt(st, "Wdp%d" % i, [128, 22, 128], BF16) for i in range(3)]
            hb = [sbt(st, "hb%d" % i, [128, 8, TB], F32) for i in range(2)]
            hn = sbt(st, "hn2", [128, 8, TB], BF16)
            actT = sbt(st, "actT", [128, 22, TB], BF16)
            sq = [sbt(st, "sq%d" % i, [128, TB], F32) for i in range(2)]
            tmpn = [sbt(st, "tmpn%d" % i, [128, TB], F32) for i in range(2)]
            rr = sbt(st, "rr", [128, TB], F32)
            sg = [sbt(st, "sg%d" % i, [128, TB], F32) for i in range(2)]
            for c in range(8):
                DMA("pool", Wg[:, c, :], wg0[c * 128:(c + 1) * 128, :], [], [Wg])
                DMA("pool", Wu[:, c, :], wu0[c * 128:(c + 1) * 128, :], [], [Wu])
            hnc = sbt(st, "hnc", [128, 8, 32], BF16)
            actc = sbt(st, "actc", [128, 22, 32], BF16)
            wdn = [0]

            def a2_norm(h, hn_t, N):
                norm_mod(h, 8, N, float(D), gsc[:, 8:16], SH(0, 2), hn_t, (sq, tmpn, rr))

            def a2_body(h, hn_t, act_t, N, mid=None):
                for fc in range(22):
                    ag, au = pb[2 + 2 * (fc % 2)], pb[3 + 2 * (fc % 2)]
                    for c in range(8):
                        MM(ag[:, :N], Wg[:, c, fc * 128:(fc + 1) * 128], hn_t[:, c, :], c == 0, c == 7, [Wg, hn_t], [ag])
                    for c in range(8):
                        MM(au[:, :N], Wu[:, c, fc * 128:(fc + 1) * 128], hn_t[:, c, :], c == 0, c == 7, [Wu, hn_t], [au])
                    s_ = sg[fc % 2]
                    ACT(s_[:, :N], ag[:, :N], AF.Silu, [ag], [s_])
                    TT("dve", act_t[:, fc, :], s_[:, :N], au[:, :N], ALU.mult, [s_, au], [act_t])
                if mid is not None:
                    mid()
                for dc in range(8):
                    w = Wdp[wdn[0] % 3]
                    wdn[0] += 1
                    DMA("pool", w[:], wd0[:, dc * 128:(dc + 1) * 128].rearrange("(f p) m -> p f m", p=128), [], [w])
                    a = nacc()
                    for fc in range(22):
                        MM(a[:, :N], w[:, fc, :], act_t[:, fc, :], fc == 0, fc == 21, [w, act_t], [a])
                    STT("dve", h[:, dc, :], a[:, :N], GATE(0, 2)[:, dc:dc + 1], h[:, dc, :], ALU.mult, ALU.add,
                        [a, modT[0], h], [h])

            hnB = sbt(st, "hn2b", [128, 8, TB], BF16)
            hns = [hn, hnB]
            load_hT(hb[0], 0)
            a2_norm(hcomp, hnc, 32)
            a2_body(hcomp, hnc, actc, 32, mid=lambda: a2_norm(hb[0], hns[0], TB))
            for blk in range(NPOS):
                h = hb[blk % 2]
                if blk + 1 < NPOS:
                    load_hT(hb[(blk + 1) % 2], blk + 1)
                    nxt = (lambda b=blk: a2_norm(hb[(b + 1) % 2], hns[(b + 1) % 2], TB))
                else:
                    nxt = None
                a2_body(h, hns[blk % 2], actT, TB, mid=nxt)
                store_hT(h, blk)
                if dbg:
                    DMA("sp", dbg_out["dbg_h"][:, blk * TB:(blk + 1) * TB].rearrange("(c p) n -> p c n", p=128), h[:],
                        [h], [])
        p.barrier()

        with contextlib.ExitStack() as st:
            Win1 = sbt(st, "Win1", [128, 8, 2336], BF16)
            Wkpe = sbt(st, "Wkpe", [128, 8, 96], BF16)
            Wkper = sbt(st, "Wkper", [128, 8, 96], BF16)
            Wqb = sbt(st, "Wqb", [128, 4, 768], BF16)
            Wqbr = sbt(st, "Wqbr", [128, 4, 768], BF16)
            Wkn = sbt(st, "Wkn", [128, 2, 512], BF16)
            Wv = sbt(st, "Wv", [128, 2, 512], BF16)
            hb = [sbt(st, "hb%d" % i, [128, 8, TB], F32) for i in range(2)]
            hn2 = [sbt(st, "hn1", [128, 8, TB], BF16), sbt(st, "hn1b", [128, 8, TB], BF16)]
            hncur = [hn2[0]]
            sq = [sbt(st, "sq%d" % i, [128, TB], F32) for i in range(2)]
            tmpn = [sbt(st, "tmpn%d" % i, [128, TB], F32) for i in range(2)]
            rr = sbt(st, "rr", [128, TB], F32)
            scr = (sq, tmpn, rr)
            gbT = sbt(st, "gbT", [128, 4, TB], F32)
            gcT = sbt(st, "gcT", [128, 4, TB], F32)
            z = sbt(st, "z", [128, 4, TB + 2], F32)
            zh = sbt(st, "zh", [128, 4, NPOS, 2], F32)
            yc = [sbt(st, "yc%d" % i, [128, TB], F32) for i in range(2)]
            cTt = sbt(st, "cTt", [128, 4, TB], BF16)
            qlT = sbt(st, "qlT", [128, 4, TB], F32)
            qnT = sbt(st, "qnT", [128, 4, TB], BF16)
            kvlT = sbt(st, "kvlT", [128, 2, TB], F32)
            kvnT = sbt(st, "kvnT", [128, 2, TB], BF16)
            QTh = [sbt(st, "QTh%d" % i, [128, TB], BF16) for i in range(2)]
            rt = [sbt(st, "rt%d" % i, [128, TB], F32) for i in range(2)]
            knt = [sbt(st, "knt%d" % i, [128, TB], BF16) for i in range(2)]
            vt = sbt(st, "vt", [128, 4, 512], BF16)
            kpe = sbt(st, "kpe", [128, TB], BF16)
            rk = sbt(st, "rk", [128, 2, TB], F32)
            rq = sbt(st, "rq", [128, 2, TB], F32)

            DMA("pool", Win1[:], w_in1.rearrange("(c p) f -> p c f", p=128), [], [Win1])
            MEMSET("pool", Wkpe[:], 0.0, [Wkpe])
            MEMSET("pool", Wkper[:], 0.0, [Wkper])
            MEMSET("pool", Wqbr[:], 0.0, [Wqbr])
            CP("pool", Wkpe[:, :, 64:96], Win1[:, :, 2304:2336], [Win1], [Wkpe])
            TS("pool", Wkper[:, :, 64:80], Win1[:, :, 2320:2336], -1.0, ALU.mult, [Win1], [Wkper])
            CP("pool", Wkper[:, :, 80:96], Win1[:, :, 2304:2320], [Win1], [Wkper])
            DMA("pool", Wqb[:], w_qb.rearrange("(c p) f -> p c f", p=128), [], [Wqb])
            Wqb_v = Wqb[:].rearrange("p c (h e) -> p c h e", e=96)
            Wqbr_v = Wqbr[:].rearrange("p c (h e) -> p c h e", e=96)
            for c in range(4):
                TS("pool", Wqbr_v[:, c, :, 64:80], Wqb_v[:, c, :, 80:96], -1.0, ALU.mult, [Wqb], [Wqbr])
                CP("pool", Wqbr_v[:, c, :, 80:96], Wqb_v[:, c, :, 64:80], [Wqb], [Wqbr])
            kv_v = w_kvb.rearrange("(c p) (h t d) -> p c h t d", p=128, t=2, d=64)
            for c in range(2):
                DMA("pool", Wkn[:, c, :].rearrange("p (h d) -> p h d", d=64), kv_v[:, c, :, 0, :], [], [Wkn])
                DMA("pool", Wv[:, c, :].rearrange("p (h d) -> p h d", d=64), kv_v[:, c, :, 1, :], [], [Wv])
            nqh = [0]

            def proj_fm(col0, nchunk, dst_fn):
                for k in range(nchunk):
                    a = nacc()
                    for c in range(8):
                        MM(a[:], Win1[:, c, col0 + k * 128:col0 + (k + 1) * 128], hncur[0][:, c, :], c == 0, c == 7,
                           [Win1, hncur[0]], [a])
                    dst_fn(k, a)

            hnc1 = sbt(st, "hnc1", [128, 8, 32], BF16)
            gcc = sbt(st, "gcc", [128, 4, 32], F32)
            zh_v = zh[:].rearrange("p c n k -> p c (n k)")
            norm_mod(hcomp, 8, 32, float(D), gsc[:, 16:24], SH(1, 1), hnc1, scr)
            for k in range(4):
                a = nacc()
                for c in range(8):
                    MM(a[:, :32], Win1[:, c, 512 + k * 128:512 + (k + 1) * 128], hnc1[:, c, :], c == 0, c == 7, [Win1, hnc1], [a])
                EVAC(gcc[:, k, :], a[:, :32], [a], [gcc])
            for k in range(4):
                a = nacc()
                for c in range(8):
                    MM(a[:, :32], Win1[:, c, 1024 + k * 128:1024 + (k + 1) * 128], hnc1[:, c, :], c == 0, c == 7, [Win1, hnc1], [a])
                TT("dve", zh_v[:, k, :], gcc[:, k, :], a[:, :32], ALU.mult, [gcc, a], [zh])
            load_hT(hb[0], 0)
            norm_mod(hb[0], 8, TB, float(D), gsc[:, 16:24], SH(1, 1), hn2[0], scr)
            for pos in range(NPOS):
                blk = pos
                h = hb[pos % 2]
                hncur[0] = hn2[pos % 2]
                if pos + 1 < NPOS:
                    load_hT(hb[(pos + 1) % 2], pos + 1)
                DMA("sp", rk[64:96, :, :], ropek_d[:, :, pos * TB:(pos + 1) * TB].rearrange("a r n -> r a n"), [], [rk])
                if pos < NOWN:
                    DMA("sp", rq[64:96, :, :], ropeq_d[:, :, pos * TB:(pos + 1) * TB].rearrange("a r n -> r a n"), [], [rq])
                proj_fm(512, 4, lambda k, a: EVAC(gcT[:, k, :], a[:], [a], [gcT]))
                proj_fm(1024, 4, lambda k, a: TT("dve", z[:, k, 2:TB + 2], gcT[:, k, :], a[:], ALU.mult, [gcT, a], [z]))
                TS("pool", z[:, :, 0:2], zh[:, :, pos, :], flcv[:, pos:pos + 1], ALU.mult, [zh, flcv], [z])
                proj_fm(0, 4, lambda k, a: EVAC(gbT[:, k, :], a[:], [a], [gbT]))
                for cc in range(4):
                    y_ = yc[cc % 2]
                    TS("dve", y_[:], z[:, cc, 0:TB], vec[:, V_CW + cc:V_CW + cc + 1], ALU.mult, [z, vec], [y_])
                    STT("dve", y_[:], z[:, cc, 1:TB + 1], vec[:, V_CW + 4 + cc:V_CW + 5 + cc], y_[:], ALU.mult, ALU.add,
                        [z, vec, y_], [y_])
                    STT("dve", y_[:], z[:, cc, 2:TB + 2], vec[:, V_CW + 8 + cc:V_CW + 9 + cc], y_[:], ALU.mult, ALU.add,
                        [z, vec, y_], [y_])
                    TT("pool", cTt[:, cc, :], gbT[:, cc, :], y_[:], ALU.mult, [gbT, y_], [cTt])
                DMA("sp", cT_d[:, pos * TB:(pos + 1) * TB].rearrange("(c p) n -> p c n", p=128), cTt[:], [cTt], [b_cT[pos]])
                if pos + 1 < NPOS:
                    norm_mod(hb[(pos + 1) % 2], 8, TB, float(D), gsc[:, 16:24], SH(1, 1), hn2[(pos + 1) % 2], scr)
                if pos < NOWN:
                    proj_fm(1536, 4, lambda k, a: EVAC(qlT[:, k, :], a[:], [a], [qlT]))
                    norm_mod(qlT, 4, TB, 512.0, vec[:, V_QG:V_QG + 4], None, qnT, scr)
                    for hh in range(8):
                        aa, ab = pb[2 + 2 * (hh % 2)], pb[3 + 2 * (hh % 2)]
                        for c in range(4):
                            MM(aa[0:96, :], Wqb[:, c, hh * 96:(hh + 1) * 96], qnT[:, c, :], c == 0, c == 3, [Wqb, qnT], [aa])
                        for c in range(4):
                            MM(ab[0:96, :], Wqbr[:, c, hh * 96:(hh + 1) * 96], qnT[:, c, :], c == 0, c == 3, [Wqbr, qnT], [ab])
                        qt = QTh[nqh[0] % 2]
                        r1, r2 = rt[0], rt[1]
                        nqh[0] += 1
                        ACT(qt[0:64, :], aa[0:64, :], AF.Copy, [aa], [qt], scale=float(96 ** -0.5))
                        TT("dve", r1[64:96, :], aa[64:96, :], rq[64:96, 0, :], ALU.mult, [aa, rq], [r1])
                        TT("dve", r2[64:96, :], ab[64:96, :], rq[64:96, 1, :], ALU.mult, [ab, rq], [r2])
                        TT("pool", qt[64:96, :], r1[64:96, :], r2[64:96, :], ALU.add, [r1, r2], [qt])
                        DMA("sp", qT_d[hh, :, pos * TB:(pos + 1) * TB], qt[0:96, :], [qt], [b_q])
                proj_fm(2048, 2, lambda k, a: EVAC(kvlT[:, k, :], a[:], [a], [kvlT]))
                norm_mod(kvlT, 2, TB, 256.0, vec[:, V_KG:V_KG + 2], None, kvnT, scr)
                for hc in range(4):
                    a = nacc()
                    for c in range(2):
                        MM(a[:], Wkn[:, c, hc * 128:(hc + 1) * 128], kvnT[:, c, :], c == 0, c == 1, [Wkn, kvnT], [a])
                    kt_ = knt[hc % 2]
                    EVAC(kt_[:], a[:], [a], [kt_])
                    DMA("sp", knT_d[hc * 128:(hc + 1) * 128, pos * TB:(pos + 1) * TB], kt_[:], [kt_], [b_kn])
                for s in range(4):
                    a = nacc()
                    for c in range(2):
                        MM(a[:], kvnT[:, c, s * 128:(s + 1) * 128], Wv[:, c, :], c == 0, c == 1, [Wv, kvnT], [a])
                    EVAC(vt[:, s, :], a[:], [a], [vt])
                DMA("sp", v_d[pos * TB:(pos + 1) * TB, :].rearrange("(s p) f -> p s f", p=128), vt[:], [vt], [b_v])
                aa, ab = pb[2], pb[3]
                for c in range(8):
                    MM(aa[0:96, :], Wkpe[:, c, :], hncur[0][:, c, :], c == 0, c == 7, [Wkpe, hncur[0]], [aa])
                for c in range(8):
                    MM(ab[0:96, :], Wkper[:, c, :], hncur[0][:, c, :], c == 0, c == 7, [Wkper, hncur[0]], [ab])
                r1, r2 = rt[0], rt[1]
                TT("dve", r1[64:96, :], aa[64:96, :], rk[64:96, 0, :], ALU.mult, [aa, rk], [r1])
                TT("dve", r2[64:96, :], ab[64:96, :], rk[64:96, 1, :], ALU.mult, [ab, rk], [r2])
                TT("pool", kpe[64:96, :], r1[64:96, :], r2[64:96, :], ALU.add, [r1, r2], [kpe])
                DMA("sp", kpeT_d[:, pos * TB:(pos + 1) * TB], kpe[64:96, :], [kpe], [b_kpe])
        p.barrier()

        with contextlib.ExitStack() as st:
            KT = [sbt(st, "KT%d" % i, [128, S], BF16) for i in range(2)]
            VA = [sbt(st, "VA%d" % i, [128, 64, 128], BF16) for i in range(2)]
            QA = [sbt(st, "QA%d" % i, [128, NOWN * TB], BF16) for i in range(2)]
            PTm = [sbt(st, "PTm%d" % i, [128, TB], BF16) for i in range(4)]
            msk = sbt(st, "msk", [128, 4, TB], BF16)
            ODs = [sbt(st, "ODs%d" % i, [128, TB], F32) for i in range(2)]
            rden = [sbt(st, "rden%d" % i, [128, TB], F32) for i in range(2)]
            dout = [sbt(st, "dout%d" % i, [128, TB], BF16) for i in range(2)]
            SEL = [sbt(st, "SEL%d" % i, [128, 128], F32) for i in range(2)]
            DMA("pool", msk[:], mlam_d.rearrange("p (a b) -> p a b", a=4), [], [msk])
            flob = sbt(st, "flob", [128, NOWN], F32)
            TS("dve", flob[:], flot[:], -NEG, ALU.mult, [flot], [flob], s2=NEG, op1=ALU.add)
            for i_, off in ((0, -64), (1, 64)):
                MEMSET("pool", SEL[i_][:], 0.0, [SEL[i_]])
                p.op("pool", lambda e, t=SEL[i_], off=off: e.affine_select(
                    out=t[:], in_=t[:], pattern=[[-1, 128]], compare_op=ALU.not_equal, fill=1.0, base=off,
                    channel_multiplier=1), (), [SEL[i_].b])
            MEMSET("pool", VA[0][:, :, 64:128], 1.0, [VA[0]])
            MEMSET("pool", VA[1][:, :, 0:64], 1.0, [VA[1]])
            def b2_load(hh):
                par = hh % 2
                kt, va, qa = KT[par], VA[par], QA[par]
                voff = 0 if par == 0 else 64
                DMA("sp", kt[0:64, :], knT_d[hh * 64:(hh + 1) * 64, :], [b_kn], [kt])
                DMA("sp", kt[64:96, :], kpeT_d[:, :], [b_kpe], [kt])
                for q4 in range(4):
                    DMA("sp", va[:, q4 * 16:(q4 + 1) * 16, voff:voff + 64],
                        v_d[q4 * 2048:(q4 + 1) * 2048, hh * 64:(hh + 1) * 64].rearrange("(kb p) d -> p kb d", p=128),
                        [b_v], [va])
                DMA("sp", qa[0:96, :], qT_d[hh, :, :], [b_q], [qa])

            items = []
            for hh in range(8):
                for i in range(NOWN):
                    klist = []
                    for i2 in range(i):
                        for sub in range(4):
                            klist.append((i2, sub, None))
                            klist.append((8 + i2, sub, None))
                    for sub in range(4):
                        klist.append((8 + i, sub, "oth"))
                    for sub in range(4):
                        klist.append((i, sub, "diag"))
                    for n, (pos, sub, kind) in enumerate(klist):
                        items.append((hh, i, n, len(klist), pos, sub, kind))
            sb3 = [pb[1], pb[2], pb[3]]

            def emit_S(t):
                hh, i, n, nk, pos, sub, kind = items[t]
                par = hh % 2
                kt, qa = KT[par], QA[par]
                kc = pos * TB + sub * 128
                sbk = sb3[t % 3]
                pt = PTm[t % 4]
                MM(sbk[:], kt[0:96, kc:kc + 128], qa[0:96, i * TB:(i + 1) * TB], True, True, [kt, qa], [sbk])
                if kind == "oth":
                    ACT(pt[:], sbk[:], AF.Exp, [sbk, flob], [pt], bias=flob[:, i:i + 1], scale=1.0)
                else:
                    ACT(pt[:], sbk[:], AF.Exp, [sbk], [pt])
                if kind == "diag":
                    TT("dve", pt[:], pt[:], msk[:, sub, :], ALU.mult, [pt, msk], [pt])

            b2_load(0)
            SKEW = 2
            for t in range(min(SKEW, len(items))):
                emit_S(t)
            nod = 0
            for t in range(len(items)):
                hh, i, n, nk, pos, sub, kind = items[t]
                par = hh % 2
                va = VA[par]
                if i == 0 and n == 0 and hh + 1 < 8:
                    b2_load(hh + 1)
                if t + SKEW < len(items):
                    emit_S(t + SKEW)
                od = pb[4 + nod % 2]
                MM(od[:], va[:, pos * 4 + sub, :], PTm[t % 4][:], n == 0, n == nk - 1, [va, PTm[t % 4]], [od])
                if n == nk - 1:
                    os_ = ODs[nod % 2]
                    rd = rden[nod % 2]
                    do = dout[nod % 2]
                    nod += 1
                    CP("act", os_[:], od[:], [od], [os_])
                    db = pb[6]
                    MM(db[:], SEL[par][:], os_[:], True, True, [SEL[par], os_], [db])
                    r0 = par * 64
                    RCP(rd[r0:r0 + 64, :], db[r0:r0 + 64, :], [db], [rd])
                    TT("dve", do[r0:r0 + 64, :], os_[r0:r0 + 64, :], rd[r0:r0 + 64, :], ALU.mult, [os_, rd], [do])
                    DMA("sp", dT_d[hh * 64:(hh + 1) * 64, i * TB:(i + 1) * TB], do[r0:r0 + 64, :], [do], [b_dT[i]])
        p.barrier()

        Mall = sbt(top, "Mall", [128, 32, 8], F32)
        Gall = sbt(top, "Gall", [128, 32, 8], F32)
        with contextlib.ExitStack() as st:
            Wo1 = sbt(st, "Wo1", [128, 8, D], BF16)
            wr = sbt(st, "wr", [128, 8, 8], F32)
            brb = sbt(st, "brb", [128, 8], F32)
            mix = [sbt(st, "mix%d" % i, [128, 8, TB], BF16) for i in range(2)]
            hb = [sbt(st, "hb%d" % i, [128, 8, TB], F32) for i in range(2)]
            hnf = sbt(st, "hnf", [128, 8, TB], F32)
            sq = [sbt(st, "sq%d" % i, [128, TB], F32) for i in range(2)]
            tmpn = [sbt(st, "tmpn%d" % i, [128, TB], F32) for i in range(2)]
            rr = sbt(st, "rr", [128, TB], F32)
            lg = [sbt(st, "lg%d" % i, [128, 8], F32) for i in range(2)]
            e8 = [sbt(st, "e8_%d" % i, [128, 8], F32) for i in range(2)]
            s4 = [sbt(st, "s4_%d" % i, [128, 4], F32) for i in range(2)]
            tk = [sbt(st, "tk%d" % i, [128, D], F32) for i in range(4)]
            ntk = [0]
            DMA("pool", Wo1[:], w_o1.rearrange("(c p) m -> p c m", p=128), [], [Wo1])
            DMA("sp", wr[:], w_rt.rearrange("(c p) e -> p c e", p=128), [], [wr])
            DMA("sp", brb[:], brt_d.partition_broadcast(128), [], [brb])
            DMA("sp", g2row_d.rearrange("(c p) -> p c", p=128), modT[1][:, 40:48], [modT[1]], [b_g2row],
                allow_slow_non_contiguous=True)

            def to_rows(src, dst_d, bufs, i):
                for s in range(4):
                    t_ = tk[ntk[0] % 4]
                    ntk[0] += 1
                    for half in range(2):
                        a = nacc()
                        for c4 in range(4):
                            TR(a[:, c4 * 128:(c4 + 1) * 128], src[:, half * 4 + c4, s * 128:(s + 1) * 128], ident[:],
                               [src, ident], [a])
                        EVAC(t_[:, half * 512:(half + 1) * 512], a[:], [a], [t_])
                    ch = i * 4 + s
                    DMA("sp", dst_d[ch * 128:(ch + 1) * 128, :], t_[:], [t_], [bufs[ch]])

            def b3a_fetch(i):
                m_ = mix[i % 2]
                DMA("sp", m_[:, 0:4, :], cT_d[:, i * TB:(i + 1) * TB].rearrange("(c p) n -> p c n", p=128), [b_cT[i]], [m_])
                DMA("sp", m_[:, 4:8, :], dT_d[:, i * TB:(i + 1) * TB].rearrange("(c p) n -> p c n", p=128), [b_dT[i]], [m_])
                load_hT(hb[i % 2], i)

            b3a_fetch(0)
            for i in range(NOWN):
                m_ = mix[i % 2]
                h = hb[i % 2]
                if i + 1 < NOWN:
                    b3a_fetch(i + 1)
                for dc in range(8):
                    a = nacc()
                    for mc in range(8):
                        MM(a[:], Wo1[:, mc, dc * 128:(dc + 1) * 128], m_[:, mc, :], mc == 0, mc == 7, [Wo1, m_], [a])
                    STT("dve", h[:, dc, :], a[:], GATE(1, 1)[:, dc:dc + 1], h[:, dc, :], ALU.mult, ALU.add,
                        [a, modT[1], h], [h])
                to_rows(h, h3tok_d, b_h3t, i)
                norm_mod(h, 8, TB, float(D), gsc[:, 24:32], SH(1, 2), hnf, (sq, tmpn, rr))
                to_rows(hnf, hn3tok_d, b_hn3t, i)
                for s in range(4):
                    ch = i * 4 + s
                    a = nacc()
                    for c in range(8):
                        MM(a[:, 0:8], hnf[:, c, s * 128:(s + 1) * 128], wr[:, c, :], c == 0, c == 7, [hnf, wr], [a])
                    L, E8, S4 = lg[s % 2], e8[s % 2], s4[s % 2]
                    W8 = Mall[:, ch, :]
                    TT("dve", L[:], a[:, 0:8], brb[:], ALU.add, [a, brb], [L])
                    RED(S4[:, 0:1], L[:], ALU.max, [L], [S4])
                    TS("dve", W8, L[:], S4[:, 0:1], ALU.is_equal, [L, S4], [Mall])
                    STT("dve", W8, W8, -1e30, L[:], ALU.mult, ALU.add, [Mall, L], [Mall])
                    RED(S4[:, 1:2], W8, ALU.max, [Mall], [S4])
                    TS("dve", W8, L[:], S4[:, 1:2], ALU.is_ge, [L, S4], [Mall])
                    TS("dve", S4[:, 2:3], S4[:, 0:1], -1.0, ALU.mult, [S4], [S4])
                    ACT(E8[:], L[:], AF.Exp, [L, S4], [E8], bias=S4[:, 2:3], scale=1.0)
                    TT("dve", E8[:], E8[:], W8, ALU.mult, [E8, Mall], [E8])
                    RED(S4[:, 3:4], E8[:], ALU.add, [E8], [S4])
                    RCP(S4[:, 3:4], S4[:, 3:4], [S4], [S4])
                    TS("dve", Gall[:, ch, :], E8[:], S4[:, 3:4], ALU.mult, [E8, S4], [Gall])
        p.barrier()

        I32 = mybir.dt.int32
        idx_hi = sbt(top, "idx_hi", [128, 32], I32)
        idx_lo = sbt(top, "idx_lo", [128, 32], I32)
        g_hi = sbt(top, "g_hi", [128, 32], F32)
        g_lo = sbt(top, "g_lo", [128, 32], F32)
        widx = sbt(top, "widx", [128, NBLK, 7], I32)
        with contextlib.ExitStack() as st:
            cst = sbt(st, "cst", [128, 32], F32)
            Uex = sbt(st, "Uex", [128, 128], F32)
            tot = sbt(st, "tot", [128, 32, 8], F32)
            offs = sbt(st, "offs", [128, 32, 8], F32)
            slot = sbt(st, "slot", [128, 32, 8], F32)
            vidx = sbt(st, "vidx", [128, 32, 8], F32)
            t256 = sbt(st, "t256", [128, 32, 8], F32)
            eqh = sbt(st, "eqh", [128, 32, 8], F32)
            ne = sbt(st, "ne", [128, 8], F32)
            nb8 = sbt(st, "nb8", [128, 8], F32)
            c8 = sbt(st, "c8", [128, 8], F32)
            pend = sbt(st, "pend", [128, 8], F32)
            pstart = sbt(st, "pstart", [128, 8], F32)
            hi_f = sbt(st, "hi_f", [128, 32], F32)
            lo_f = sbt(st, "lo_f", [128, 32], F32)
            gs = sbt(st, "gs", [128, 32], F32)
            be = sbt(st, "be", [128, NBLK], F32)
            t24 = sbt(st, "t24", [128, NBLK], F32)
            wf = sbt(st, "wf", [128, NBLK, 7], F32)
            DMA("sp", cst[:], cst_d, [], [cst])
            MEMSET("pool", Uex[:], 1.0, [Uex])
            p.op("pool", lambda e: e.affine_select(out=Uex[:], in_=Uex[:], pattern=[[1, 128]],
                                                   compare_op=ALU.is_gt, fill=0.0, base=0, channel_multiplier=-1),
                 (), [Uex.b])
            Mf = Mall[:].rearrange("p a b -> p (a b)")
            MM(pb[2][:, 0:256], Uex[:], Mf, True, True, [Uex, Mall], [pb[2]])
            MM(pb[3][:, 0:256], ones_f[:], Mf, True, True, [ones_f, Mall], [pb[3]])
            CP("dve", tot[:].rearrange("p a b -> p (a b)"), pb[3][:, 0:256], [pb[3]], [tot])
            MEMSET("pool", offs[:, 0, :], 0.0, [offs])
            for ch in range(1, 32):
                TT("dve", offs[:, ch, :], offs[:, ch - 1, :], tot[:, ch - 1, :], ALU.add, [offs, tot], [offs])
            TT("dve", ne[:], offs[:, 31, :], tot[:, 31, :], ALU.add, [offs, tot], [ne])
            TS("dve", nb8[:], ne[:], 0.0, ALU.is_gt, [ne], [nb8])
            for k in range(1, 8):
                TS("dve", c8[:], ne[:], 512.0 * k, ALU.is_gt, [ne], [c8])
                TT("dve", nb8[:], nb8[:], c8[:], ALU.add, [nb8, c8], [nb8])
            CP("dve", pend[:, 0:1], nb8[:, 0:1], [nb8], [pend])
            for e in range(1, 8):
                TT("dve", pend[:, e:e + 1], pend[:, e - 1:e], nb8[:, e:e + 1], ALU.add, [pend, nb8], [pend])
            TT("dve", pstart[:], pend[:], nb8[:], ALU.subtract, [pend, nb8], [pstart])
            TS("dve", pstart[:], pstart[:], 512.0, ALU.mult, [pstart], [pstart])
            TT("dve", slot[:].rearrange("p a b -> p (a b)"), pb[2][:, 0:256], offs[:].rearrange("p a b -> p (a b)"),
               ALU.add, [pb[2], offs], [slot])
            for e in range(8):
                TS("dve", slot[:, :, e], slot[:, :, e], pstart[:, e:e + 1], ALU.add, [slot, pstart], [slot])
            TT("dve", vidx[:], slot[:], Mall[:], ALU.mult, [slot, Mall], [vidx])
            RED(hi_f[:], vidx[:], ALU.max, [vidx], [hi_f])
            BIG = 1.0e6
            TS("dve", t256[:], Mall[:], -BIG, ALU.mult, [Mall], [t256], s2=BIG, op1=ALU.add)
            TT("dve", t256[:], t256[:], slot[:], ALU.add, [t256, slot], [t256])
            RED(lo_f[:], t256[:], ALU.min, [t256], [lo_f])
            TS("dve", lo_f[:], lo_f[:], float(LSLOT - 1), ALU.min, [lo_f], [lo_f])
            for e in range(8):
                TT("dve", eqh[:, :, e], vidx[:, :, e], hi_f[:], ALU.is_equal, [vidx, hi_f], [eqh])
            TT("dve", eqh[:], eqh[:], Gall[:], ALU.mult, [eqh, Gall], [eqh])
            RED(g_hi[:], eqh[:], ALU.add, [eqh], [g_hi])
            RED(gs[:], Gall[:], ALU.add, [Gall], [gs])
            TT("dve", g_lo[:], gs[:], g_hi[:], ALU.subtract, [gs, g_hi], [g_lo])
            CP("dve", idx_hi[:], hi_f[:], [hi_f], [idx_hi])
            CP("dve", idx_lo[:], lo_f[:], [lo_f], [idx_lo])
            MEMSET("pool", be[:], 0.0, [be])
            for e in range(8):
                TS("dve", t24[:], cst[:, 0:NBLK], pend[:, e:e + 1], ALU.is_ge, [cst, pend], [t24])
                TT("dve", be[:], be[:], t24[:], ALU.add, [be, t24], [be])
            TS("dve", be[:], be[:], 7.0, ALU.min, [be], [be])
            TS("dve", be[:], be[:], 896.0, ALU.mult, [be], [be], s2=cst[:, 24:25], op1=ALU.add)
            for fg in range(7):
                TS("dve", wf[:, :, fg], be[:], 128.0 * fg, ALU.add, [be], [wf])
            CP("dve", widx[:], wf[:], [wf], [widx])
            xr = [sbt(st, "xr%d" % i, [128, D], F32) for i in range(3)]
            for ch in range(32):
                x_ = xr[ch % 3]
                DMA("sp", x_[:], hn3tok_d[ch * 128:(ch + 1) * 128, :], [b_hn3t[ch]], [x_])
                for it in (idx_hi, idx_lo):
                    p.idma(lambda e, x_=x_, it=it, ch=ch: e.indirect_dma_start(
                        out=xs_h[:, :], out_offset=bass.IndirectOffsetOnAxis(ap=it[:, ch:ch + 1], axis=0),
                        in_=x_[:, :], in_offset=None),
                        [x_.b, it.b], [b_xs])
        p.barrier()

        with contextlib.ExitStack() as st:
            WG = [sbt(st, "WG%d" % i, [128, 8, 512], BF16) for i in range(2)]
            WU = [sbt(st, "WU%d" % i, [128, 8, 512], BF16) for i in range(2)]
            WD = [sbt(st, "WD%d" % i, [128, 4, D], BF16) for i in range(2)]
            XT = [sbt(st, "XT%d" % i, [128, 8, TB], BF16) for i in range(2)]
            Yb = [sbt(st, "Yb%d" % i, [128, 4, D], F32) for i in range(2)]
            xrow = [sbt(st, "xrow%d" % i, [128, D], F32) for i in range(8)]
            actm = [sbt(st, "actm%d" % i, [128, 4, TB], BF16) for i in range(2)]
            sg = [sbt(st, "sgm%d" % i, [128, TB], F32) for i in range(2)]
            nsg = [0]
            nxr = [0]

            def w_load(n):
                b_, fg_ = n // 7, n % 7
                k_ = n % 2
                for (tiles, srcs, v) in ((WG, ew_bf["g"], "g"), (WU, ew_bf["u"], "u"), (WD, ew_bf["d"], "d")):
                    t_ = tiles[k_]
                    for h in range(2):
                        if v == "d":
                            o_ = t_[:, 2 * h:2 * h + 2, :].rearrange("p a b -> p (a b)")
                        else:
                            o_ = t_[:, 4 * h:4 * h + 4, :].rearrange("p a b -> p (a b)")
                        p.idma(lambda e, o_=o_, src=srcs[h], b_=b_, fg_=fg_: e.indirect_dma_start(
                            out=o_, out_offset=None, in_=src[:, :],
                            in_offset=bass.IndirectOffsetOnAxis(ap=widx[:, b_, fg_:fg_ + 1], axis=0)),
                            [widx.b] + b_cv, [t_.b])

            def x_issue(b_):
                for s in range(4):
                    xr_ = xrow[(b_ % 2) * 4 + s]
                    DMA("sp", xr_[:], xs_d[b_ * TB + s * 128:b_ * TB + (s + 1) * 128, :], [b_xs], [xr_])

            def x_trans(b_):
                xt_ = XT[b_ % 2]
                for s in range(4):
                    xr_ = xrow[(b_ % 2) * 4 + s]
                    for half in range(2):
                        a = nacc()
                        for c4 in range(4):
                            TR(a[:, c4 * 128:(c4 + 1) * 128], xr_[:, (half * 4 + c4) * 128:(half * 4 + c4 + 1) * 128], ident[:],
                               [xr_, ident], [a])
                        EVAC(xt_[:, half * 4:(half + 1) * 4, s * 128:(s + 1) * 128],
                             a[:].rearrange("p (a b) -> p a b", a=4), [a], [xt_])

            w_load(0)
            x_issue(0)
            x_trans(0)
            for b_ in range(NBLK):
                xt_ = XT[b_ % 2]
                yb = Yb[b_ % 2]
                if b_ + 1 < NBLK:
                    x_issue(b_ + 1)
                for fg in range(7):
                    n_ = b_ * 7 + fg
                    if fg == 3 and b_ + 1 < NBLK:
                        x_trans(b_ + 1)
                    k_ = n_ % 2
                    wgt, wut, wdt = WG[k_], WU[k_], WD[k_]
                    if n_ + 1 < NBLK * 7:
                        w_load(n_ + 1)
                    am = actm[n_ % 2]
                    for f4 in range(4):
                        ag, au = pb[2 + 2 * (f4 % 2)], pb[3 + 2 * (f4 % 2)]
                        for c in range(8):
                            MM(ag[:], wgt[:, c, f4 * 128:(f4 + 1) * 128], xt_[:, c, :], c == 0, c == 7, [wgt, xt_], [ag])
                        for c in range(8):
                            MM(au[:], wut[:, c, f4 * 128:(f4 + 1) * 128], xt_[:, c, :], c == 0, c == 7, [wut, xt_], [au])
                        s_ = sg[nsg[0] % 2]
                        nsg[0] += 1
                        ACT(s_[:], ag[:], AF.Silu, [ag], [s_])
                        TT("dve", am[:, f4, :], s_[:], au[:], ALU.mult, [s_, au], [am])
                    for s4_ in range(4):
                        for half in range(2):
                            a = nacc()
                            for f4 in range(4):
                                MM(a[:], am[:, f4, s4_ * 128:(s4_ + 1) * 128], wdt[:, f4, half * 512:(half + 1) * 512],
                                   f4 == 0, f4 == 3, [wdt, am], [a])
                            dst = yb[:, s4_, half * 512:(half + 1) * 512]
                            if fg == 0:
                                EVAC(dst, a[:], [a], [yb])
                            else:
                                TT("dve", dst, dst, a[:], ALU.add, [yb, a], [yb])
                DMA("sp", ys_d[b_ * TB:(b_ + 1) * TB, :].rearrange("(s p) m -> p s m", p=128), yb[:], [yb], [b_ys[b_]])
        p.barrier()

        fin = []
        with contextlib.ExitStack() as st:
            g2b = sbt(st, "g2b", [128, D], F32)
            fgb = sbt(st, "fgb", [128, D], F32)
            Yh = [sbt(st, "Yh%d" % i, [128, D], F32) for i in range(2)]
            Yl = [sbt(st, "Yl%d" % i, [128, D], F32) for i in range(2)]
            h3r = [sbt(st, "h3r%d" % i, [128, D], F32) for i in range(2)]
            ot = [sbt(st, "ot%d" % i, [128, D], F32) for i in range(2)]
            junk = sbt(st, "junk", [128, D], F32)
            ss = [sbt(st, "ss%d" % i, [128, 2], F32) for i in range(2)]
            DMA("sp", g2b[:], g2row_d.rearrange("(o n) -> o n", o=1).partition_broadcast(128), [b_g2row], [g2b])
            DMA("sp", fgb[:], fgrow_d.partition_broadcast(128), [], [fgb])
            def cmb_fetch(ch):
                for (yt, it) in ((Yh[ch % 2], idx_hi), (Yl[ch % 2], idx_lo)):
                    p.idma(lambda e, yt=yt, it=it, ch=ch: e.indirect_dma_start(
                        out=yt[:, :], out_offset=None, in_=ys_h[:, :],
                        in_offset=bass.IndirectOffsetOnAxis(ap=it[:, ch:ch + 1], axis=0)),
                        [it.b] + b_ys, [yt.b])
                DMA("sp", h3r[ch % 2][:], h3tok_d[ch * 128:(ch + 1) * 128, :], [b_h3t[ch]], [h3r[ch % 2]])

            cmb_fetch(0)
            for ch in range(32):
                yh, yl, hr, o, s2 = Yh[ch % 2], Yl[ch % 2], h3r[ch % 2], ot[ch % 2], ss[ch % 2]
                if ch + 1 < 32:
                    cmb_fetch(ch + 1)
                ACT(yl[:], yl[:], AF.Copy, [yl, g_lo], [yl], scale=g_lo[:, ch:ch + 1])
                STT("dve", yh[:], yh[:], g_hi[:, ch:ch + 1], yl[:], ALU.mult, ALU.add, [yh, g_hi, yl], [yh])
                TT("dve", yh[:], yh[:], g2b[:], ALU.mult, [yh, g2b], [yh])
                TT("dve", hr[:], hr[:], yh[:], ALU.add, [hr, yh], [hr])
                MEMSET("pool", s2[:], 0.0, [s2])
                ACT(junk[:], hr[:], AF.Square, [hr], [junk, s2], accum_out=s2[:, 0:1])
                ACT(s2[:, 1:2], s2[:, 0:1], AF.Sqrt, [s2], [s2], bias=EPS_AP[:, 0:1], scale=1.0 / D)
                RCP(s2[:, 1:2], s2[:, 1:2], [s2], [s2])
                STT("dve", o[:], hr[:], s2[:, 1:2], fgb[:], ALU.mult, ALU.mult, [hr, s2, fgb], [o])
                fin.append(DMA("sp", y_out[ch * 128:(ch + 1) * 128, :], o[:], [o], []))
        for t in fin:
            p.wait_tok("sp", t)
        for t in list(p.last_dma.values()):
            p.wait_tok("sp", t)
        p.emit()
    return nc


def _pc(v, k):
    return np.ascontiguousarray(np.asarray(v, np.float32).reshape(k, 128).T)


def _host_inputs(inp, dbg_cores=None):
    f = lambda k: np.asarray(inp[k], np.float32)
    x = f("x")
    B = x.shape[0]
    slopes = (2.0 ** (-8.0 * np.arange(1, 9, dtype=np.float32) / 8)).astype(np.float32)
    k_i = np.arange(128)[:, None]
    q_i = np.arange(128)[None, :]
    swab = np.zeros((128, 2, 2, 4, 128), np.float32)
    for g in range(2):
        for j in range(4):
            sl = slopes[g * 4 + j]
            dist_prev = q_i + 128 - k_i
            dist_cur = q_i - k_i
            bp = np.where((dist_prev >= 0) & (dist_prev < 128), -sl * dist_prev, NEG)
            bc = np.where((dist_cur >= 0) & (dist_cur < 128), -sl * dist_cur, NEG)
            swab[:, 0, g, j, :] = bp
            swab[:, 1, g, j, :] = bc
    swab = swab.reshape(128, 2 * 8 * 128)
    mlam = np.zeros((128, 4, TB), np.float32)
    for d in range(4):
        mlam[:, d, :] = ((d * 128 + np.arange(128))[:, None] <= np.arange(TB)[None, :]).astype(np.float32)
    mlam = mlam.reshape(128, 4 * TB)
    inv = (10000.0 ** (-np.arange(0, 32, 2, dtype=np.float32) / 32)).astype(np.float32)
    ang = np.arange(S, dtype=np.float32)[:, None] * inv[None, :]
    cosf = np.cos(ang).astype(np.float32).T
    sinf = np.sin(ang).astype(np.float32).T
    cos32 = np.concatenate([cosf, cosf], 0)
    sin32 = np.concatenate([sinf, sinf], 0)
    qs = np.float32(96 ** -0.5)

    sinks = f("even_sinks")[0]
    esk = np.zeros((128, 4), np.float32)
    for g in range(2):
        for j in range(4):
            esk[g * 64:(g + 1) * 64, j] = sinks[g * 4 + j]
    bs = f("even_b_s")[0]
    bsb = np.zeros((128, 4, 128), np.float32)
    for pr in range(4):
        bsb[0:64, pr, :] = bs[2 * pr][None, :]
        bsb[64:128, pr, :] = bs[2 * pr + 1][None, :]
    bsb = bsb.reshape(128, 512)
    vecs_common = np.zeros((128, 160), np.float32)
    vecs_common[:, 0:8] = _pc(f("even_norm_mix_g")[0], 8)
    vecs_common[:, 8:16] = _pc(f("even_norm_ffn_g")[0], 8)
    vecs_common[:, 16:24] = _pc(f("odd_norm_mix_g")[0], 8)
    vecs_common[:, 24:32] = _pc(f("odd_norm_ffn_g")[0], 8)
    vecs_common[:, 32:40] = _pc(f("final_norm_g"), 8)
    vecs_common[:, 40:88] = _pc(f("even_ada_b")[0], 48)
    vecs_common[:, 88:136] = _pc(f("odd_ada_b")[0], 48)
    vecs_common[:, 136:140] = _pc(f("odd_q_norm_g")[0], 4)
    vecs_common[:, 140:142] = _pc(f("odd_kv_norm_g")[0], 2)
    cw = f("odd_conv_w")[0]
    for j in range(3):
        vecs_common[:, 142 + j * 4:142 + (j + 1) * 4] = _pc(cw[j], 4)

    shared = {
        "vecs": vecs_common, "swab": swab, "mlam": mlam, "esk_sink": esk,
        "lng": f("even_gmlp_ln_g")[0][None, :].copy(), "lnb": f("even_gmlp_ln_b")[0][None, :].copy(),
        "bsb": bsb, "brt": f("odd_router_b")[0][None, :].copy(),
        "ada_w0": f("even_ada_w")[0], "ada_w1": f("odd_ada_w")[0],
        "w_in0": f("even_w_in")[0], "w_s": f("even_w_s")[0], "w_o0": f("even_w_o")[0],
        "wg0": f("even_ffn_w_gate")[0], "wu0": f("even_ffn_w_up")[0], "wd0": f("even_ffn_w_down")[0],
        "w_in1": f("odd_w_in")[0], "w_qb": f("odd_w_q_b")[0], "w_kvb": f("odd_w_kv_b")[0], "w_o1": f("odd_w_o")[0],
        "w_rt": f("odd_router_w")[0], "fgrow": f("final_norm_g")[None, :].copy(),
    }
    cst = np.zeros((128, 32), np.float32)
    cst[:, 0:24] = np.arange(24, dtype=np.float32)[None, :]
    cst[:, 24] = np.arange(128, dtype=np.float32)
    shared["cst"] = cst
    for nm, key in (("ewg", "odd_exp_w_gate"), ("ewu", "odd_exp_w_up")):
        w = f(key)[0].reshape(NEXP, 2, 4, 128, 7, 512)
        w = w.transpose(1, 0, 4, 3, 2, 5)
        for h in range(2):
            shared["%s_h%d" % (nm, h)] = w[h].reshape(NEXP * 7 * 128, 2048)
    w = f("odd_exp_w_down")[0].reshape(NEXP, 7, 2, 2, 128, D)
    w = w.transpose(2, 0, 1, 4, 3, 5)
    for h in range(2):
        shared["ewd_h%d" % h] = w[h].reshape(NEXP * 7 * 128, 2048)
    shared = {k: np.ascontiguousarray(v, dtype=np.float32) for k, v in shared.items()}
    cvec = f("c")
    maps = []
    orders = []
    cores = range(2 * B) if dbg_cores is None else dbg_cores
    for core in cores:
        b, hf = core // 2, core % 2
        order = list(OWN[hf]) + list(OWN[1 - hf])
        orders.append((b, order))
        xb = x[b]
        xpad = np.concatenate([np.zeros((256, D), np.float32), xb], 0)
        xm_ = np.concatenate([xb[j * TB:(j + 1) * TB] for j in order], 0)
        xh_ = np.concatenate([xpad[j * TB + 128:j * TB + 256] for j in order], 0)
        xh2_ = np.concatenate([xpad[j * TB:j * TB + 128] for j in order], 0)
        fl_swa = np.zeros((128, NPOS), np.float32)
        fl_conv = np.ones((128, NPOS), np.float32)
        for pos, j in enumerate(order):
            if j == 0:
                fl_swa[:, pos] = NEG
                fl_conv[:, pos] = 0.0
        fl_oth = np.zeros((128, NOWN), np.float32)
        for i in range(NOWN):
            fl_oth[:, i] = 1.0 if order[8 + i] < order[i] else 0.0
        tok = np.concatenate([np.arange(j * TB, (j + 1) * TB) for j in order])
        ropek = np.stack([cos32[:, tok], sin32[:, tok]], 0)
        ropeq = np.stack([cos32[:, tok[:NOWN * TB]] * qs, sin32[:, tok[:NOWN * TB]] * qs], 0)
        m = dict(shared)
        m.update({
            "xm": np.ascontiguousarray(xm_), "xh": np.ascontiguousarray(xh_), "xh2": np.ascontiguousarray(xh2_),
            "c_pc": _pc(cvec[b], 8), "fl_swa": fl_swa, "fl_conv": fl_conv, "fl_oth": fl_oth,
            "ropek": np.ascontiguousarray(ropek, dtype=np.float32),
            "ropeq": np.ascontiguousarray(ropeq, dtype=np.float32),
        })
        maps.append(m)
    return maps, orders


def kernel(**inputs):
    x = np.asarray(inputs["x"])
    B = x.shape[0]
    maps, orders = _host_inputs(inputs)
    nc = build(False)
    res = run_bass_kernel_spmd(nc, maps, core_ids=list(range(len(maps))))
    out = np.zeros((B, S, D), np.float32)
    for core, (b, order) in enumerate(orders):
        y = res.results[core]["y"]
        for i in range(NOWN):
            j = order[i]
            out[b, j * TB:(j + 1) * TB, :] = y[i * TB:(i + 1) * TB, :]
    return out
```
